# Optimizing a Trainium2 kernel written in Bass

```python
import jax, jax.numpy as jnp
from jax import lax
import numpy as np

D_MODEL = 1024
BATCH = 8
SEQ = 4096
DEPTH = 1

GRID_W = 64
CTX_LEN = 256

POOL_GROUPS = 4
POOL_WINDOWS = (2, 4, 8, 16)
POOL_WIDTH = D_MODEL // 2
POOL_GROUP_W = POOL_WIDTH // POOL_GROUPS

MLSTM_WIDTH = D_MODEL
MLSTM_HEADS = 4
MLSTM_HEAD_DIM = MLSTM_WIDTH // MLSTM_HEADS
MLSTM_CHUNK = 64
CONV_W = 5
N_DIRS = 2
N_BRANCHES = 2

N_EXPERTS = 16
EXPERT_FF = D_MODEL
EC_CAPACITY = 2

NORM_EPS = 1e-6

POOL_OFF = 0
Q_OFF = POOL_OFF + POOL_WIDTH
K_OFF = Q_OFF + MLSTM_WIDTH
V_OFF = K_OFF + MLSTM_WIDTH
O_OFF = V_OFF + MLSTM_WIDTH
IF_OFF = O_OFF + MLSTM_WIDTH
GATE_OFF = IF_OFF + N_DIRS * 2 * MLSTM_HEADS
IN_WIDTH = GATE_OFF + N_BRANCHES * D_MODEL

kernel_name = "hybrid_pool_mlstm_ec_moe_dit"


def rmsnorm(x, g):
    xf = x.astype(jnp.float32)
    y = xf * lax.rsqrt(jnp.mean(xf * xf, axis=-1, keepdims=True) + NORM_EPS)
    return (y * g.astype(jnp.float32)).astype(x.dtype)


def modulate(h, shift, scale):
    return h * (1 + scale) + shift


def short_conv(u, w, b):
    pad = CONV_W // 2
    y = lax.conv_general_dilated(u, w[:, None, :].astype(u.dtype), window_strides=(1,),
                                 padding=[(pad, pad)], dimension_numbers=('NWC', 'WIO', 'NWC'),
                                 feature_group_count=u.shape[-1])
    return y + b.astype(u.dtype)


def grid_box_mean(u, side):
    _, R, W, _ = u.shape
    sat = jnp.cumsum(jnp.cumsum(u.astype(jnp.float32), axis=1), axis=2)
    sat = jnp.pad(sat, ((0, 0), (1, 0), (1, 0), (0, 0)))
    lo, hi = side // 2, side - side // 2
    r = jnp.arange(R)
    col = jnp.arange(W)
    r0, r1 = jnp.clip(r - lo, 0, R), jnp.clip(r + hi, 0, R)
    c0, c1 = jnp.clip(col - lo, 0, W), jnp.clip(col + hi, 0, W)

    def corner(ri, ci):
        return jnp.take(jnp.take(sat, ri, axis=1), ci, axis=2)

    s = corner(r1, c1) - corner(r0, c1) - corner(r1, c0) + corner(r0, c0)
    cnt = ((r1 - r0)[:, None] * (c1 - c0)[None, :]).astype(jnp.float32)
    return (s / cnt[None, :, :, None]).astype(u.dtype)


def pool_branch(u, mix, scale, rows):
    B_, T, _ = u.shape
    g = u.reshape(B_, rows, GRID_W, POOL_GROUPS, POOL_GROUP_W)
    outs = [grid_box_mean(g[..., i, :], s) - g[..., i, :] for i, s in enumerate(POOL_WINDOWS)]
    p = jnp.stack(outs, axis=-2)
    p = jnp.einsum('brwgc,gcd->brwgd', p, mix)
    return p.reshape(B_, T, POOL_WIDTH) * scale


def split_heads(a):
    B_, T, _ = a.shape
    return a.reshape(B_, T, MLSTM_HEADS, MLSTM_HEAD_DIM).transpose(0, 2, 1, 3)


def mlstm_gates(g, b_if):
    B_, T, _ = g.shape
    g = (g.astype(jnp.float32) + b_if.astype(jnp.float32)).reshape(B_, T, N_DIRS, 2, MLSTM_HEADS)
    g = g.transpose(2, 3, 0, 4, 1)
    return g[:, 0], jax.nn.log_sigmoid(g[:, 1])


def rev_time(a):
    return jnp.flip(a, axis=2)


def mlstm_final_state(k, v, log_i, log_f):
    k = k.astype(jnp.float32)
    v = v.astype(jnp.float32)
    F = jnp.cumsum(log_f, axis=-1)
    w = F[..., -1:] - F + log_i
    m = jnp.max(w, axis=-1)
    wk = jnp.exp(w - m[..., None])[..., None] * k
    C = jnp.einsum('bhsd,bhse->bhde', wk, v)
    n = jnp.sum(wk, axis=-2)
    return C, n, m


def mlstm_chunkwise(q, k, v, log_i, log_f, C0, n0, m0):
    out_dtype = q.dtype
    B_, H, T, dh = q.shape
    L = MLSTM_CHUNK
    nc = T // L

    def chunks(a):
        a = a.astype(jnp.float32)
        return jnp.moveaxis(a.reshape(B_, H, nc, L, *a.shape[3:]), 2, 0)

    xs = (chunks(q), chunks(k), chunks(v), chunks(log_i), chunks(log_f))
    lower = jnp.tril(jnp.ones((L, L), dtype=bool))

    def step(carry, inp):
        C, n, m = carry
        qb, kb, vb, li, lf = inp
        b = jnp.cumsum(lf, axis=-1)
        Dm = jnp.where(lower, b[..., :, None] - b[..., None, :] + li[..., None, :], -jnp.inf)
        inter = b + m[..., None]
        mt = jnp.maximum(inter, jnp.max(Dm, axis=-1))
        w_inter = jnp.exp(inter - mt)
        s = jnp.einsum('bhtd,bhsd->bhts', qb, kb) * jnp.exp(Dm - mt[..., None])
        num = w_inter[..., None] * jnp.einsum('bhtd,bhde->bhte', qb, C) + jnp.einsum('bhts,bhse->bhte', s, vb)
        den = w_inter * jnp.einsum('bhtd,bhd->bht', qb, n) + jnp.sum(s, axis=-1)
        h = num / jnp.maximum(jnp.abs(den), jnp.exp(-mt))[..., None]
        g = b[..., -1]
        a = g[..., None] - b + li
        m_new = jnp.maximum(g + m, jnp.max(a, axis=-1))
        decay = jnp.exp(g + m - m_new)
        wk = jnp.exp(a - m_new[..., None])[..., None] * kb
        C_new = decay[..., None, None] * C + jnp.einsum('bhsd,bhse->bhde', wk, vb)
        n_new = decay[..., None] * n + jnp.sum(wk, axis=-2)
        return (C_new, n_new, m_new), h

    _, hs = lax.scan(step, (C0, n0, m0), xs)
    return jnp.moveaxis(hs, 0, 2).reshape(B_, H, T, dh).astype(out_dtype)


def mlstm_context_states(hc, w_in, conv_w, conv_b, b_if):
    k = jax.nn.silu(short_conv(hc @ w_in[:, K_OFF:V_OFF], conv_w[:, MLSTM_WIDTH:], conv_b[MLSTM_WIDTH:]))
    k = split_heads(k) * MLSTM_HEAD_DIM ** -0.5
    v = split_heads(hc @ w_in[:, V_OFF:O_OFF])
    log_i, log_f = mlstm_gates(hc @ w_in[:, IF_OFF:GATE_OFF], b_if)
    fwd = mlstm_final_state(k, v, log_i[0], log_f[0])
    bwd = mlstm_final_state(rev_time(k), rev_time(v), rev_time(log_i[1]), rev_time(log_f[1]))
    return fwd, bwd


def mlstm_branch(proj, conv_w, conv_b, b_if, norm_g, ctx_fwd, ctx_bwd):
    B_, T, _ = proj.shape
    qk = jax.nn.silu(short_conv(proj[..., Q_OFF:V_OFF], conv_w, conv_b))
    q = split_heads(qk[..., :MLSTM_WIDTH])
    k = split_heads(qk[..., MLSTM_WIDTH:]) * MLSTM_HEAD_DIM ** -0.5
    v = split_heads(proj[..., V_OFF:O_OFF])
    log_i, log_f = mlstm_gates(proj[..., IF_OFF:GATE_OFF], b_if)
    h_fwd = mlstm_chunkwise(q, k, v, log_i[0], log_f[0], *ctx_fwd)
    h_bwd = rev_time(mlstm_chunkwise(rev_time(q), rev_time(k), rev_time(v),
                                     rev_time(log_i[1]), rev_time(log_f[1]), *ctx_bwd))
    h = (h_fwd + h_bwd).transpose(0, 2, 1, 3)
    h = rmsnorm(h, norm_g.reshape(MLSTM_HEADS, MLSTM_HEAD_DIM)).reshape(B_, T, MLSTM_WIDTH)
    return h * jax.nn.sigmoid(proj[..., O_OFF:IF_OFF])


def expert_choice_ffn(h, w_router, w_gate, w_up, w_down):
    B_, T, D = h.shape
    cap = EC_CAPACITY * T // N_EXPERTS
    aff = jax.nn.softmax((h @ w_router).astype(jnp.float32), axis=-1)
    top_aff, top_idx = lax.top_k(jnp.swapaxes(aff, 1, 2), cap)
    xe = jax.vmap(lambda hb, ib: hb[ib])(h, top_idx)
    hid = jax.nn.silu(jnp.einsum('becd,edf->becf', xe, w_gate)) * jnp.einsum('becd,edf->becf', xe, w_up)
    ye = jnp.einsum('becf,efd->becd', hid, w_down) * top_aff[..., None].astype(h.dtype)
    return jax.vmap(lambda yb, ib: jnp.zeros((T, D), yb.dtype).at[ib.reshape(-1)].add(yb.reshape(-1, D)))(ye, top_idx)


def setup_inputs(seed: int = 0) -> dict:
    key = jax.random.key(seed)
    ks = jax.random.split(key, 24)
    D = D_MODEL
    f32 = jnp.float32

    def nrm(k, shape, fan_in):
        return jax.random.normal(k, shape, f32) * fan_in ** -0.5

    def gain(k, shape):
        return 1.0 + 0.02 * jax.random.normal(k, shape, f32)

    b_if = 0.1 * jax.random.normal(ks[10], (DEPTH, N_DIRS, 2, MLSTM_HEADS), f32)
    b_if = b_if.at[:, :, 1, :].add(jnp.linspace(3.0, 6.0, MLSTM_HEADS, dtype=f32))
    return {
        "x": jax.random.normal(ks[0], (BATCH, SEQ, D), f32),
        "c": jax.random.normal(ks[1], (BATCH, D), f32),
        "ctx": jax.random.normal(ks[2], (BATCH, CTX_LEN, D), f32),
        "c_ctx": jax.random.normal(ks[3], (D,), f32),
        "w_mod": nrm(ks[4], (DEPTH, D, 6 * D), D),
        "b_mod": 0.02 * jax.random.normal(ks[5], (DEPTH, 6 * D), f32),
        "norm1_g": gain(ks[6], (DEPTH, D)),
        "norm2_g": gain(ks[7], (DEPTH, D)),
        "w_in": nrm(ks[8], (DEPTH, D, IN_WIDTH), D),
        "conv_w": nrm(ks[9], (DEPTH, CONV_W, 2 * MLSTM_WIDTH), CONV_W),
        "conv_b": 0.02 * jax.random.normal(ks[11], (DEPTH, 2 * MLSTM_WIDTH), f32),
        "b_if": b_if.reshape(DEPTH, N_DIRS * 2 * MLSTM_HEADS),
        "pool_mix": nrm(ks[12], (DEPTH, POOL_GROUPS, POOL_GROUP_W, POOL_GROUP_W), POOL_GROUP_W),
        "pool_scale": 1.0 + 0.1 * jax.random.normal(ks[13], (DEPTH, POOL_WIDTH), f32),
        "mlstm_norm_g": gain(ks[14], (DEPTH, MLSTM_WIDTH)),
        "w_pool_out": nrm(ks[15], (DEPTH, POOL_WIDTH, D), POOL_WIDTH),
        "w_mlstm_out": nrm(ks[16], (DEPTH, MLSTM_WIDTH, D), MLSTM_WIDTH),
        "w_out": nrm(ks[17], (DEPTH, D, D), D),
        "w_router": nrm(ks[18], (DEPTH, D, N_EXPERTS), D),
        "w_gate": nrm(ks[19], (DEPTH, N_EXPERTS, D, EXPERT_FF), D),
        "w_up": nrm(ks[20], (DEPTH, N_EXPERTS, D, EXPERT_FF), D),
        "w_down": nrm(ks[21], (DEPTH, N_EXPERTS, EXPERT_FF, D), EXPERT_FF),
        "final_g": gain(ks[22], (D,)),
    }


def reference(x, c, ctx, c_ctx, w_mod, b_mod, norm1_g, norm2_g, w_in, conv_w, conv_b, b_if,
              pool_mix, pool_scale, mlstm_norm_g, w_pool_out, w_mlstm_out, w_out,
              w_router, w_gate, w_up, w_down, final_g):
    D = D_MODEL
    rows = x.shape[1] // GRID_W
    for l in range(DEPTH):
        mod = jax.nn.silu(c) @ w_mod[l] + b_mod[l]
        shift1, scale1, gate1, shift2, scale2, gate2 = jnp.split(mod[:, None, :], 6, axis=-1)
        mod_c = jax.nn.silu(c_ctx) @ w_mod[l] + b_mod[l]

        hc = modulate(rmsnorm(ctx, norm1_g[l]), mod_c[:D], mod_c[D:2 * D])
        ctx_fwd, ctx_bwd = mlstm_context_states(hc, w_in[l], conv_w[l], conv_b[l], b_if[l])

        hx = modulate(rmsnorm(x, norm1_g[l]), shift1, scale1)
        proj = hx @ w_in[l]
        a = pool_branch(proj[..., POOL_OFF:Q_OFF], pool_mix[l], pool_scale[l], rows)
        m = mlstm_branch(proj, conv_w[l], conv_b[l], b_if[l], mlstm_norm_g[l], ctx_fwd, ctx_bwd)
        g = jax.nn.sigmoid(proj[..., GATE_OFF:])
        mixed = g[..., :D] * (a @ w_pool_out[l]) + g[..., D:] * (m @ w_mlstm_out[l])
        x = x + gate1 * (mixed @ w_out[l])

        h2 = modulate(rmsnorm(x, norm2_g[l]), shift2, scale2)
        x = x + gate2 * expert_choice_ffn(h2, w_router[l], w_gate[l], w_up[l], w_down[l])
    return rmsnorm(x, final_g)
```

```python
import contextlib
import numpy as np
import concourse.bass as bass
import concourse.mybir as mybir

F32 = mybir.dt.float32
BF16 = mybir.dt.bfloat16
I32 = mybir.dt.int32
AF = mybir.ActivationFunctionType
ALU = mybir.AluOpType
AX = mybir.AxisListType

ENGS = ["pe", "act", "dve", "pool", "sp"]


class Buf:
    __slots__ = ("name", "last_w", "readers", "sync_all")

    def __init__(self, name="", sync_all=False):
        self.name = name
        self.last_w = None
        self.readers = []
        self.sync_all = sync_all


class _Proxy:
    def __init__(self):
        self.call = None

    def __getattr__(self, name):
        def f(*a, **k):
            self.call = (name, a, k)
            return None
        return f


class Rec:
    __slots__ = ("eng", "fn", "deps", "dma", "dma_id", "signal", "sigval", "pos")

    def __init__(self, eng, fn, dma):
        self.eng = eng
        if fn is not None:
            pr = _Proxy()
            fn(pr)
            fn = pr.call
        self.fn = fn
        self.deps = []
        self.dma = dma
        self.dma_id = -1
        self.signal = False
        self.sigval = 0
        self.pos = 0


class Sched:
    def __init__(self, nc, n_dma_sems=48):
        self.nc = nc
        self.streams = {e: [] for e in ENGS}
        self.NS = n_dma_sems
        self.dmas = []
        self.all_out_dmas = []

    def op(self, eng, fn, reads=(), writes=(), dma=False, after=()):
        rec = Rec(eng, fn, dma)
        for r_ in after:
            rec.deps.append((r_, "x"))
        for b in reads:
            if b.last_w is not None:
                rec.deps.append((b.last_w, "raw"))
        for b in writes:
            if b.last_w is not None:
                rec.deps.append((b.last_w, "x" if b.sync_all else "waw"))
            for r in b.readers:
                rec.deps.append((r, "war"))
        if dma:
            rec.dma_id = len(self.dmas)
            if rec.dma_id >= self.NS:
                rec.deps.append((self.dmas[rec.dma_id - self.NS], "x"))
            self.dmas.append(rec)
        for b in reads:
            b.readers.append(rec)
        for b in writes:
            b.last_w = rec
            b.readers = []
        rec.pos = len(self.streams[eng])
        self.streams[eng].append(rec)
        return rec

    def pe(self, fn, reads=(), writes=()):
        return self.op("pe", fn, reads, writes)

    def act(self, fn, reads=(), writes=()):
        return self.op("act", fn, reads, writes)

    def dve(self, fn, reads=(), writes=()):
        return self.op("dve", fn, reads, writes)

    def pool(self, fn, reads=(), writes=()):
        return self.op("pool", fn, reads, writes)

    def dma(self, q, out, in_, reads=(), writes=(), **kw):
        return self.op(q, lambda e: e.dma_start(out=out, in_=in_, **kw), reads, writes, dma=True)

    def fence(self):
        lasts = []
        for e in ENGS:
            for r in reversed(self.streams[e]):
                if not r.dma and r.fn is not None:
                    lasts.append(r)
                    break
        pend = list(self.dmas[-self.NS:])
        for e in ENGS:
            rec = Rec(e, None, False)
            for r in lasts:
                rec.deps.append((r, "x"))
            for r in pend:
                rec.deps.append((r, "x"))
            rec.pos = len(self.streams[e])
            self.streams[e].append(rec)

    def emit(self):
        nc = self.nc
        for e in ENGS:
            for rec in self.streams[e]:
                for (d, kind) in rec.deps:
                    if d.dma:
                        continue
                    if d.eng == rec.eng:
                        if rec.dma:
                            d.signal = True
                        elif d.eng == "pe":
                            continue
                        elif kind in ("raw", "x") or d.eng == "pool":
                            d.signal = True
                    else:
                        d.signal = True
        for e in ENGS:
            c = 0
            for rec in self.streams[e]:
                if rec.signal and not rec.dma:
                    c += 1
                    rec.sigval = c
        with contextlib.ExitStack() as es:
            esem = {e: es.enter_context(nc.semaphore("S_" + e)) for e in ENGS}
            dsem = [es.enter_context(nc.semaphore("D%d" % i)) for i in range(self.NS)]
            block = es.enter_context(nc.Block())
            hw = {"pe": "tensor", "act": "scalar", "dve": "vector", "pool": "gpsimd", "sp": "sync"}

            def make(ename):
                stream = self.streams[ename]

                def body(eng):
                    seen = {}
                    for rec in stream:
                        need = {}
                        for (d, kind) in rec.deps:
                            if d.dma:
                                key = ("d", d.dma_id % self.NS)
                                val = 16 * (d.dma_id // self.NS + 1)
                            else:
                                if d.eng == rec.eng and not rec.dma:
                                    if d.eng == "pe" or (kind not in ("raw", "x") and d.eng != "pool"):
                                        continue
                                key = ("e", d.eng)
                                val = d.sigval
                            if need.get(key, 0) < val:
                                need[key] = val
                        for key, val in need.items():
                            if seen.get(key, 0) >= val:
                                continue
                            seen[key] = val
                            sem = dsem[key[1]] if key[0] == "d" else esem[key[1]]
                            eng.wait_ge(sem, val)
                        if rec.fn is None:
                            continue
                        name, a_, k_ = rec.fn
                        ins = getattr(eng, name)(*a_, **k_)
                        if rec.dma:
                            ins.then_inc(dsem[rec.dma_id % self.NS], 16)
                        elif rec.signal:
                            ins.then_inc(esem[ename], 1)
                return body

            for ename in ENGS:
                if not self.streams[ename]:
                    continue
                getattr(block, hw[ename])(make(ename))


from concourse.bass_utils import run_bass_kernel_spmd

D = 1024
T = 4096
CT = 256
NT = 32
NS_ = 34
SEQ = 4352
INW = 6672
POOL_OFF, Q_OFF, K_OFF, V_OFF, O_OFF, IF_OFF, GATE_OFF = 0, 512, 1536, 2560, 3584, 4608, 4624
NE = 16
CAP = 512
EPS = 1e-6
WIN_BLOCKS = [(0, 1536), (1536, 2560), (2560, 3584), (3584, 4624), (4624, 5648), (5648, 6672)]


def win_buf(K, c0):
    for i, (a, b) in enumerate(WIN_BLOCKS):
        if a <= c0 < b:
            return K.Bwin[i]
    raise ValueError(c0)

C_ID, C_TRI, C_TRIT, C_MSUM, C_ONES = 0, 128, 256, 384, 512
C_IOJ = 640
C_TIDX = 1152
C_IOP = 1664
C_SEL = 1665
NCONST = 1665 + 512


def make_consts():
    c = np.zeros((128, NCONST), np.float32)
    p = np.arange(128)
    c[:, C_ID:C_ID + 128] = np.eye(128)
    c[:, C_TRI:C_TRI + 128] = (p[:, None] <= p[None, :])
    c[:, C_TRIT:C_TRIT + 128] = (p[:, None] >= p[None, :])
    c[:, C_MSUM:C_MSUM + 128] = ((p[:, None] % 16) == (p[None, :] % 16))
    c[:, C_ONES:C_ONES + 128] = 1.0
    c[:, C_IOJ:C_IOJ + 512] = np.arange(512)[None, :]
    c[:, C_TIDX:C_TIDX + 512] = (np.arange(512) // 16)[None, :]
    c[:, C_IOP] = p
    for h in range(4):
        c[h, C_SEL + h * 128:C_SEL + (h + 1) * 128] = 1.0
    return c


def make_invcnt():
    out = np.zeros((4, 64, 64), np.float32)
    for gi, s in enumerate((2, 4, 8, 16)):
        lo, hi = s // 2, s - s // 2
        r = np.arange(64)
        r0, r1 = np.clip(r - lo, 0, 64), np.clip(r + hi, 0, 64)
        cnt = (r1 - r0)[:, None] * (r1 - r0)[None, :]
        out[gi] = 1.0 / cnt
    return out.reshape(4, 4096)


class Ctx:
    pass


def build(stop_after=99, debug=False):
    nc = bass.Bass("TRN2", target_bir_lowering=False)
    S = Sched(nc, n_dma_sems=56)
    K = Ctx()
    K.nc, K.S = nc, S

    def din(name, shape, dt=F32):
        return nc.dram_tensor(name, list(shape), dt, kind="ExternalInput").ap()

    def dscr(name, shape, dt):
        if debug:
            return nc.dram_tensor(name, list(shape), dt, kind="ExternalOutput").ap()
        return nc.dram_tensor(name, list(shape), dt).ap()

    I = {}
    I["x"] = din("x", [T, D]); I["c"] = din("c", [D]); I["ctx"] = din("ctx", [CT, D]); I["c_ctx"] = din("c_ctx", [D])
    I["w_mod"] = din("w_mod", [D, 6 * D]); I["b_mod"] = din("b_mod", [6 * D])
    I["norm1_g"] = din("norm1_g", [D]); I["norm2_g"] = din("norm2_g", [D])
    I["w_in"] = din("w_in", [D, INW]); I["conv_w"] = din("conv_w", [5, 2 * D]); I["conv_b"] = din("conv_b", [2 * D])
    I["b_if"] = din("b_if", [16]); I["pool_mix"] = din("pool_mix", [4, 128, 128]); I["pool_scale"] = din("pool_scale", [512])
    I["mlstm_norm_g"] = din("mlstm_norm_g", [D]); I["w_pool_out"] = din("w_pool_out", [512, D])
    I["w_mlstm_out"] = din("w_mlstm_out", [D, D]); I["w_out"] = din("w_out", [D, D]); I["w_router"] = din("w_router", [D, NE])
    I["w_gate"] = din("w_gate", [NE, D, D]); I["w_up"] = din("w_up", [NE, D, D]); I["w_down"] = din("w_down", [NE, D, D])
    I["final_g"] = din("final_g", [D]); I["consts"] = din("consts", [128, NCONST]); I["invcnt"] = din("invcnt", [4, 4096])
    out_d = nc.dram_tensor("out", [T, D], F32, kind="ExternalOutput").ap()

    Dm = {}
    Dm["u_pool"] = dscr("u_pool", [512, T], F32)
    Dm["qk_raw"] = dscr("qk_raw", [2048, T], BF16)
    Dm["kc_raw"] = dscr("kc_raw", [1024, CT], BF16)
    Dm["qk_act"] = dscr("qk_act", [2048, T], BF16)
    Dm["kc_act"] = dscr("kc_act", [1024, CT], BF16)
    Dm["v_tok"] = dscr("v_tok", [SEQ, D], BF16)
    Dm["sigo"] = dscr("sigo", [D, T], BF16)
    Dm["sigg"] = dscr("sigg", [2 * D, T], BF16)
    Dm["gates"] = dscr("gates", [4, 4, SEQ], F32)
    Dm["bvec"] = dscr("bvec", [4, D], F32)
    Dm["h_f"] = dscr("h_f", [T, D], F32)
    Dm["hn"] = dscr("hn", [T, D], BF16)
    Dm["x1"] = dscr("x1", [T, D], F32)
    Dm["h2"] = dscr("h2", [T + 128, D], BF16)
    Dm["yacc"] = dscr("yacc", [T + 128, D], F32)
    DB = {k: Buf("d_" + k) for k in Dm}
    dbg = {}
    if debug:
        dbg["modT"] = nc.dram_tensor("dbg_modT", [128, 96], F32, kind="ExternalOutput").ap()
        dbg["tokS"] = nc.dram_tensor("dbg_tokS", [128, 2 * 3 * 34 * 4], F32, kind="ExternalOutput").ap()
        dbg["dec"] = nc.dram_tensor("dbg_dec", [128, 2 * 4 * 35], F32, kind="ExternalOutput").ap()

    with contextlib.ExitStack() as es0:
        def sbp(name, shape, dt):
            return es0.enter_context(nc.sbuf_tensor(name, list(shape), dt))
        cst = sbp("cst", [128, NCONST], F32); Bcst = Buf("cst")
        identb = sbp("identb", [128, 128], BF16); trib = sbp("trib", [128, 128], BF16)
        tritb = sbp("tritb", [128, 128], BF16); onesb = sbp("onesb", [128, 128], BF16)
        Bcb = Buf("cstb")
        modT = sbp("modT", [128, 48, 2], F32); Bmod = Buf("modT")
        vecs = sbp("vecs", [128, 8, 8], F32); Bvec = Buf("vecs")
        rstd1 = sbp("rstd1", [128, NS_], F32); Brstd1 = Buf("rstd1")
        rstd2 = sbp("rstd2", [128, NT], F32); Brstd2 = Buf("rstd2")
        tokS = sbp("tokS", [128, 2, 3, NS_, 4], F32); BtokS = Buf("tokS")
        decb = sbp("decb", [128, 2, 4, NS_ + 1], F32); Bdec = Buf("decb")
        ps_all = [es0.enter_context(nc.psum_tensor("ps%d" % i, [128, 512], F32)) for i in range(8)]
        Bps_all = [Buf("ps%d" % i) for i in range(8)]
        psF = ps_all[0:6]
        psB = [ps_all[6][:].bitcast(BF16), ps_all[7][:].bitcast(BF16)]
        BpsF = Bps_all[0:6]
        BpsB = Bps_all[6:8]
        rr = {"f": 0, "b": 0}

        def nextF():
            i = rr["f"]; rr["f"] = (i + 1) % 6
            return psF[i], BpsF[i]

        def nextB():
            i = rr["b"]; rr["b"] = (i + 1) % 2
            return psB[i], BpsB[i]

        S.dma("sp", cst[:], I["consts"], writes=[Bcst])
        S.dve(lambda e: e.tensor_copy(out=identb[:], in_=cst[:, C_ID:C_ID + 128]), reads=[Bcst], writes=[Bcb])
        S.dve(lambda e: e.tensor_copy(out=trib[:], in_=cst[:, C_TRI:C_TRI + 128]), reads=[Bcst], writes=[Bcb])
        S.dve(lambda e: e.tensor_copy(out=tritb[:], in_=cst[:, C_TRIT:C_TRIT + 128]), reads=[Bcst], writes=[Bcb])
        S.dve(lambda e: e.tensor_copy(out=onesb[:], in_=cst[:, C_ONES:C_ONES + 128]), reads=[Bcst], writes=[Bcb])
        identf = cst[:, C_ID:C_ID + 128]
        vpA = sbp("vpA", [128, 108], F32); vpB = sbp("vpB", [128, 80], F32); Bvp = Buf("vp")
        with contextlib.ExitStack() as esv:
            VA = esv.enter_context(nc.sbuf_tensor("VA", [108, 128], F32)); VB = esv.enter_context(nc.sbuf_tensor("VB", [80, 128], F32))
            BVA = Buf("VA"); BVB = Buf("VB")
            def rows(ap):
                return ap.rearrange("(k p) -> k p", p=128)
            for (r0, r1, src) in ((0, 8, rows(I["c"])), (8, 16, rows(I["c_ctx"])), (16, 64, rows(I["b_mod"])), (64, 72, rows(I["norm1_g"])),
                                  (72, 80, rows(I["norm2_g"])), (80, 84, rows(I["pool_scale"])), (84, 92, rows(I["mlstm_norm_g"])),
                                  (92, 108, rows(I["conv_b"]))):
                S.dma("sp", VA[r0:r1, :], src, writes=[BVA])
            S.dma("sp", VB[:, :], I["conv_w"].rearrange("j (c p) -> (j c) p", p=128), writes=[BVB])
            pvA, BpvA = nextF()
            S.pe(lambda e: e.transpose(out=pvA[:, 0:108], in_=VA[:, :], identity=cst[0:108, C_ID:C_ID + 108]), reads=[BVA, Bcst], writes=[BpvA])
            S.dve(lambda e: e.tensor_copy(out=vpA[:], in_=pvA[:, 0:108]), reads=[BpvA], writes=[Bvp])
            pvB, BpvB = nextF()
            S.pe(lambda e: e.transpose(out=pvB[:, 0:80], in_=VB[:, :], identity=cst[0:80, C_ID:C_ID + 80]), reads=[BVB, Bcst], writes=[BpvB])
            S.dve(lambda e: e.tensor_copy(out=vpB[:], in_=pvB[:, 0:80]), reads=[BpvB], writes=[Bvp])
            S.fence()

        def dump(name, ap, bufs):
            if not debug:
                return
            t_ = nc.dram_tensor("dbg_" + name, list(ap.shape), ap.dtype, kind="ExternalOutput").ap()
            S.dma("sp", t_, ap, reads=bufs)
        es_win = contextlib.ExitStack()
        win = es_win.enter_context(nc.sbuf_tensor("win", [128, 8, INW], BF16)); Bwin = [Buf("win%d" % i) for i in range(len(WIN_BLOCKS))]
        K.__dict__.update(locals())
        phase0(K)
        if stop_after >= 1:
            phase1(K)
        es_win.close()
        if stop_after >= 3:
            phase23(K)
        if stop_after >= 4:
            phase4(K)
        if stop_after >= 5:
            phase5(K)
        if stop_after >= 6:
            phase6(K)
        if debug:
            S.dma("sp", dbg["modT"], modT[:].rearrange("p a b -> p (a b)"), reads=[Bmod])
            S.dma("sp", dbg["tokS"], tokS[:].rearrange("p a b c d -> p (a b c d)"), reads=[BtokS])
            S.dma("sp", dbg["dec"], decb[:].rearrange("p a b c -> p (a b c)"), reads=[Bdec])
        S.fence()
        S.emit()
    return nc


def phase0(K):
    nc, S, I = K.nc, K.S, K.I
    with contextlib.ExitStack() as es:
        def sb(name, shape, dt):
            return es.enter_context(nc.sbuf_tensor(name, list(shape), dt))
        NCH = 3
        CW = 6 * D // NCH
        wmb = [sb("wm%d" % i, [128, 8, CW], BF16) for i in range(2)]; Bwmb = [Buf("wm") for i in range(2)]
        cs = sb("cs", [128, 8, 2], BF16); Bcs = Buf("cs")
        vpA, Bvp = K.vpA, K.Bvp
        bm = vpA[:, 16:64]; Bbm = Bvp
        Bg = Bvp
        S.act(lambda e: e.activation(out=cs[:, :, 0], in_=vpA[:, 0:8], func=AF.Silu), reads=[Bvp], writes=[Bcs])
        S.act(lambda e: e.activation(out=cs[:, :, 1], in_=vpA[:, 8:16], func=AF.Silu), reads=[Bvp], writes=[Bcs])
        pm, Bpm = K.nextF()
        for ch in range(NCH):
            wb, Bwb = wmb[ch % 2], Bwmb[ch % 2]
            S.dma("pool", wb[:], I["w_mod"][:, ch * CW:(ch + 1) * CW].rearrange("(k p) n -> p k n", p=128), writes=[Bwb])
            if ch == NCH - 1:
                for bi_, (c0, c1) in enumerate(WIN_BLOCKS):
                    S.dma("pool", K.win[:, :, c0:c1], I["w_in"][:, c0:c1].rearrange("(k p) n -> p k n", p=128), writes=[K.Bwin[bi_]])
            for ocl in range(CW // 128):
                oc = ch * (CW // 128) + ocl
                for k in range(8):
                    S.pe(lambda e, oc=oc, k=k, ocl=ocl, wb=wb: e.matmul(pm[:, oc * 2:oc * 2 + 2], lhsT=wb[:, k, ocl * 128:(ocl + 1) * 128],
                                                                         rhs=cs[:, k, :], start=(k == 0), stop=(k == 7)),
                         reads=[Bwb, Bcs], writes=[Bpm])
        modT, Bmod, vecs, Bvec = K.modT, K.Bmod, K.vecs, K.Bvec
        for col in range(2):
            S.dve(lambda e, col=col: e.tensor_tensor(out=modT[:, :, col], in0=pm[:, col:96:2], in1=bm, op=ALU.add),
                  reads=[Bpm, Bbm], writes=[Bmod])
        def mv(v, col):
            return modT[:, v * 8:(v + 1) * 8, col]
        for (kind, col) in ((0, 0), (2, 1)):
            S.dve(lambda e, kind=kind, col=col: e.scalar_tensor_tensor(out=vecs[:, kind, :], in0=mv(1, col), scalar=1.0, in1=vpA[:, 64:72],
                                                                        op0=ALU.add, op1=ALU.mult), reads=[Bmod, Bg], writes=[Bvec])
            S.dve(lambda e, kind=kind, col=col: e.tensor_copy(out=vecs[:, kind + 1, :], in_=mv(0, col)), reads=[Bmod], writes=[Bvec])
        S.dve(lambda e: e.tensor_copy(out=vecs[:, 4, :], in_=mv(2, 0)), reads=[Bmod], writes=[Bvec])
        S.dve(lambda e: e.scalar_tensor_tensor(out=vecs[:, 5, :], in0=mv(4, 0), scalar=1.0, in1=vpA[:, 72:80], op0=ALU.add, op1=ALU.mult),
              reads=[Bmod, Bg], writes=[Bvec])
        S.dve(lambda e: e.tensor_copy(out=vecs[:, 6, :], in_=mv(3, 0)), reads=[Bmod], writes=[Bvec])
        S.dve(lambda e: e.tensor_copy(out=vecs[:, 7, :], in_=mv(5, 0)), reads=[Bmod], writes=[Bvec])
        vrow = sb("vrow", [8, 4, 128], F32); Bvrow = Buf("vrow")
        pv, Bpv = K.nextF()
        for r, kind in enumerate((4, 5, 6, 7)):
            S.pe(lambda e, r=r, kind=kind: e.transpose(out=pv[0:8, r * 128:(r + 1) * 128], in_=vecs[:, kind, :], identity=K.cst[:, C_ID:C_ID + 128]),
                 reads=[Bvec, K.Bcst], writes=[Bpv])
        S.dve(lambda e: e.tensor_copy(out=vrow[:], in_=pv[0:8, 0:512].rearrange("k (r p) -> k r p", p=128)), reads=[Bpv], writes=[Bvrow])
        for r in range(4):
            S.dma("pool", K.Dm["bvec"][r].rearrange("(k p) -> k p", p=128), vrow[:, r, :], reads=[Bvrow])
        S.fence()


def phase1(K):
    nc, S, I, Dm, DB = K.nc, K.S, K.I, K.Dm, K.DB
    vecs, Bvec = K.vecs, K.Bvec
    with contextlib.ExitStack() as es:
        def sb(name, shape, dt):
            return es.enter_context(nc.sbuf_tensor(name, list(shape), dt))
        win, Bwin = K.win, K.Bwin
        xg = [[sb("xg%d_%d" % (a, i), [128, D], F32) for i in range(4)] for a in range(2)]
        Bxg = [[Buf("xg") for i in range(4)] for a in range(2)]
        junk = sb("junk1", [128, D], F32); Bjunk = Buf("junk1", sync_all=True)
        ssq = [sb("ssq1_%d" % a, [128, 4], F32) for a in range(2)]; Bssq = [Buf("ssq1") for a in range(2)]
        xn = [sb("xn%d" % i, [128, D], BF16) for i in range(2)]; Bxn = [Buf("xn%d" % i) for i in range(2)]
        hxT = [sb("hxT%d" % i, [128, 8, 512], BF16) for i in range(2)]; BhxT = [Buf("hxT%d" % i) for i in range(2)]
        stF = [sb("stF%d" % i, [128, 512], F32) for i in range(2)]; BstF = [Buf("stF%d" % i) for i in range(2)]
        stB = [sb("stB%d" % i, [128, 512], BF16) for i in range(6)]; BstB = [Buf("stB%d" % i) for i in range(6)]
        cnt = {"x": 0, "f": 0, "b": 0}

        def xsrc(c):
            return I["ctx"][c * 128:(c + 1) * 128, :] if c < 2 else I["x"][(c - 2) * 128:(c - 1) * 128, :]

        def stage_b():
            i = cnt["b"] % 6; cnt["b"] += 1
            return stB[i], BstB[i]

        def stage_f():
            i = cnt["f"] % 2; cnt["f"] += 1
            return stF[i], BstF[i]

        groups = [("ctx", [0, 1])] + [("lat", [2 + 4 * g + t for t in range(4)]) for g in range(8)]

        def prep(gi):
            a = gi % 2
            tiles_ = groups[gi][1]
            S.dve(lambda e: e.memset(ssq[a][:], 0.0), writes=[Bssq[a]])
            for ti, c in enumerate(tiles_):
                S.dma("sp", xg[a][ti][:], xsrc(c), writes=[Bxg[a][ti]])
                S.act(lambda e, ti=ti: e.activation(out=junk[:], in_=xg[a][ti][:], func=AF.Square, accum_out=ssq[a][:, ti:ti + 1]),
                      reads=[Bxg[a][ti]], writes=[Bssq[a], Bjunk])
            n_ = len(tiles_)
            S.dve(lambda e: e.tensor_scalar(out=ssq[a][:, 0:n_], in0=ssq[a][:, 0:n_], scalar1=1.0 / D, scalar2=EPS, op0=ALU.mult, op1=ALU.add),
                  reads=[Bssq[a]], writes=[Bssq[a]])
            S.act(lambda e: e.activation(out=ssq[a][:, 0:n_], in_=ssq[a][:, 0:n_], func=AF.Sqrt), reads=[Bssq[a]], writes=[Bssq[a]])
            S.dve(lambda e: e.reciprocal(out=K.rstd1[:, tiles_[0]:tiles_[0] + n_], in_=ssq[a][:, 0:n_]), reads=[Bssq[a]], writes=[K.Brstd1])

        def make_hx(gi):
            gkind, tiles = groups[gi]
            hb, Bhb = hxT[gi % 2], BhxT[gi % 2]
            kind = 2 if gkind == "ctx" else 0
            for ti, c in enumerate(tiles):
                xb, Bxb = xn[c % 2], Bxn[c % 2]
                S.dve(lambda e, c=c, xb=xb, ti=ti: e.tensor_scalar(out=xb[:], in0=xg[gi % 2][ti][:], scalar1=K.rstd1[:, c:c + 1], scalar2=None,
                                                                 op0=ALU.mult), reads=[Bxg[gi % 2][ti], K.Brstd1], writes=[Bxb])
                pt, Bpt = K.nextB()
                for j in range(8):
                    S.pe(lambda e, j=j, xb=xb, pt=pt: e.transpose(out=pt[:, j * 128:(j + 1) * 128], in_=xb[:, j * 128:(j + 1) * 128],
                                                                   identity=K.identb[:]), reads=[Bxb, K.Bcb], writes=[Bpt])
                for j in range(8):
                    S.act(lambda e, j=j, pt=pt, hb=hb, ti=ti: e.activation(out=hb[:, j, ti * 128:(ti + 1) * 128], in_=pt[:, j * 128:(j + 1) * 128],
                                                                            func=AF.Identity, scale=vecs[:, kind, j:j + 1], bias=vecs[:, kind + 1, j:j + 1]),
                          reads=[Bpt, Bvec], writes=[Bhb])

        prep(0)
        prep(1)
        make_hx(0)
        for gi, (gkind, tiles) in enumerate(groups):
            N = 128 * len(tiles)
            hb, Bhb = hxT[gi % 2], BhxT[gi % 2]
            seq0 = tiles[0] * 128
            tok0 = seq0 - CT

            def proj_fm(oc, M=128, col0=None):
                ps, Bps = K.nextF()
                c0 = oc * 128 if col0 is None else col0
                for k in range(8):
                    S.pe(lambda e, k=k, ps=ps, c0=c0, M=M: e.matmul(ps[0:M, 0:N], lhsT=win[:, k, c0:c0 + M], rhs=hb[:, k, 0:N],
                                                                       start=(k == 0), stop=(k == 7)), reads=[win_buf(K, c0), Bhb], writes=[Bps])
                return ps, Bps

            if gkind == "lat":
                for oc in range(4):
                    ps, Bps = proj_fm(oc)
                    st, Bst = stage_f()
                    S.dve(lambda e, ps=ps, st=st: e.tensor_copy(out=st[:, 0:N], in_=ps[:, 0:N]), reads=[Bps], writes=[Bst])
                    S.dma("pool", Dm["u_pool"][oc * 128:(oc + 1) * 128, tok0:tok0 + N], st[:, 0:N], reads=[Bst])
            qk_chunks = range(4, 20) if gkind == "lat" else range(12, 20)
            for oc in qk_chunks:
                ps, Bps = proj_fm(oc)
                st, Bst = stage_b()
                if oc % 2 == 0:
                    S.dve(lambda e, ps=ps, st=st: e.tensor_copy(out=st[:, 0:N], in_=ps[:, 0:N]), reads=[Bps], writes=[Bst])
                else:
                    S.act(lambda e, ps=ps, st=st: e.activation(out=st[:, 0:N], in_=ps[:, 0:N], func=AF.Identity), reads=[Bps], writes=[Bst])
                if gkind == "lat":
                    S.dma("pool", Dm["qk_raw"][(oc - 4) * 128:(oc - 3) * 128, tok0:tok0 + N], st[:, 0:N], reads=[Bst])
                else:
                    S.dma("pool", Dm["kc_raw"][(oc - 12) * 128:(oc - 11) * 128, 0:N], st[:, 0:N], reads=[Bst])
            if gi + 1 < len(groups):
                make_hx(gi + 1)
            if gi + 2 < len(groups):
                prep(gi + 2)
            for ti, c in enumerate(tiles):
                for half in range(2):
                    ps, Bps = K.nextF()
                    for k in range(8):
                        S.pe(lambda e, k=k, ps=ps, ti=ti, half=half: e.matmul(ps[:, :], lhsT=hb[:, k, ti * 128:(ti + 1) * 128],
                                                                             rhs=win[:, k, V_OFF + half * 512:V_OFF + (half + 1) * 512],
                                                                             start=(k == 0), stop=(k == 7)), reads=[win_buf(K, V_OFF + half * 512), Bhb], writes=[Bps])
                    st, Bst = stage_b()
                    S.dve(lambda e, ps=ps, st=st: e.tensor_copy(out=st[:], in_=ps[:]), reads=[Bps], writes=[Bst])
                    S.dma("pool", Dm["v_tok"][c * 128:(c + 1) * 128, half * 512:(half + 1) * 512], st[:], reads=[Bst])
            for q in range(4):
                ps, Bps = proj_fm(0, M=4, col0=IF_OFF + q * 4)
                st, Bst = stage_f()
                S.dve(lambda e, ps=ps, st=st: e.tensor_copy(out=st[0:4, 0:N], in_=ps[0:4, 0:N]), reads=[Bps], writes=[Bst])
                S.dma("pool", Dm["gates"][q, :, seq0:seq0 + N], st[0:4, 0:N], reads=[Bst])
            if gkind == "lat":
                for oc in range(28, 52):
                    ps, Bps = proj_fm(oc, col0=(O_OFF + (oc - 28) * 128) if oc < 36 else (GATE_OFF + (oc - 36) * 128))
                    st, Bst = stage_b()
                    S.act(lambda e, ps=ps, st=st: e.activation(out=st[:, 0:N], in_=ps[:, 0:N], func=AF.Sigmoid), reads=[Bps], writes=[Bst])
                    if oc < 36:
                        S.dma("pool", Dm["sigo"][(oc - 28) * 128:(oc - 27) * 128, tok0:tok0 + N], st[:, 0:N], reads=[Bst])
                    else:
                        S.dma("pool", Dm["sigg"][(oc - 36) * 128:(oc - 35) * 128, tok0:tok0 + N], st[:, 0:N], reads=[Bst])
        S.fence()


def phase2_gen(K):
    nc, S, I, Dm = K.nc, K.S, K.I, K.Dm
    es = K.es23
    if True:
        def sb(name, shape, dt):
            return es.enter_context(nc.sbuf_tensor(name, list(shape), dt))
        Bcw = K.Bvp; Bcbias = K.Bvp
        cb = K.vpA[:, 92:108]

        class _CW:
            def __getitem__(self, key):
                _, cc, js = key
                return K.vpB[:, js.start * 16 + cc:js.start * 16 + cc + 1]
        cw = _CW()
        dg = sb("dg", [128, 16, 5, 128], BF16); Bdg = Buf("dg")
        xp = [sb("xp%d" % i, [128, T + 4], BF16) for i in range(2)]; Bxp = [Buf("xp%d" % i) for i in range(2)]
        xc = [sb("xpc%d" % i, [128, CT + 4], BF16) for i in range(2)]; Bxc = [Buf("xpc%d" % i) for i in range(2)]
        so = [sb("so%d" % i, [128, T], BF16) for i in range(2)]; Bso = [Buf("so%d" % i) for i in range(2)]
        soc = [sb("soc%d" % i, [128, CT], BF16) for i in range(2)]; Bsoc = [Buf("soc%d" % i) for i in range(2)]
        for cc in range(16):
            for j in range(5):
                S.dve(lambda e, cc=cc, j=j: e.tensor_scalar(out=dg[:, cc, j, :], in0=K.cst[:, C_ID:C_ID + 128], scalar1=cw[:, cc, j:j + 1],
                                                             scalar2=None, op0=ALU.mult), reads=[K.Bcst, Bcw], writes=[Bdg])
        for i in range(2):
            S.dve(lambda e, i=i: e.memset(xp[i][:, 0:2], 0.0), writes=[Bxp[i]])
            S.dve(lambda e, i=i: e.memset(xp[i][:, T + 2:T + 4], 0.0), writes=[Bxp[i]])
            S.dve(lambda e, i=i: e.memset(xc[i][:, 0:2], 0.0), writes=[Bxc[i]])
            S.dve(lambda e, i=i: e.memset(xc[i][:, CT + 2:CT + 4], 0.0), writes=[Bxc[i]])
        for cc in range(16):
            b = cc % 2
            S.dma("sp", xp[b][:, 2:T + 2], Dm["qk_raw"][cc * 128:(cc + 1) * 128, :], writes=[Bxp[b]])
            for g in range(8):
                ps, Bps = K.nextF()
                for j in range(5):
                    S.pe(lambda e, j=j, g=g, ps=ps, b=b, cc=cc: e.matmul(ps[:, :], lhsT=dg[:, cc, j, :], rhs=xp[b][:, g * 512 + j:g * 512 + j + 512],
                                                                         start=(j == 0), stop=(j == 4)), reads=[Bdg, Bxp[b]], writes=[Bps])
                S.act(lambda e, g=g, ps=ps, b=b, cc=cc: e.activation(out=so[b][:, g * 512:(g + 1) * 512], in_=ps[:, :], func=AF.Silu,
                                                                     bias=cb[:, cc:cc + 1]), reads=[Bps, Bcbias], writes=[Bso[b]])
            S.dma("pool", Dm["qk_act"][cc * 128:(cc + 1) * 128, :], so[b][:], reads=[Bso[b]])
            yield
            if cc >= 8:
                S.dma("sp", xc[b][:, 2:CT + 2], Dm["kc_raw"][(cc - 8) * 128:(cc - 7) * 128, :], writes=[Bxc[b]])
                ps, Bps = K.nextF()
                for j in range(5):
                    S.pe(lambda e, j=j, ps=ps, b=b, cc=cc: e.matmul(ps[:, 0:CT], lhsT=dg[:, cc, j, :], rhs=xc[b][:, j:j + CT],
                                                                    start=(j == 0), stop=(j == 4)), reads=[Bdg, Bxc[b]], writes=[Bps])
                S.act(lambda e, ps=ps, b=b, cc=cc: e.activation(out=soc[b][:], in_=ps[:, 0:CT], func=AF.Silu, bias=cb[:, cc:cc + 1]),
                      reads=[Bps, Bcbias], writes=[Bsoc[b]])
                S.dma("pool", Dm["kc_act"][(cc - 8) * 128:(cc - 7) * 128, :], soc[b][:], reads=[Bsoc[b]])
        yield


def phase3_gen(K):
    import math
    nc, S, I, Dm = K.nc, K.S, K.I, K.Dm
    tokS, BtokS, decb, Bdec = K.tokS, K.BtokS, K.decb, K.Bdec
    es = K.es23
    if True:
        def sb(name, shape, dt):
            return es.enter_context(nc.sbuf_tensor(name, list(shape), dt))
        G2 = sb("G2", [4, 2, SEQ], F32); BG = Buf("G2")
        R2b = sb("R2", [4, 2, SEQ], F32); BR2b = Buf("R2")
        nbif = sb("nbif", [4, 4], F32)
        XB = sb("XB", [4, SEQ], F32); BXB = Buf("XB")
        W = sb("W3", [4, SEQ], F32); BW = Buf("W3")
        E3 = sb("E3", [4, SEQ], F32); BE = Buf("E3")
        bif = sb("bif", [4, 4], F32); Bbif = Buf("bif")
        ones4 = sb("ones4", [4, SEQ], F32); Bo4 = Buf("ones4")
        dl = sb("dl3", [4, NS_ + 1], F32); Bdl = Buf("dl3")
        lkt = sb("lkt", [4, 1], F32); Blk = Buf("lkt")
        S.dma("sp", bif[:], I["b_if"].rearrange("(q h) -> h q", h=4), writes=[Bbif], allow_slow_non_contiguous=True)
        S.dve(lambda e: e.memset(ones4[:], 1.0), writes=[Bo4])
        S.dve(lambda e: e.tensor_scalar(out=nbif[:], in0=bif[:], scalar1=-1.0, scalar2=None, op0=ALU.mult), reads=[Bbif], writes=[Bbif])
        S.dve(lambda e: e.memset(lkt[:], math.log(1.0 / 16.0)), writes=[Blk])
        for d in range(2):
            S.dma("sp", G2[:], Dm["gates"][2 * d:2 * d + 2].rearrange("q h s -> h q s"), writes=[BG])
            S.act(lambda e, d=d: e.activation(out=G2[:, 1, :], in_=G2[:, 1, :], func=AF.Exp, scale=-1.0, bias=nbif[:, 2 * d + 1:2 * d + 2]),
                  reads=[BG, Bbif], writes=[BG])
            S.act(lambda e: e.activation(out=G2[:, 1, :], in_=G2[:, 1, :], func=AF.Ln, bias=1.0), reads=[BG], writes=[BG])
            if d == 0:
                R2, BR = G2, BG
            else:
                R2, BR = R2b, BR2b
                for q in range(2):
                    S.dve(lambda e, q=q: e.tensor_copy(out=R2[:, q, 0:CT], in_=G2[:, q, 0:CT][:, ::-1]), reads=[BG], writes=[BR])
                    S.dve(lambda e, q=q: e.tensor_copy(out=R2[:, q, CT:SEQ], in_=G2[:, q, CT:SEQ][:, ::-1]), reads=[BG], writes=[BR])
            S.dve(lambda e: e.tensor_tensor_scan(out=XB[:], data0=ones4[:], data1=R2[:, 1, :], initial=0.0, op0=ALU.mult, op1=ALU.add),
                  reads=[BR, Bo4], writes=[BXB])
            S.dve(lambda e, d=d: e.scalar_tensor_tensor(out=R2[:, 0, :], in0=R2[:, 0, :], scalar=bif[:, 2 * d:2 * d + 1], in1=XB[:], op0=ALU.add, op1=ALU.add),
                  reads=[BR, BXB, Bbif], writes=[BR])
            S.dve(lambda e: e.tensor_tensor_scan(out=R2[:, 1, :], data0=ones4[:], data1=R2[:, 0, :], initial=-1e30, op0=ALU.mult, op1=ALU.max),
                  reads=[BR, Bo4], writes=[BR])
            S.dve(lambda e: e.memset(dl[:], 0.0), writes=[Bdl])
            S.dve(lambda e: e.tensor_tensor(out=dl[:, 1:NS_], in0=R2[:, 1, 127:SEQ - 128:128], in1=R2[:, 1, 255:SEQ:128], op=ALU.subtract),
                  reads=[BR], writes=[Bdl])
            yield
            A3 = R2[:, 0, :].rearrange("p (c t) -> p c t", t=128)
            G3 = R2[:, 1, :].rearrange("p (c t) -> p c t", t=128)
            W3 = W[:].rearrange("p (c t) -> p c t", t=128)
            X3 = XB[:].rearrange("p (c t) -> p c t", t=128)
            ge_b = G3[:, :, 127:128].to_broadcast([4, NS_, 128])
            gn_b = G3[:, 1:NS_, 127:128].to_broadcast([4, NS_ - 1, 128])
            gl_b = G3[:, NS_ - 1:NS_, 127:128].to_broadcast([4, 1, 128])
            S.dve(lambda e: e.tensor_tensor(out=W3, in0=A3, in1=ge_b, op=ALU.subtract), reads=[BR], writes=[BW])
            S.dve(lambda e: e.tensor_tensor(out=X3, in0=X3, in1=ge_b, op=ALU.subtract), reads=[BR, BXB], writes=[BXB])
            S.dve(lambda e: e.tensor_tensor(out=A3[:, 0:NS_ - 1, :], in0=A3[:, 0:NS_ - 1, :], in1=gn_b, op=ALU.subtract), reads=[BR], writes=[BR])
            S.dve(lambda e: e.tensor_tensor(out=A3[:, NS_ - 1:NS_, :], in0=A3[:, NS_ - 1:NS_, :], in1=gl_b, op=ALU.subtract), reads=[BR], writes=[BR])
            yield
            S.act(lambda e: e.activation(out=W[:], in_=W[:], func=AF.Exp, bias=lkt[:, 0:1]), reads=[BW, Blk], writes=[BW])
            S.act(lambda e: e.activation(out=R2[:, 0, :], in_=R2[:, 0, :], func=AF.Exp, bias=lkt[:, 0:1]), reads=[BR, Blk], writes=[BR])
            S.act(lambda e: e.activation(out=XB[:], in_=XB[:], func=AF.Exp), reads=[BXB], writes=[BXB])
            S.act(lambda e: e.activation(out=dl[:], in_=dl[:], func=AF.Exp), reads=[Bdl], writes=[Bdl])
            yield
            srcs = [(W, BW, W[:]), (R2, BR, R2[:, 0, :]), (XB, BXB, XB[:])]
            if d == 1:
                dsts = [(G2, BG, G2[:, 0, :]), (G2, BG, G2[:, 1, :]), (E3, BE, E3[:])]
                for (st, Bs, sap), (dt_, Bd, dap) in zip(srcs, dsts):
                    S.dve(lambda e, sap=sap, dap=dap: e.tensor_copy(out=dap[:, 0:CT], in_=sap[:, 0:CT][:, ::-1]), reads=[Bs], writes=[Bd])
                    S.dve(lambda e, sap=sap, dap=dap: e.tensor_copy(out=dap[:, CT:SEQ], in_=sap[:, CT:SEQ][:, ::-1]), reads=[Bs], writes=[Bd])
                srcs = dsts
            for k, (st, Bs, sap) in enumerate(srcs):
                ps, Bps = K.nextF()
                for c in range(NS_):
                    S.pe(lambda e, c=c, ps=ps, sap=sap: e.transpose(out=ps[:, c * 4:(c + 1) * 4], in_=sap[:, c * 128:(c + 1) * 128],
                                                                    identity=K.cst[0:4, C_ID:C_ID + 4]), reads=[Bs, K.Bcst], writes=[Bps])
                S.dve(lambda e, d=d, k=k, ps=ps: e.tensor_copy(out=tokS[:, d, k, :, :], in_=ps[:, 0:NS_ * 4].rearrange("p (c h) -> p c h", h=4)),
                      reads=[Bps], writes=[BtokS])
            ps, Bps = K.nextF()
            for h in range(4):
                S.pe(lambda e, h=h, ps=ps: e.matmul(ps[:, h * (NS_ + 1):(h + 1) * (NS_ + 1)], lhsT=K.cst[0:4, C_SEL + h * 128:C_SEL + (h + 1) * 128],
                                                    rhs=dl[:], start=True, stop=True), reads=[Bdl, K.Bcst], writes=[Bps])
            S.dve(lambda e, d=d, ps=ps: e.tensor_copy(out=decb[:, d, :, :], in_=ps[:, 0:4 * (NS_ + 1)].rearrange("p (h r) -> p h r", h=4)),
                  reads=[Bps], writes=[Bdec])
        yield


def phase2(K):
    pass


def phase3(K):
    pass


def phase23(K):
    K.es23 = contextlib.ExitStack()
    g2 = phase2_gen(K)
    g3 = phase3_gen(K)
    alive = [g2, g3]
    while alive:
        for g in list(alive):
            try:
                next(g)
            except StopIteration:
                alive.remove(g)
    K.S.fence()
    K.es23.close()


def phase4(K):
    nc, S, I, Dm = K.nc, K.S, K.I, K.Dm
    tokS, BtokS, decb, Bdec = K.tokS, K.BtokS, K.decb, K.Bdec
    psF, psB = K.psF, K.psB
    with contextlib.ExitStack() as es:
        def sb(name, shape, dt):
            return es.enter_context(nc.sbuf_tensor(name, list(shape), dt))
        Va = sb("Va", [128, NS_, 4, 257], BF16); BVaT = [Buf("Va%d" % c) for c in range(NS_)]
        S.dve(lambda e: e.memset(Va[:, :, :, 256:257], 1.0), writes=BVaT)
        va_loaded = set()

        def va_load(c):
            if c in va_loaded:
                return
            va_loaded.add(c)
            S.dma("sp", Va[:, c, :, 0:256], Dm["v_tok"][c * 128:(c + 1) * 128, :].rearrange("p (h e) -> p h e", h=4), writes=[BVaT[c]])
        maskf = K.cst[:, C_TRI:C_TRI + 128]
        maskb = K.cst[:, C_TRIT:C_TRIT + 128]
        NB = 3
        qt = [[sb("qt%d_%d" % (d, i), [128, 8, 128], BF16) for i in range(NB)] for d in range(2)]
        kt = [[sb("kt%d_%d" % (d, i), [128, 8, 128], BF16) for i in range(NB)] for d in range(2)]
        ktok = [[sb("ktok%d_%d" % (d, i), [128, D], BF16) for i in range(NB)] for d in range(2)]
        Bktok = [[Buf("ktok") for i in range(NB)] for d in range(2)]
        ps, Bps = K.ps_all, K.Bps_all
        kTb = ps[7][:].bitcast(BF16)
        Bqt = [[Buf("qt") for i in range(NB)] for d in range(2)]
        Bkt = [[Buf("kt") for i in range(NB)] for d in range(2)]
        S32 = [[sb("S32_%d_%d" % (d, h), [128, 2, 257], F32) for h in range(4)] for d in range(2)]
        Sbf = [[sb("Sbf_%d_%d" % (d, h), [128, 2, 257], BF16) for h in range(4)] for d in range(2)]
        BS32 = [[Buf("S32") for h in range(4)] for d in range(2)]
        BSbf = [[Buf("Sbf") for h in range(4)] for d in range(2)]
        NR = 6
        Sm = [sb("Sm%d" % i, [128, 128], BF16) for i in range(NR)]; BSm = [Buf("Sm") for i in range(NR)]
        ktl = [sb("ktl%d" % i, [128, 256], BF16) for i in range(NR)]; Bktl = [Buf("ktl") for i in range(NR)]
        den = [sb("den%d" % i, [128, 2], F32) for i in range(NR)]; Bden = [Buf("den") for i in range(NR)]
        hb = [sb("hb%d" % i, [128, D], F32) for i in range(4)]; Bhb = [Buf("hb") for i in range(4)]
        hl = [sb("hl%d" % i, [128, D], F32) for i in range(3)]; Bhl = [Buf("hl") for i in range(3)]
        hnb = [sb("hnb%d" % i, [128, D], BF16) for i in range(2)]; Bhnb = [Buf("hnb") for i in range(2)]
        junk = sb("junk4", [128, 256], F32); Bjunk = Buf("junk4", sync_all=True)
        ssq = [sb("ssq4_%d" % i, [128, 4], F32) for i in range(4)]; Bssq = [Buf("ssq4") for i in range(4)]
        epi = {"n": 0, "q": [], "it": 0}
        Bh = [Buf("h_f%d" % c) for c in range(NS_)]
        first_done = [False] * NS_
        BsS = [K.Bps_all[0], K.Bps_all[1]]
        Bnum = [K.Bps_all[2], K.Bps_all[3], K.Bps_all[4]]
        BP = [K.Bps_all[5], K.Bps_all[6]]
        for d in range(2):
            for h in range(4):
                S.dve(lambda e, d=d, h=h: e.memset(S32[d][h][:], 0.0), writes=[BS32[d][h]])
                S.dve(lambda e, d=d, h=h: e.memset(Sbf[d][h][:], 0.0), writes=[BSbf[d][h]])
        items = []
        rot = {"hb": 0, "hl": 0}
        for step in range(NS_):
            for d in range(2):
                c = step if d == 0 else ((1 - step) if step < 2 else (35 - step))
                for h in range(4):
                    items.append(dict(step=step, d=d, c=c, h=h, lat=(c >= 2), n=len(items)))

        def tile_loads(step, d):
            c = step if d == 0 else ((1 - step) if step < 2 else (35 - step))
            bi = step % NB
            va_load(c)
            if c >= 2:
                tk = (c - 2) * 128
                S.dma("sp", qt[d][bi][:], Dm["qk_act"][0:D, tk:tk + 128].rearrange("(j p) t -> p j t", p=128), writes=[Bqt[d][bi]])
                S.dma("sp", kt[d][bi][:], Dm["qk_act"][D:2 * D, tk:tk + 128].rearrange("(j p) t -> p j t", p=128), writes=[Bkt[d][bi]])
            else:
                S.dma("sp", kt[d][bi][:], Dm["kc_act"][:, c * 128:(c + 1) * 128].rearrange("(j p) t -> p j t", p=128), writes=[Bkt[d][bi]])

        def tile_setup(it):
            step, d, c = it["step"], it["d"], it["c"]
            bi = step % NB
            if step == 0:
                tile_loads(step, d)
            if d == 0 and step + 1 < NS_:
                tile_loads(step + 1, 0)
                tile_loads(step + 1, 1)
            info = dict(bi=bi)
            if step < NS_ - 1:
                for j in range(8):
                    S.pe(lambda e, j=j: e.transpose(out=kTb[:, j * 128:(j + 1) * 128], in_=kt[d][bi][:, j, :], identity=K.identb[:]),
                         reads=[Bkt[d][bi], K.Bcb], writes=[Bps[7]])
                S.act(lambda e: e.activation(out=ktok[d][bi][:], in_=kTb[:, :], func=AF.Copy), reads=[Bps[7]], writes=[Bktok[d][bi]])
            if it["lat"]:
                hi = rot["hb"] % 4; rot["hb"] += 1
                info["hi"] = hi
                info["second"] = first_done[c]
                if info["second"]:
                    li_ = rot["hl"] % 3; rot["hl"] += 1
                    info["li"] = li_
                    info["deferred"] = Bh[c].last_w is None
                    if not info["deferred"]:
                        S.dma("sp", hl[li_][:], Dm["h_f"][(c - 2) * 128:(c - 1) * 128, :], reads=[Bh[c]], writes=[Bhl[li_]])
                else:
                    first_done[c] = True
            return info

        tinfo = {}

        def stageA(it):
            step, d, c, h, n = it["step"], it["d"], it["c"], it["h"], it["n"]
            if h == 0:
                tinfo[(step, d)] = tile_setup(it)
            bi = tinfo[(step, d)]["bi"]
            q_, k_, Bq_, Bk_ = qt[d][bi], kt[d][bi], Bqt[d][bi], Bkt[d][bi]
            sl = n % 2
            if it["lat"]:
                for j in range(2):
                    S.pe(lambda e, j=j: e.matmul(psF[sl][:, 0:128], lhsT=k_[:, 2 * h + j, :], rhs=q_[:, 2 * h + j, :],
                                                 start=(j == 0), stop=(j == 1)), reads=[Bq_, Bk_], writes=[BsS[sl]])

        def stageB(it):
            step, d, c, h, n = it["step"], it["d"], it["c"], it["h"], it["n"]
            sl = n % 2; r = n % NR
            mask = maskf if d == 0 else maskb
            if it["lat"]:
                S.dve(lambda e: e.scalar_tensor_tensor(out=Sm[r][:], in0=psF[sl][:, 0:128], scalar=tokS[:, d, 0, c, h:h + 1], in1=mask,
                                                       op0=ALU.mult, op1=ALU.mult), reads=[BsS[sl], BtokS, K.Bcst], writes=[BSm[r]])
            if step < NS_ - 1:
                bi = tinfo[(step, d)]["bi"]
                S.act(lambda e: e.activation(out=ktl[r][:], in_=ktok[d][bi][:, h * 256:(h + 1) * 256], func=AF.Copy, scale=tokS[:, d, 1, c, h:h + 1]),
                      reads=[Bktok[d][bi], BtokS], writes=[Bktl[r]])

        def stageC(it):
            step, d, c, h, n = it["step"], it["d"], it["c"], it["h"], it["n"]
            bi = tinfo[(step, d)]["bi"]
            q_, Bq_ = qt[d][bi], Bqt[d][bi]
            r = n % NR; st = n % 3; sp_ = n % 2
            pn = ps[2 + st]; pp = ps[5 + sp_]
            if it["lat"]:
                for j in range(2):
                    S.pe(lambda e, j=j: e.matmul(pn[:, 0:257], lhsT=q_[:, 2 * h + j, :], rhs=Sbf[d][h][:, j, :], start=(j == 0), stop=False),
                         reads=[Bq_, BSbf[d][h]], writes=[Bnum[st]])
                S.pe(lambda e: e.matmul(pn[:, 0:257], lhsT=Sm[r][:], rhs=Va[:, c, h, :], start=False, stop=True),
                     reads=[BSm[r], BVaT[c]], writes=[Bnum[st]])
            if step < NS_ - 1:
                for j in range(2):
                    S.pe(lambda e, j=j: e.matmul(pp[:, j * 256:(j + 1) * 256], lhsT=ktl[r][:, j * 128:(j + 1) * 128], rhs=Va[:, c, h, 0:256], start=True, stop=True),
                         reads=[Bktl[r], BVaT[c]], writes=[BP[sp_]])
                    S.pe(lambda e, j=j: e.matmul(pn[:, 260 + j:261 + j], lhsT=ktl[r][:, j * 128:(j + 1) * 128], rhs=Va[:, c, h, 256:257], start=True, stop=True),
                         reads=[Bktl[r], BVaT[c]], writes=[Bnum[st]])

        def stageD1(it):
            step, d, c, h, n = it["step"], it["d"], it["c"], it["h"], it["n"]
            r = n % NR; st = n % 3; sp_ = n % 2
            pn = ps[2 + st]; pp = ps[5 + sp_]
            upd = step < NS_ - 1
            dk = decb[:, d, h, step + 1:step + 2] if upd else None
            if it["lat"]:
                S.dve(lambda e: e.tensor_scalar(out=den[r][:, 0:1], in0=pn[:, 256:257], scalar1=tokS[:, d, 2, c, h:h + 1], scalar2=None, op0=ALU.max),
                      reads=[Bnum[st], BtokS], writes=[Bden[r]])
            if upd:
                S.dve(lambda e: e.scalar_tensor_tensor(out=S32[d][h][:, :, 0:256], in0=S32[d][h][:, :, 0:256], scalar=dk,
                                                       in1=pp[:, 0:512].rearrange("p (j e) -> p j e", j=2), op0=ALU.mult, op1=ALU.add),
                      reads=[BP[sp_], Bdec, BS32[d][h]], writes=[BS32[d][h]])
            if it["lat"]:
                S.dve(lambda e: e.scalar_tensor_tensor(out=den[r][:, 0:1], in0=pn[:, 256:257], scalar=-1.0, in1=den[r][:, 0:1], op0=ALU.mult, op1=ALU.max),
                      reads=[Bnum[st], Bden[r]], writes=[Bden[r]])
            if upd:
                S.dve(lambda e: e.scalar_tensor_tensor(out=S32[d][h][:, :, 256], in0=S32[d][h][:, :, 256], scalar=dk,
                                                       in1=pn[:, 260:262], op0=ALU.mult, op1=ALU.add),
                      reads=[Bnum[st], Bdec, BS32[d][h]], writes=[BS32[d][h]])
                S.pool(lambda e: e.tensor_copy(out=Sbf[d][h][:], in_=S32[d][h][:]), reads=[BS32[d][h]], writes=[BSbf[d][h]])
            if it["lat"]:
                S.dve(lambda e: e.reciprocal(out=den[r][:, 1:2], in_=den[r][:, 0:1]), reads=[Bden[r]], writes=[Bden[r]])

        def stageD2(it):
            step, d, c, h, n = it["step"], it["d"], it["c"], it["h"], it["n"]
            r = n % NR; st = n % 3
            pn = ps[2 + st]
            ti = tinfo[(step, d)]
            if it["lat"] and h == 0 and ti["second"] and ti["deferred"]:
                assert Bh[c].last_w is not None
                S.dma("sp", hl[ti["li"]][:], Dm["h_f"][(c - 2) * 128:(c - 1) * 128, :], reads=[Bh[c]], writes=[Bhl[ti["li"]]])
            if it["lat"]:
                hbuf, Bhbuf = hb[ti["hi"]], Bhb[ti["hi"]]
                if not ti["second"]:
                    S.act(lambda e: e.activation(out=hbuf[:, h * 256:(h + 1) * 256], in_=pn[:, 0:256], func=AF.Copy, scale=den[r][:, 1:2]),
                          reads=[Bnum[st], Bden[r]], writes=[Bhbuf])
                else:
                    li_ = ti["li"]
                    S.dve(lambda e: e.scalar_tensor_tensor(out=hbuf[:, h * 256:(h + 1) * 256], in0=pn[:, 0:256], scalar=den[r][:, 1:2],
                                                           in1=hl[li_][:, h * 256:(h + 1) * 256], op0=ALU.mult, op1=ALU.add),
                          reads=[Bnum[st], Bden[r], Bhl[li_]], writes=[Bhbuf])
            if it["lat"] and h == 3:
                hbuf, Bhbuf = hb[ti["hi"]], Bhb[ti["hi"]]
                if not ti["second"]:
                    S.dma("pool", Dm["h_f"][(c - 2) * 128:(c - 1) * 128, :], hbuf[:], reads=[Bhbuf], writes=[Bh[c]])
                else:
                    si = epi["n"] % 4; epi["n"] += 1
                    cc = c

                    def e1():
                        S.dve(lambda e: e.memset(ssq[si][:], 0.0), writes=[Bssq[si]])
                        for hh in range(4):
                            S.act(lambda e, hh=hh: e.activation(out=junk[:], in_=hbuf[:, hh * 256:(hh + 1) * 256], func=AF.Square, accum_out=ssq[si][:, hh:hh + 1]),
                                  reads=[Bhbuf], writes=[Bssq[si], Bjunk])

                    def e2():
                        S.dve(lambda e: e.tensor_scalar(out=ssq[si][:], in0=ssq[si][:], scalar1=1.0 / 256, scalar2=EPS, op0=ALU.mult, op1=ALU.add),
                              reads=[Bssq[si]], writes=[Bssq[si]])

                    def e3():
                        S.act(lambda e: e.activation(out=ssq[si][:], in_=ssq[si][:], func=AF.Sqrt), reads=[Bssq[si]], writes=[Bssq[si]])

                    def e4():
                        S.dve(lambda e: e.reciprocal(out=ssq[si][:], in_=ssq[si][:]), reads=[Bssq[si]], writes=[Bssq[si]])

                    def e5():
                        for hh in range(4):
                            S.act(lambda e, hh=hh: e.activation(out=hnb[si % 2][:, hh * 256:(hh + 1) * 256], in_=hbuf[:, hh * 256:(hh + 1) * 256],
                                                                func=AF.Copy, scale=ssq[si][:, hh:hh + 1]),
                                  reads=[Bhbuf, Bssq[si]], writes=[Bhnb[si % 2]])
                        S.dma("pool", Dm["hn"][(cc - 2) * 128:(cc - 1) * 128, :], hnb[si % 2][:], reads=[Bhnb[si % 2]])
                    for k_, fn_ in enumerate((e1, e2, e3, e4, e5)):
                        epi["q"].append((epi["it"] + k_, fn_))

        NI = len(items)
        stageA(items[0]); stageA(items[1]); stageB(items[0])
        for n in range(NI + 2):
            if n + 2 < NI:
                stageA(items[n + 2])
            if n + 1 < NI:
                stageB(items[n + 1])
            if n < NI:
                stageC(items[n])
            if 1 <= n <= NI:
                stageD1(items[n - 1])
            epi["it"] = n
            if 2 <= n <= NI + 1:
                stageD2(items[n - 2])
            due = [f for (t_, f) in epi["q"] if t_ <= n]
            epi["q"] = [(t_, f) for (t_, f) in epi["q"] if t_ > n]
            for f in due:
                f()
        for (t_, f) in sorted(epi["q"], key=lambda x: x[0]):
            f()
        S.fence()


def phase5(K):
    nc, S, I, Dm = K.nc, K.S, K.I, K.Dm
    with contextlib.ExitStack() as es:
        def sb(name, shape, dt):
            return es.enter_context(nc.sbuf_tensor(name, list(shape), dt))
        aT = [sb("aT%d" % g, [128, T], BF16) for g in range(4)]; BaT = [Buf("aT") for g in range(4)]
        wpo = sb("wpo", [128, 4, D], BF16); wmo = sb("wmo", [128, 8, D], BF16); wout = sb("wout", [128, 8, D], BF16)
        Bw5 = Buf("w5")
        S.dma("pool", wpo[:], I["w_pool_out"].rearrange("(g p) n -> p g n", p=128), writes=[Bw5])
        S.dma("pool", wmo[:], I["w_mlstm_out"].rearrange("(g p) n -> p g n", p=128), writes=[Bw5])
        S.dma("pool", wout[:], I["w_out"].rearrange("(g p) n -> p g n", p=128), writes=[Bw5])
        mixb = sb("mixb", [128, 4, 128], BF16); Bmix = Buf("mixb")
        S.dma("pool", mixb[:], I["pool_mix"].rearrange("g c d -> c g d"), writes=[Bmix])
        psc = K.vpA[:, 80:84]; Bpsc = K.Bvp
        gmn = K.vpA[:, 84:92]; Bgmn = K.Bvp
        g1bc = sb("g1bc", [128, D], F32); Bg1 = Buf("g1bc")
        S.dma("sp", g1bc[:], Dm["bvec"][0:1, :].partition_broadcast(128), writes=[Bg1])
        with contextlib.ExitStack() as es2:
            def sb2(name, shape, dt):
                return es2.enter_context(nc.sbuf_tensor(name, list(shape), dt))
            U = sb2("U5", [128, 80, 80], F32); BU = Buf("U5")
            PA = sb2("PA5", [128, 80, 80], F32); BPA = Buf("PA5")
            PB = sb2("PB5", [128, 80, 80], F32); BPB = Buf("PB5")
            icn = sb2("icn", [128, T], F32); Bicn = Buf("icn")
            tmp = sb2("tmp5", [128, T], F32); Btmp = Buf("tmp5")
            apre = sb2("apre", [128, T], BF16); Bap = Buf("apre")
            S.dve(lambda e: e.memset(U[:], 0.0), writes=[BU])
            for gq in range(4):
                n = gq + 1
                if gq == 1:
                    for j in range(8):
                        S.dve(lambda e, j=j: e.tensor_scalar(out=wmo[:, j, :], in0=wmo[:, j, :], scalar1=gmn[:, j:j + 1], scalar2=None, op0=ALU.mult),
                              reads=[Bw5, Bgmn], writes=[Bw5])
                    for j in range(8):
                        S.dve(lambda e, j=j: e.tensor_tensor(out=wout[:, j, :], in0=wout[:, j, :], in1=g1bc[:], op=ALU.mult),
                              reads=[Bw5, Bg1], writes=[Bw5])
                S.dma("sp", tmp[:], Dm["u_pool"][gq * 128:(gq + 1) * 128, :], writes=[Btmp])
                S.act(lambda e: e.activation(out=U[:, 8:72, 8:72], in_=tmp[:].rearrange("p (r c) -> p r c", c=64), func=AF.Copy),
                      reads=[Btmp], writes=[BU])
                S.dma("sp", icn[:], I["invcnt"][gq:gq + 1, :].partition_broadcast(128), writes=[Bicn])
                lo = [0] * (n + 1); hi = [0] * (n + 1)
                lo[n], hi[n] = 0, 64
                for k in range(n - 1, 0, -1):
                    sh = 2 ** (k - 1)
                    lo[k], hi[k] = lo[k + 1] - sh, hi[k + 1] + sh
                bufs = [(PA, BPA), (PB, BPB)]
                src, Bsrc = U, BU
                bi = 0
                for axis in (1, 0):
                    for k in range(1, n + 1):
                        dst, Bdst = bufs[bi]; bi ^= 1
                        a, b = lo[k] + 8, hi[k] + 8
                        if k == 1:
                            s0, s1 = -1, 0
                        else:
                            s0, s1 = -(2 ** (k - 2)), 2 ** (k - 2)
                        if axis == 1:
                            o = dst[:, :, a:b]; i0 = src[:, :, a + s0:b + s0]; i1 = src[:, :, a + s1:b + s1]
                        else:
                            o = dst[:, a:b, 8:72]; i0 = src[:, a + s0:b + s0, 8:72]; i1 = src[:, a + s1:b + s1, 8:72]
                        S.dve(lambda e, o=o, i0=i0, i1=i1: e.tensor_tensor(out=o, in0=i0, in1=i1, op=ALU.add), reads=[Bsrc], writes=[Bdst])
                        src, Bsrc = dst, Bdst
                S.dve(lambda e: e.tensor_tensor(out=tmp[:].rearrange("p (r c) -> p r c", c=64), in0=src[:, 8:72, 8:72],
                                                in1=icn[:].rearrange("p (r c) -> p r c", c=64), op=ALU.mult), reads=[Bsrc, Bicn], writes=[Btmp])
                S.dve(lambda e: e.tensor_tensor(out=apre[:].rearrange("p (r c) -> p r c", c=64), in0=tmp[:].rearrange("p (r c) -> p r c", c=64),
                                                in1=U[:, 8:72, 8:72], op=ALU.subtract), reads=[Btmp, BU], writes=[Bap])
                for g in range(8):
                    ps, Bps = K.nextF()
                    S.pe(lambda e: e.matmul(ps[:, :], lhsT=mixb[:, gq, :], rhs=apre[:, g * 512:(g + 1) * 512], start=True, stop=True),
                         reads=[Bmix, Bap], writes=[Bps])
                    S.act(lambda e: e.activation(out=aT[gq][:, g * 512:(g + 1) * 512], in_=ps[:, :], func=AF.Copy, scale=psc[:, gq:gq + 1]),
                          reads=[Bps, Bpsc], writes=[BaT[gq]])
        S.fence()
        for gq in range(4):
            K.dump("aT%d" % gq, aT[gq][:], [BaT[gq]])
        sga = sb("sga", [128, 8, 512], BF16); Bsga = Buf("sga")
        sgm = sb("sgm", [128, 8, 512], BF16); Bsgm = Buf("sgm")
        sgo = [sb("sgo%d" % i, [128, 8, 512], BF16) for i in range(2)]; Bsgo = [Buf("sgo") for i in range(2)]
        hnt = [sb("hnt%d" % i, [128, D], BF16) for i in range(8)]; Bhnt = [Buf("hnt") for i in range(8)]
        xt = [sb("x5_%d" % i, [128, D], F32) for i in range(2)]; Bxt = [Buf("x5") for i in range(2)]
        x1t = [sb("x1t%d" % i, [128, D], F32) for i in range(2)]; Bx1 = [Buf("x1t") for i in range(2)]
        mixA = sb("mixA", [128, 8, 512], BF16); BmixA = Buf("mixA")
        mTb = [sb("mT%d" % i, [128, 8, 512], BF16) for i in range(2)]; BmTb = [Buf("mT") for i in range(2)]
        tmpm = [sb("tmpm%d" % i, [128, 512], F32) for i in range(2)]; Btmpm = [Buf("tmpm") for i in range(2)]
        mixT = sb("mixT", [128, 8, 512], BF16); BmixT = Buf("mixT")
        junk = sb("junk5", [128, D], F32); Bjunk = Buf("junk5", sync_all=True)
        ssq = sb("ssq5", [128, NT], F32); Bssq = Buf("ssq5")
        S.dve(lambda e: e.memset(ssq[:], 0.0), writes=[Bssq])

        def loads_b(g):
            t0 = g * 512
            S.dma("sp", sgo[g % 2][:], Dm["sigo"][:, t0:t0 + 512].rearrange("(j p) t -> p j t", p=128), writes=[Bsgo[g % 2]])
            for ti in range(4):
                hi_ = (g % 2) * 4 + ti
                S.dma("sp", hnt[hi_][:], Dm["hn"][t0 + ti * 128:t0 + (ti + 1) * 128, :], writes=[Bhnt[hi_]])

        def loads_ac(g):
            t0 = g * 512
            S.dma("sp", sga[:], Dm["sigg"][0:D, t0:t0 + 512].rearrange("(j p) t -> p j t", p=128), writes=[Bsga])
            S.dma("sp", sgm[:], Dm["sigg"][D:2 * D, t0:t0 + 512].rearrange("(j p) t -> p j t", p=128), writes=[Bsgm])

        def stage_a(g):
            t0 = g * 512
            for dm in range(8):
                ps, Bps = K.nextF()
                for gq in range(4):
                    S.pe(lambda e, gq=gq: e.matmul(ps[:, :], lhsT=wpo[:, gq, dm * 128:(dm + 1) * 128], rhs=aT[gq][:, t0:t0 + 512],
                                                   start=(gq == 0), stop=(gq == 3)), reads=[Bw5, BaT[gq]], writes=[Bps])
                S.dve(lambda e: e.tensor_tensor(out=mixA[:, dm, :], in0=ps[:, :], in1=sga[:, dm, :], op=ALU.mult), reads=[Bps, Bsga], writes=[BmixA])

        def stage_b(g):
            mT, BmT = mTb[g % 2], BmTb[g % 2]
            for ti in range(4):
                hi_ = (g % 2) * 4 + ti
                pt, Bpt = K.nextB()
                for j in range(8):
                    S.pe(lambda e, j=j: e.transpose(out=pt[:, j * 128:(j + 1) * 128], in_=hnt[hi_][:, j * 128:(j + 1) * 128], identity=K.identb[:]),
                         reads=[Bhnt[hi_], K.Bcb], writes=[Bpt])
                S.dve(lambda e: e.tensor_tensor(out=mT[:, :, ti * 128:(ti + 1) * 128], in0=pt[:, :].rearrange("p (j t) -> p j t", t=128),
                                                in1=sgo[g % 2][:, :, ti * 128:(ti + 1) * 128], op=ALU.mult),
                      reads=[Bpt, Bsgo[g % 2]], writes=[BmT])

        def stage_c(g):
            mT, BmT = mTb[g % 2], BmTb[g % 2]
            for dm in range(8):
                ps, Bps = K.nextF()
                for j in range(8):
                    S.pe(lambda e, j=j: e.matmul(ps[:, :], lhsT=wmo[:, j, dm * 128:(dm + 1) * 128], rhs=mT[:, j, :], start=(j == 0), stop=(j == 7)),
                         reads=[Bw5, BmT], writes=[Bps])
                tm, Btm = tmpm[dm % 2], Btmpm[dm % 2]
                S.dve(lambda e: e.tensor_tensor(out=tm[:], in0=ps[:, :], in1=sgm[:, dm, :], op=ALU.mult), reads=[Bps, Bsgm], writes=[Btm])
                S.dve(lambda e: e.tensor_tensor(out=mixT[:, dm, :], in0=tm[:], in1=mixA[:, dm, :], op=ALU.add), reads=[Btm, BmixA], writes=[BmixT])
            if g == 1:
                K.dump("mixA", mixA[:], [BmixA]); K.dump("mT", mT[:], [BmT]); K.dump("mixT", mixT[:], [BmixT])

        def stage_d(g):
            for ti in range(4):
                tile = g * 4 + ti
                xi = tile % 2
                S.dma("sp", xt[xi][:], I["x"][tile * 128:(tile + 1) * 128, :], writes=[Bxt[xi]])
                for half in range(2):
                    ps, Bps = K.nextF()
                    for j in range(8):
                        S.pe(lambda e, j=j: e.matmul(ps[:, :], lhsT=mixT[:, j, ti * 128:(ti + 1) * 128], rhs=wout[:, j, half * 512:(half + 1) * 512],
                                                     start=(j == 0), stop=(j == 7)), reads=[Bw5, BmixT], writes=[Bps])
                    S.dve(lambda e: e.tensor_tensor(out=x1t[xi][:, half * 512:(half + 1) * 512], in0=ps[:, :], in1=xt[xi][:, half * 512:(half + 1) * 512],
                                                    op=ALU.add), reads=[Bps, Bxt[xi]], writes=[Bx1[xi]])
                S.act(lambda e: e.activation(out=junk[:], in_=x1t[xi][:], func=AF.Square, accum_out=ssq[:, tile:tile + 1]),
                      reads=[Bx1[xi]], writes=[Bssq, Bjunk])
                S.dma("pool", Dm["x1"][tile * 128:(tile + 1) * 128, :], x1t[xi][:], reads=[Bx1[xi]])

        loads_b(0); loads_ac(0)
        stage_b(0)
        for g in range(8):
            if g + 1 < 8:
                loads_b(g + 1)
            stage_a(g)
            stage_c(g)
            if g + 1 < 8:
                stage_b(g + 1)
            stage_d(g)
            if g + 1 < 8:
                loads_ac(g + 1)
        S.dve(lambda e: e.tensor_scalar(out=ssq[:], in0=ssq[:], scalar1=1.0 / D, scalar2=EPS, op0=ALU.mult, op1=ALU.add), reads=[Bssq], writes=[Bssq])
        S.act(lambda e: e.activation(out=ssq[:], in_=ssq[:], func=AF.Sqrt), reads=[Bssq], writes=[Bssq])
        S.dve(lambda e: e.reciprocal(out=K.rstd2[:], in_=ssq[:]), reads=[Bssq], writes=[K.Brstd2])
        S.fence()


def phase6(K):
    nc, S, I, Dm = K.nc, K.S, K.I, K.Dm
    cst, Bcst = K.cst, K.Bcst
    identf = cst[:, C_ID:C_ID + 128]
    with contextlib.ExitStack() as es:
        def sb(name, shape, dt):
            return es.enter_context(nc.sbuf_tensor(name, list(shape), dt))
        aff = sb("aff", [128, NT, NE], F32); Baff = Buf("aff")
        L = sb("L6", [128, NT, NE, 5], BF16); BL = Buf("L6")
        cm1 = sb("cm1", [128, NT, NE], F32); Bcm1 = Buf("cm1")
        zt = sb("zt", [128, D], F32); Bzt = Buf("zt")
        Byacc = Buf("yacc")
        S.dve(lambda e: e.memset(zt[:], 0.0), writes=[Bzt])
        for r in range(33):
            S.dma("pool", Dm["yacc"][r * 128:(r + 1) * 128, :], zt[:], reads=[Bzt])
        S.dma("pool", Dm["h2"][T:T + 128, :], zt[:].bitcast(BF16)[:, 0:D], reads=[Bzt])
        with contextlib.ExitStack() as es2:
            def sb2(name, shape, dt):
                return es2.enter_context(nc.sbuf_tensor(name, list(shape), dt))
            bc = sb2("bc6", [128, 2, D], F32); Bbc = Buf("bc6")
            S.dma("sp", bc[:, 0, :], Dm["bvec"][1:2, :].partition_broadcast(128), writes=[Bbc])
            S.dma("sp", bc[:, 1, :], Dm["bvec"][2:3, :].partition_broadcast(128), writes=[Bbc])
            wr = sb2("wr", [128, 8, NE], BF16); Bwr = Buf("wr")
            S.dma("pool", wr[:], I["w_router"].rearrange("(j p) n -> p j n", p=128), writes=[Bwr])
            Apad = sb2("Apad", [128, NT, 128], F32); BApad = Buf("Apad")
            S.dve(lambda e: e.memset(Apad[:], 0.0), writes=[BApad])
            x1t = [sb2("x6_%d" % i, [128, D], F32) for i in range(2)]; Bx1 = [Buf("x6") for i in range(2)]
            hf = [sb2("hf6_%d" % i, [128, D], F32) for i in range(2)]; Bhf = [Buf("hf6") for i in range(2)]
            hbt = [sb2("hb6_%d" % i, [128, D], BF16) for i in range(2)]; Bhbt = [Buf("hb6") for i in range(2)]
            h2T = [sb2("h2T_%d" % i, [128, D], BF16) for i in range(2)]; Bh2T = [Buf("h2T") for i in range(2)]
            sm = [sb2("sm6_%d" % i, [128, 4], F32) for i in range(4)]; Bsm = [Buf("sm6") for i in range(4)]
            ex = [sb2("ex6_%d" % i, [128, NE], F32) for i in range(4)]; Bex = [Buf("ex6") for i in range(4)]
            def s1(tile):
                b = tile % 2
                S.dma("sp", x1t[b][:], Dm["x1"][tile * 128:(tile + 1) * 128, :], writes=[Bx1[b]])
                S.dve(lambda e: e.scalar_tensor_tensor(out=hf[b][:], in0=x1t[b][:], scalar=K.rstd2[:, tile:tile + 1], in1=bc[:, 0, :],
                                                       op0=ALU.mult, op1=ALU.mult), reads=[Bx1[b], K.Brstd2, Bbc], writes=[Bhf[b]])
                S.dve(lambda e: e.tensor_tensor(out=hbt[b][:], in0=hf[b][:], in1=bc[:, 1, :], op=ALU.add), reads=[Bhf[b], Bbc], writes=[Bhbt[b]])
                S.dma("pool", Dm["h2"][tile * 128:(tile + 1) * 128, :], hbt[b][:], reads=[Bhbt[b]])
                pt, Bpt = K.nextB()
                for j in range(8):
                    S.pe(lambda e, j=j: e.transpose(out=pt[:, j * 128:(j + 1) * 128], in_=hbt[b][:, j * 128:(j + 1) * 128], identity=K.identb[:]),
                         reads=[Bhbt[b], K.Bcb], writes=[Bpt])
                S.act(lambda e: e.activation(out=h2T[b][:], in_=pt[:], func=AF.Copy), reads=[Bpt], writes=[Bh2T[b]])

            def s2a(tile):
                b = tile % 2; b4 = tile % 4
                ps, Bps = K.nextF()
                for j in range(8):
                    S.pe(lambda e, j=j: e.matmul(ps[:, 0:NE], lhsT=h2T[b][:, j * 128:(j + 1) * 128], rhs=wr[:, j, :], start=(j == 0), stop=(j == 7)),
                         reads=[Bh2T[b], Bwr], writes=[Bps])
                S.dve(lambda e: e.reduce_max(out=sm[b4][:, 0:1], in_=ps[:, 0:NE], axis=AX.X), reads=[Bps], writes=[Bsm[b4]])
                S.dve(lambda e: e.memset(sm[b4][:, 2:3], 0.0), writes=[Bsm[b4]])
                S.dve(lambda e: e.tensor_scalar(out=sm[b4][:, 1:2], in0=sm[b4][:, 0:1], scalar1=-1.0, scalar2=None, op0=ALU.mult),
                      reads=[Bsm[b4]], writes=[Bsm[b4]])
                S.act(lambda e: e.activation(out=ex[b4][:], in_=ps[:, 0:NE], func=AF.Exp, bias=sm[b4][:, 1:2], accum_out=sm[b4][:, 2:3]),
                      reads=[Bps, Bsm[b4]], writes=[Bex[b4], Bsm[b4]])

            def s2b(tile):
                b4 = tile % 4
                seg = tile // 4
                S.dve(lambda e: e.reciprocal(out=sm[b4][:, 3:4], in_=sm[b4][:, 2:3]), reads=[Bsm[b4]], writes=[Bsm[b4]])
                S.dve(lambda e: e.tensor_scalar(out=aff[:, tile, :], in0=ex[b4][:], scalar1=sm[b4][:, 3:4], scalar2=None, op0=ALU.mult),
                      reads=[Bex[b4], Bsm[b4]], writes=[Baff])
                S.dve(lambda e: e.tensor_scalar(out=Apad[:, tile, seg * 16:(seg + 1) * 16], in0=ex[b4][:], scalar1=sm[b4][:, 3:4], scalar2=None, op0=ALU.mult),
                      reads=[Bex[b4], Bsm[b4]], writes=[BApad])

            s1(0)
            for tile in range(NT + 1):
                if tile + 1 < NT:
                    s1(tile + 1)
                if tile < NT:
                    s2a(tile)
                if tile >= 1:
                    s2b(tile - 1)
            affE = sb2("affE", [128, 512], F32); BaffE = Buf("affE")
            pa, Bpa = K.nextF()
            for q in range(4):
                for seg in range(8):
                    S.pe(lambda e, q=q, seg=seg: e.matmul(pa[:, q * 128:(q + 1) * 128], lhsT=Apad[:, seg * 4 + q, :], rhs=identf,
                                                          start=(seg == 0), stop=(seg == 7)), reads=[BApad, Bcst], writes=[Bpa])
            S.dve(lambda e: e.tensor_copy(out=affE[:], in_=pa[:]), reads=[Bpa], writes=[BaffE])
            lo = sb2("lo6", [128, 1], F32); Blo = Buf("lo6")
            mid = sb2("mid6", [128, 1], F32); Bmid = Buf("mid6")
            cmpt = sb2("cmp6", [128, 512], F32); Bcmp = Buf("cmp6")
            cnt = sb2("cnt6", [128, 1], F32); Bcnt = Buf("cnt6")
            ge = sb2("ge6", [128, 1], F32); Bge = Buf("ge6")
            S.dve(lambda e: e.memset(lo[:], 0.0), writes=[Blo])
            cmp3 = [sb2("cmp6_%d" % k, [128, 512], F32) for k in range(3)]; Bcmp3 = [Buf("cmp6") for k in range(3)]
            cnt3 = sb2("cnt6_3", [128, 3], F32); Bcnt3 = Buf("cnt6_3")
            ge3 = sb2("ge6_3", [128, 3], F32); Bge3 = Buf("ge6_3")
            w = 1.0
            for it in range(10):
                w *= 0.25
                for k in range(3):
                    S.dve(lambda e, w=w, k=k: e.tensor_scalar(out=cmp3[k][:], in0=affE[:], scalar1=lo[:, 0:1], scalar2=w * (k + 1), op0=ALU.subtract, op1=ALU.is_ge),
                          reads=[BaffE, Blo], writes=[Bcmp3[k]])
                for k in range(3):
                    S.dve(lambda e, k=k: e.reduce_sum(out=cnt3[:, k:k + 1], in_=cmp3[k][:], axis=AX.X), reads=[Bcmp3[k]], writes=[Bcnt3])
                pc, Bpc = K.nextF()
                S.pe(lambda e: e.matmul(pc[:, 0:3], lhsT=cst[:, C_MSUM:C_MSUM + 128], rhs=cnt3[:], start=True, stop=True), reads=[Bcnt3, Bcst], writes=[Bpc])
                S.dve(lambda e, w=w: e.tensor_scalar(out=ge3[:], in0=pc[:, 0:3], scalar1=float(CAP) - 0.5, scalar2=w, op0=ALU.is_ge, op1=ALU.mult),
                      reads=[Bpc], writes=[Bge3])
                S.dve(lambda e: e.reduce_sum(out=ge[:], in_=ge3[:], axis=AX.X), reads=[Bge3], writes=[Bge])
                S.dve(lambda e: e.tensor_tensor(out=lo[:], in0=lo[:], in1=ge[:], op=ALU.add), reads=[Blo, Bge], writes=[Blo])
            rhsd = sb2("rhsd", [128, NE], F32); Brhsd = Buf("rhsd")
            thrb = sb2("thrb", [128, NE], F32); Bthrb = Buf("thrb")
            S.dve(lambda e: e.tensor_scalar(out=rhsd[:], in0=cst[:, C_ID:C_ID + NE], scalar1=lo[:, 0:1], scalar2=None, op0=ALU.mult),
                  reads=[Blo, Bcst], writes=[Brhsd])
            pb_, Bpb_ = K.nextF()
            S.pe(lambda e: e.matmul(pb_[:, 0:NE], lhsT=cst[:, C_ONES:C_ONES + 128], rhs=rhsd[:], start=True, stop=True), reads=[Brhsd, Bcst], writes=[Bpb_])
            S.dve(lambda e: e.tensor_copy(out=thrb[:], in_=pb_[:, 0:NE]), reads=[Bpb_], writes=[Bthrb])
            mk = sb2("mk6", [128, NT, NE], F32); Bmk = Buf("mk6")
            mkb = sb2("mkb6", [128, NT * NE], BF16); Bmkb = Buf("mkb6")
            for i in range(NT):
                S.dve(lambda e, i=i: e.tensor_tensor(out=mk[:, i, :], in0=aff[:, i, :], in1=thrb[:], op=ALU.is_ge), reads=[Baff, Bthrb], writes=[Bmk])
            S.dve(lambda e: e.tensor_copy(out=mkb[:], in_=mk[:].rearrange("p i e -> p (i e)")), reads=[Bmk], writes=[Bmkb])
            pw, Bpw = K.nextF()
            S.pe(lambda e: e.matmul(pw[:, :], lhsT=K.trib[:], rhs=mkb[:], start=True, stop=True), reads=[Bmkb, K.Bcb], writes=[Bpw])
            ptot, Bptot = K.nextF()
            S.pe(lambda e: e.matmul(ptot[:, :], lhsT=K.onesb[:], rhs=mkb[:], start=True, stop=True), reads=[Bmkb, K.Bcb], writes=[Bptot])
            tot = sb2("tot6", [128, NT, NE], F32); Btot = Buf("tot6")
            H = [sb2("H6_%d" % i, [128, NT, NE], F32) for i in range(2)]; BH = [Buf("H6") for i in range(2)]
            S.dve(lambda e: e.tensor_copy(out=tot[:].rearrange("p i e -> p (i e)"), in_=ptot[:, :]), reads=[Bptot], writes=[Btot])
            S.dve(lambda e: e.tensor_copy(out=H[0][:], in_=tot[:]), reads=[Btot], writes=[BH[0]])
            cur = 0
            for sh in (1, 2, 4, 8, 16):
                nx = 1 - cur
                S.dve(lambda e, sh=sh, cur=cur, nx=nx: e.tensor_tensor(out=H[nx][:, sh:, :], in0=H[cur][:, sh:, :], in1=H[cur][:, 0:NT - sh, :], op=ALU.add),
                      reads=[BH[cur]], writes=[BH[nx]])
                S.dve(lambda e, sh=sh, cur=cur, nx=nx: e.tensor_copy(out=H[nx][:, 0:sh, :], in_=H[cur][:, 0:sh, :]), reads=[BH[cur]], writes=[BH[nx]])
                cur = nx
            S.dve(lambda e: e.tensor_tensor(out=cm1[:].rearrange("p i e -> p (i e)"), in0=pw[:, :], in1=H[cur][:].rearrange("p i e -> p (i e)"), op=ALU.add),
                  reads=[Bpw, BH[cur]], writes=[Bcm1])
            S.dve(lambda e: e.scalar_tensor_tensor(out=cm1[:], in0=cm1[:], scalar=-1.0, in1=tot[:], op0=ALU.add, op1=ALU.subtract),
                  reads=[Bcm1, Btot], writes=[Bcm1])
            am = sb2("am6", [128, NT, NE], F32); Bam = Buf("am6")
            S.dve(lambda e: e.tensor_scalar(out=L[:, :, :, 0], in0=mk[:], scalar1=cst[:, C_IOP:C_IOP + 1], scalar2=None, op0=ALU.mult),
                  reads=[Bmk, Bcst], writes=[BL])
            S.dve(lambda e: e.tensor_tensor(out=L[:, :, :, 1], in0=mk[:], in1=cst[:, C_TIDX:C_TIDX + 512].rearrange("p (i e) -> p i e", e=NE), op=ALU.mult),
                  reads=[Bmk, Bcst], writes=[BL])
            S.dve(lambda e: e.tensor_tensor(out=am[:], in0=mk[:], in1=aff[:], op=ALU.mult), reads=[Bmk, Baff], writes=[Bam])
            S.dve(lambda e: e.tensor_copy(out=L[:, :, :, 2], in_=am[:]), reads=[Bam], writes=[BL])
            S.dve(lambda e: e.tensor_tensor(out=L[:, :, :, 3], in0=am[:], in1=L[:, :, :, 2], op=ALU.subtract), reads=[Bam, BL], writes=[BL])
            S.dve(lambda e: e.tensor_copy(out=L[:, :, :, 4], in_=mk[:]), reads=[Bmk], writes=[BL])
            K.dump("aff", aff[:], [Baff]); K.dump("cm1", cm1[:], [Bcm1]); K.dump("mk", mk[:], [Bmk]); K.dump("thrb", thrb[:], [Bthrb])
            S.fence()
        bc2 = sb("bc7", [128, 2, D], F32); Bbc2 = Buf("bc7")
        es3 = contextlib.ExitStack()

        def sb3(name, shape, dt):
            return es3.enter_context(nc.sbuf_tensor(name, list(shape), dt))
        S.dma("sp", bc2[:, 0, :], Dm["bvec"][3:4, :].partition_broadcast(128), writes=[Bbc2])
        S.dma("sp", bc2[:, 1, :], I["final_g"].rearrange("(o n) -> o n", o=1).partition_broadcast(128), writes=[Bbc2])
        wg = [sb3("wg%d" % i, [128, 8, D], BF16) for i in range(2)]
        wu = [sb3("wu%d" % i, [128, 8, D], BF16) for i in range(2)]
        wd = [sb3("wd%d" % i, [128, 8, D], BF16) for i in range(2)]
        Bwg = [Buf("wg") for i in range(2)]; Bwu = [Buf("wu") for i in range(2)]; Bwd = [Buf("wd") for i in range(2)]
        NEQ = 8
        Eq = [sb3("Eq%d" % i, [128, 512], BF16) for i in range(NEQ)]; BEq = [Buf("Eq") for i in range(NEQ)]
        o5 = sb3("o5", [5, 512], F32); Bo5 = Buf("o5")
        sl = [sb3("sl%d" % i, [128, 4, 5], F32) for i in range(4)]; Bsl = [Buf("sl") for i in range(4)]
        idf = [sb3("idf%d" % i, [128, 4], F32) for i in range(4)]; Bidf = [Buf("idf") for i in range(4)]
        idx = [sb3("idx%d" % i, [128, 4], I32) for i in range(4)]; Bidx = [Buf("idx") for i in range(4)]
        afs = [sb3("afs%d" % i, [128, 4], F32) for i in range(4)]; Bafs = [Buf("afs") for i in range(4)]
        xe = [sb3("xe%d" % i, [128, 4, D], BF16) for i in range(3)]; Bxe = [[Buf("xe") for blk in range(4)] for i in range(3)]
        xeT2 = [sb3("xeT%d" % i, [128, 8, 512], BF16) for i in range(2)]; BxeT2 = [Buf("xeT") for i in range(2)]
        hid = sb3("hid", [128, 8, 512], BF16); Bhid = Buf("hid")
        sgt = [sb3("sgt%d" % i, [128, 512], F32) for i in range(2)]; Bsgt = [Buf("sgt") for i in range(2)]
        ye = [sb3("ye%d" % i, [128, D], F32) for i in range(3)]; Bye = [Buf("ye") for i in range(3)]
        ps, Bps = K.ps_all, K.Bps_all
        p5, Bp5 = ps[7], Bps[7]
        ptb, Bptb = ps[6][:].bitcast(BF16), Bps[6]
        st6 = {"eq": 0, "ye": 0, "scat_prev": [], "scat_cur": []}

        def load_gu(e_):
            b = e_ % 2
            S.dma("pool", wg[b][:], I["w_gate"][e_].rearrange("(j p) n -> p j n", p=128), writes=[Bwg[b]])
            S.dma("pool", wu[b][:], I["w_up"][e_].rearrange("(j p) n -> p j n", p=128), writes=[Bwu[b]])

        def load_d(e_):
            b = e_ % 2
            S.dma("pool", wd[b][:], I["w_down"][e_].rearrange("(j p) n -> p j n", p=128), writes=[Bwd[b]])

        eqslot = {}

        def idx_eq(e_, i_lo, i_hi):
            for i in range(i_lo, i_hi):
                q = st6["eq"] % NEQ; st6["eq"] += 1
                eqslot[(e_, i)] = q
                S.dve(lambda e, i=i, q=q: e.tensor_scalar(out=Eq[q][:], in0=cst[:, C_IOJ:C_IOJ + 512], scalar1=cm1[:, i, e_:e_ + 1], scalar2=None,
                                                          op0=ALU.is_equal), reads=[Bcm1, Bcst], writes=[BEq[q]])

        def idx_mm(e_, i_lo, i_hi):
            for i in range(i_lo, i_hi):
                q = eqslot[(e_, i)]
                S.pe(lambda e, i=i, q=q: e.matmul(p5[0:5, :], lhsT=L[:, i, e_, :], rhs=Eq[q][:], start=(i == 0), stop=(i == NT - 1)),
                     reads=[BL, BEq[q]], writes=[Bp5])

        def idx_part(e_, i_lo, i_hi):
            for i in range(i_lo, i_hi, 4):
                idx_eq(e_, i, i + 4)
                idx_mm(e_, i, i + 4)

        def idx_tail(e_):
            b = e_ % 4
            S.dve(lambda e: e.tensor_copy(out=o5[:], in_=p5[0:5, :]), reads=[Bp5], writes=[Bo5])
            pt5, Bpt5 = K.nextF()
            for blk in range(4):
                S.pe(lambda e, blk=blk: e.transpose(out=pt5[:, blk * 5:(blk + 1) * 5], in_=o5[0:5, blk * 128:(blk + 1) * 128], identity=cst[0:5, C_ID:C_ID + 5]),
                     reads=[Bo5, Bcst], writes=[Bpt5])
            S.dve(lambda e: e.tensor_copy(out=sl[b][:], in_=pt5[:, 0:20].rearrange("p (k c) -> p k c", c=5)), reads=[Bpt5], writes=[Bsl[b]])
            S.dve(lambda e: e.scalar_tensor_tensor(out=idf[b][:], in0=sl[b][:, :, 1], scalar=128.0, in1=sl[b][:, :, 0], op0=ALU.mult, op1=ALU.add),
                  reads=[Bsl[b]], writes=[Bidf[b]])
            S.dve(lambda e: e.tensor_scalar(out=afs[b][:], in0=sl[b][:, :, 4], scalar1=-float(T), scalar2=float(T), op0=ALU.mult, op1=ALU.add),
                  reads=[Bsl[b]], writes=[Bafs[b]])
            S.dve(lambda e: e.tensor_tensor(out=idf[b][:], in0=idf[b][:], in1=afs[b][:], op=ALU.add), reads=[Bidf[b], Bafs[b]], writes=[Bidf[b]])
            S.dve(lambda e: e.tensor_copy(out=idx[b][:], in_=idf[b][:]), reads=[Bidf[b]], writes=[Bidx[b]])
            S.dve(lambda e: e.tensor_tensor(out=afs[b][:], in0=sl[b][:, :, 2], in1=sl[b][:, :, 3], op=ALU.add), reads=[Bsl[b]], writes=[Bafs[b]])
            if e_ == 0:
                K.dump("idx0", idx[b][:], [Bidx[b]]); K.dump("afs0", afs[b][:], [Bafs[b]])

        def gather(e_):
            b = e_ % 4
            for blk in range(4):
                S.op("pool", lambda e, blk=blk: e.indirect_dma_start(out=xe[e_ % 3][:, blk, :], out_offset=None, in_=Dm["h2"],
                                                                       in_offset=bass.IndirectOffsetOnAxis(ap=idx[b][:, blk:blk + 1], axis=0)),
                     reads=[Bidx[b]], writes=[Bxe[e_ % 3][blk]], dma=True)

        def transpose_blk(e_, blk):
            xt_, Bxt_ = xeT2[e_ % 2], BxeT2[e_ % 2]
            for j in range(8):
                S.pe(lambda e, j=j: e.transpose(out=ptb[:, j * 128:(j + 1) * 128], in_=xe[e_ % 3][:, blk, j * 128:(j + 1) * 128], identity=K.identb[:]),
                     reads=[Bxe[e_ % 3][blk], K.Bcb], writes=[Bptb])
            S.act(lambda e: e.activation(out=xt_[:, :, blk * 128:(blk + 1) * 128], in_=ptb[:, :].rearrange("p (j t) -> p j t", t=128), func=AF.Copy),
                  reads=[Bptb], writes=[Bxt_])

        def ffn(e_):
            b = e_ % 2
            b3 = e_ % 4
            xeT, BxeT = xeT2[e_ % 2], BxeT2[e_ % 2]
            if e_ + 3 < NE:
                idx_eq(e_ + 3, 0, 4)
            for f in range(8):
                if e_ + 3 < NE:
                    if f < 7:
                        idx_eq(e_ + 3, 4 * (f + 1), 4 * (f + 1) + 4)
                    idx_mm(e_ + 3, 4 * f, 4 * f + 4)
                if e_ + 1 < NE and f % 2 == 1:
                    transpose_blk(e_ + 1, f // 2)
                pg, Bpg = K.nextF()
                for j in range(8):
                    S.pe(lambda e, j=j: e.matmul(pg[:, :], lhsT=wg[b][:, j, f * 128:(f + 1) * 128], rhs=xeT[:, j, :], start=(j == 0), stop=(j == 7)),
                         reads=[Bwg[b], BxeT], writes=[Bpg])
                pu, Bpu = K.nextF()
                for j in range(8):
                    S.pe(lambda e, j=j: e.matmul(pu[:, :], lhsT=wu[b][:, j, f * 128:(f + 1) * 128], rhs=xeT[:, j, :], start=(j == 0), stop=(j == 7)),
                         reads=[Bwu[b], BxeT], writes=[Bpu])
                s_ = f % 2
                S.act(lambda e: e.activation(out=sgt[s_][:], in_=pg[:, :], func=AF.Silu), reads=[Bpg], writes=[Bsgt[s_]])
                S.dve(lambda e: e.tensor_tensor(out=hid[:, f, :], in0=sgt[s_][:], in1=pu[:, :], op=ALU.mult), reads=[Bsgt[s_], Bpu], writes=[Bhid])
            st6["scat_cur"] = []
            for blk in range(4):
                y_ = st6["ye"] % 3; st6["ye"] += 1
                for half in range(2):
                    pd, Bpd = K.nextF()
                    for f in range(8):
                        S.pe(lambda e, f=f: e.matmul(pd[:, :], lhsT=hid[:, f, blk * 128:(blk + 1) * 128], rhs=wd[b][:, f, half * 512:(half + 1) * 512],
                                                     start=(f == 0), stop=(f == 7)), reads=[Bwd[b], Bhid], writes=[Bpd])
                    S.act(lambda e: e.activation(out=ye[y_][:, half * 512:(half + 1) * 512], in_=pd[:, :], func=AF.Copy, scale=afs[b3][:, blk:blk + 1]),
                          reads=[Bpd, Bafs[b3]], writes=[Bye[y_]])
                rec = S.op("pool", lambda e, blk=blk: e.indirect_dma_start(out=Dm["yacc"], out_offset=bass.IndirectOffsetOnAxis(ap=idx[b3][:, blk:blk + 1], axis=0),
                                                                             in_=ye[y_][:], in_offset=None, compute_op=ALU.add),
                           reads=[Bye[y_], Bidx[b3]], dma=True, after=st6["scat_prev"])
                st6["scat_cur"].append(rec)
            st6["scat_prev"] = st6["scat_cur"]
            if e_ + 2 < NE:
                load_gu(e_ + 2)
                load_d(e_ + 2)

        idx_part(0, 0, NT); idx_tail(0); gather(0)
        load_gu(0); load_d(0)
        idx_part(1, 0, NT); idx_tail(1); gather(1)
        load_gu(1); load_d(1)
        idx_part(2, 0, NT); idx_tail(2); gather(2)
        for blk in range(4):
            transpose_blk(0, blk)
        for e_ in range(NE):
            ffn(e_)
            if e_ + 3 < NE:
                idx_tail(e_ + 3)
                gather(e_ + 3)
        S.fence()
        es3.close()
        NYB = 6
        ya = [sb("ya%d" % i, [128, D], F32) for i in range(NYB)]; Bya = [Buf("ya") for i in range(NYB)]
        ot = [sb("ot%d" % i, [128, D], F32) for i in range(NYB)]; Bot = [Buf("ot") for i in range(NYB)]
        junk = sb("junk6", [128, D], F32); Bjunk = Buf("junk6", sync_all=True)
        sqall = sb("sqall", [128, NT], F32); Bsqall = Buf("sqall")
        rsall = sb("rsall", [128, NT], F32); Brsall = Buf("rsall")
        S.dve(lambda e: e.memset(sqall[:], 0.0), writes=[Bsqall])
        NXB = 8
        xa = [sb("xa6_%d" % i, [128, D], F32) for i in range(NXB)]; Bxa = [Buf("xa") for i in range(NXB)]

        def d1(tile):
            b = tile % NYB; bx = tile % NXB
            S.dma("sp", xa[bx][:], Dm["x1"][tile * 128:(tile + 1) * 128, :], writes=[Bxa[bx]])
            S.dma("sp", ya[b][:], Dm["yacc"][tile * 128:(tile + 1) * 128, :], writes=[Bya[b]])
            S.dve(lambda e: e.tensor_tensor(out=ya[b][:], in0=ya[b][:], in1=bc2[:, 0, :], op=ALU.mult), reads=[Bya[b], Bbc2], writes=[Bya[b]])
            S.dve(lambda e: e.tensor_tensor(out=xa[bx][:], in0=xa[bx][:], in1=ya[b][:], op=ALU.add), reads=[Bxa[bx], Bya[b]], writes=[Bxa[bx]])
            S.act(lambda e: e.activation(out=junk[:], in_=xa[bx][:], func=AF.Square, accum_out=sqall[:, tile:tile + 1]), reads=[Bxa[bx]], writes=[Bsqall, Bjunk])

        def d2(tile_lo, tile_hi):
            sl_ = slice(tile_lo, tile_hi)
            S.dve(lambda e: e.tensor_scalar(out=rsall[:, sl_], in0=sqall[:, sl_], scalar1=1.0 / D, scalar2=EPS, op0=ALU.mult, op1=ALU.add),
                  reads=[Bsqall], writes=[Brsall])
            S.act(lambda e: e.activation(out=rsall[:, sl_], in_=rsall[:, sl_], func=AF.Sqrt), reads=[Brsall], writes=[Brsall])
            S.dve(lambda e: e.reciprocal(out=rsall[:, sl_], in_=rsall[:, sl_]), reads=[Brsall], writes=[Brsall])

        def d3(tile):
            b = tile % NYB; bx = tile % NXB
            S.dve(lambda e: e.scalar_tensor_tensor(out=ot[b][:], in0=xa[bx][:], scalar=rsall[:, tile:tile + 1], in1=bc2[:, 1, :], op0=ALU.mult, op1=ALU.mult),
                  reads=[Bxa[bx], Brsall, Bbc2], writes=[Bot[b]])
            S.dma("pool", K.out_d[tile * 128:(tile + 1) * 128, :], ot[b][:], reads=[Bot[b]])

        d1(0); d1(1); d1(2); d1(3)
        for g in range(NT // 2):
            if g + 2 < NT // 2:
                d1(2 * g + 4); d1(2 * g + 5)
            d2(2 * g, 2 * g + 2)
            d3(2 * g); d3(2 * g + 1)
        S.fence()


_W_KEYS = ["w_mod", "b_mod", "norm1_g", "norm2_g", "w_in", "conv_w", "conv_b", "b_if", "pool_mix", "pool_scale", "mlstm_norm_g",
           "w_pool_out", "w_mlstm_out", "w_out", "w_router", "w_gate", "w_up", "w_down"]


def kernel(**inputs):
    B = inputs["x"].shape[0]
    nc = build()
    shared = {k: np.ascontiguousarray(np.asarray(inputs[k], dtype=np.float32)[0]) for k in _W_KEYS}
    shared["final_g"] = np.ascontiguousarray(np.asarray(inputs["final_g"], dtype=np.float32))
    shared["c_ctx"] = np.ascontiguousarray(np.asarray(inputs["c_ctx"], dtype=np.float32))
    shared["consts"] = make_consts()
    shared["invcnt"] = make_invcnt()
    in_maps = []
    for b in range(B):
        m = dict(shared)
        m["x"] = np.ascontiguousarray(np.asarray(inputs["x"], dtype=np.float32)[b])
        m["c"] = np.ascontiguousarray(np.asarray(inputs["c"], dtype=np.float32)[b])
        m["ctx"] = np.ascontiguousarray(np.asarray(inputs["ctx"], dtype=np.float32)[b])
        in_maps.append(m)
    res = run_bass_kernel_spmd(nc, in_maps, core_ids=list(range(B)))
    return np.stack([np.asarray(r["out"], dtype=np.float32) for r in res.results], axis=0)
```

```python
import contextlib
import numpy as np
import concourse.bass as bass
import concourse.mybir as mybir

F32 = mybir.dt.float32
BF16 = mybir.dt.bfloat16
I32 = mybir.dt.int32
AF = mybir.ActivationFunctionType
ALU = mybir.AluOpType
AX = mybir.AxisListType

ENGS = ["pe", "act", "dve", "pool", "sp"]


class Buf:
    __slots__ = ("name", "last_w", "readers", "sync_all")

    def __init__(self, name="", sync_all=False):
        self.name = name
        self.last_w = None
        self.readers = []
        self.sync_all = sync_all


class _Proxy:
    def __init__(self):
        self.call = None

    def __getattr__(self, name):
        def f(*a, **k):
            self.call = (name, a, k)
            return None
        return f


class Rec:
    __slots__ = ("eng", "fn", "deps", "dma", "dma_id", "signal", "sigval", "pos")

    def __init__(self, eng, fn, dma):
        self.eng = eng
        if fn is not None:
            pr = _Proxy()
            fn(pr)
            fn = pr.call
        self.fn = fn
        self.deps = []
        self.dma = dma
        self.dma_id = -1
        self.signal = False
        self.sigval = 0
        self.pos = 0


class Sched:
    def __init__(self, nc, n_dma_sems=48):
        self.nc = nc
        self.streams = {e: [] for e in ENGS}
        self.NS = n_dma_sems
        self.dmas = []
        self.all_out_dmas = []

    def op(self, eng, fn, reads=(), writes=(), dma=False, after=()):
        rec = Rec(eng, fn, dma)
        for r_ in after:
            rec.deps.append((r_, "x"))
        for b in reads:
            if b.last_w is not None:
                rec.deps.append((b.last_w, "raw"))
        for b in writes:
            if b.last_w is not None:
                rec.deps.append((b.last_w, "x" if b.sync_all else "waw"))
            for r in b.readers:
                rec.deps.append((r, "war"))
        if dma:
            rec.dma_id = len(self.dmas)
            if rec.dma_id >= self.NS:
                rec.deps.append((self.dmas[rec.dma_id - self.NS], "x"))
            self.dmas.append(rec)
        for b in reads:
            b.readers.append(rec)
        for b in writes:
            b.last_w = rec
            b.readers = []
        rec.pos = len(self.streams[eng])
        self.streams[eng].append(rec)
        return rec

    def pe(self, fn, reads=(), writes=()):
        return self.op("pe", fn, reads, writes)

    def act(self, fn, reads=(), writes=()):
        return self.op("act", fn, reads, writes)

    def dve(self, fn, reads=(), writes=()):
        return self.op("dve", fn, reads, writes)

    def pool(self, fn, reads=(), writes=()):
        return self.op("pool", fn, reads, writes)

    def dma(self, q, out, in_, reads=(), writes=(), **kw):
        return self.op(q, lambda e: e.dma_start(out=out, in_=in_, **kw), reads, writes, dma=True)

    def fence(self):
        lasts = []
        for e in ENGS:
            for r in reversed(self.streams[e]):
                if not r.dma and r.fn is not None:
                    lasts.append(r)
                    break
        pend = list(self.dmas[-self.NS:])
        for e in ENGS:
            rec = Rec(e, None, False)
            for r in lasts:
                rec.deps.append((r, "x"))
            for r in pend:
                rec.deps.append((r, "x"))
            rec.pos = len(self.streams[e])
            self.streams[e].append(rec)

    def emit(self):
        nc = self.nc
        for e in ENGS:
            for rec in self.streams[e]:
                for (d, kind) in rec.deps:
                    if d.dma:
                        continue
                    if d.eng == rec.eng:
                        if rec.dma:
                            d.signal = True
                        elif d.eng == "pe":
                            continue
                        elif kind in ("raw", "x") or d.eng == "pool":
                            d.signal = True
                    else:
                        d.signal = True
        for e in ENGS:
            c = 0
            for rec in self.streams[e]:
                if rec.signal and not rec.dma:
                    c += 1
                    rec.sigval = c
        with contextlib.ExitStack() as es:
            esem = {e: es.enter_context(nc.semaphore("S_" + e)) for e in ENGS}
            dsem = [es.enter_context(nc.semaphore("D%d" % i)) for i in range(self.NS)]
            block = es.enter_context(nc.Block())
            hw = {"pe": "tensor", "act": "scalar", "dve": "vector", "pool": "gpsimd", "sp": "sync"}

            def make(ename):
                stream = self.streams[ename]

                def body(eng):
                    seen = {}
                    for rec in stream:
                        need = {}
                        for (d, kind) in rec.deps:
                            if d.dma:
                                key = ("d", d.dma_id % self.NS)
                                val = 16 * (d.dma_id // self.NS + 1)
                            else:
                                if d.eng == rec.eng and not rec.dma:
                                    if d.eng == "pe" or (kind not in ("raw", "x") and d.eng != "pool"):
                                        continue
                                key = ("e", d.eng)
                                val = d.sigval
                            if need.get(key, 0) < val:
                                need[key] = val
                        for key, val in need.items():
                            if seen.get(key, 0) >= val:
                                continue
                            seen[key] = val
                            sem = dsem[key[1]] if key[0] == "d" else esem[key[1]]
                            eng.wait_ge(sem, val)
                        if rec.fn is None:
                            continue
                        name, a_, k_ = rec.fn
                        ins = getattr(eng, name)(*a_, **k_)
                        if rec.dma:
                            ins.then_inc(dsem[rec.dma_id % self.NS], 16)
                        elif rec.signal:
                            ins.then_inc(esem[ename], 1)
                return body

            for ename in ENGS:
                if not self.streams[ename]:
                    continue
                getattr(block, hw[ename])(make(ename))


from concourse.bass_utils import run_bass_kernel_spmd

D = 1024
T = 4096
CT = 256
NT = 32
NS_ = 34
SEQ = 4352
INW = 6672
POOL_OFF, Q_OFF, K_OFF, V_OFF, O_OFF, IF_OFF, GATE_OFF = 0, 512, 1536, 2560, 3584, 4608, 4624
NE = 16
CAP = 512
EPS = 1e-6
WIN_BLOCKS = [(0, 1536), (1536, 2560), (2560, 3584), (3584, 4624), (4624, 5648), (5648, 6672)]


def win_buf(K, c0):
    for i, (a, b) in enumerate(WIN_BLOCKS):
        if a <= c0 < b:
            return K.Bwin[i]
    raise ValueError(c0)

C_ID, C_TRI, C_TRIT, C_MSUM, C_ONES = 0, 128, 256, 384, 512
C_IOJ = 640
C_TIDX = 1152
C_IOP = 1664
C_SEL = 1665
NCONST = 1665 + 512


def make_consts():
    c = np.zeros((128, NCONST), np.float32)
    p = np.arange(128)
    c[:, C_ID:C_ID + 128] = np.eye(128)
    c[:, C_TRI:C_TRI + 128] = (p[:, None] <= p[None, :])
    c[:, C_TRIT:C_TRIT + 128] = (p[:, None] >= p[None, :])
    c[:, C_MSUM:C_MSUM + 128] = ((p[:, None] % 16) == (p[None, :] % 16))
    c[:, C_ONES:C_ONES + 128] = 1.0
    c[:, C_IOJ:C_IOJ + 512] = np.arange(512)[None, :]
    c[:, C_TIDX:C_TIDX + 512] = (np.arange(512) // 16)[None, :]
    c[:, C_IOP] = p
    for h in range(4):
        c[h, C_SEL + h * 128:C_SEL + (h + 1) * 128] = 1.0
    return c


def make_invcnt():
    out = np.zeros((4, 64, 64), np.float32)
    for gi, s in enumerate((2, 4, 8, 16)):
        lo, hi = s // 2, s - s // 2
        r = np.arange(64)
        r0, r1 = np.clip(r - lo, 0, 64), np.clip(r + hi, 0, 64)
        cnt = (r1 - r0)[:, None] * (r1 - r0)[None, :]
        out[gi] = 1.0 / cnt
    return out.reshape(4, 4096)


class Ctx:
    pass


def build(stop_after=99, debug=False):
    nc = bass.Bass("TRN2", target_bir_lowering=False)
    S = Sched(nc, n_dma_sems=56)
    K = Ctx()
    K.nc, K.S = nc, S

    def din(name, shape, dt=F32):
        return nc.dram_tensor(name, list(shape), dt, kind="ExternalInput").ap()

    def dscr(name, shape, dt):
        if debug:
            return nc.dram_tensor(name, list(shape), dt, kind="ExternalOutput").ap()
        return nc.dram_tensor(name, list(shape), dt).ap()

    I = {}
    I["x"] = din("x", [T, D]); I["c"] = din("c", [D]); I["ctx"] = din("ctx", [CT, D]); I["c_ctx"] = din("c_ctx", [D])
    I["w_mod"] = din("w_mod", [D, 6 * D]); I["b_mod"] = din("b_mod", [6 * D])
    I["norm1_g"] = din("norm1_g", [D]); I["norm2_g"] = din("norm2_g", [D])
    I["w_in"] = din("w_in", [D, INW]); I["conv_w"] = din("conv_w", [5, 2 * D]); I["conv_b"] = din("conv_b", [2 * D])
    I["b_if"] = din("b_if", [16]); I["pool_mix"] = din("pool_mix", [4, 128, 128]); I["pool_scale"] = din("pool_scale", [512])
    I["mlstm_norm_g"] = din("mlstm_norm_g", [D]); I["w_pool_out"] = din("w_pool_out", [512, D])
    I["w_mlstm_out"] = din("w_mlstm_out", [D, D]); I["w_out"] = din("w_out", [D, D]); I["w_router"] = din("w_router", [D, NE])
    I["w_gate"] = din("w_gate", [NE, D, D]); I["w_up"] = din("w_up", [NE, D, D]); I["w_down"] = din("w_down", [NE, D, D])
    I["final_g"] = din("final_g", [D]); I["consts"] = din("consts", [128, NCONST]); I["invcnt"] = din("invcnt", [4, 4096])
    out_d = nc.dram_tensor("out", [T, D], F32, kind="ExternalOutput").ap()

    Dm = {}
    Dm["u_pool"] = dscr("u_pool", [512, T], F32)
    Dm["qk_raw"] = dscr("qk_raw", [2048, T], BF16)
    Dm["kc_raw"] = dscr("kc_raw", [1024, CT], BF16)
    Dm["qk_act"] = dscr("qk_act", [2048, T], BF16)
    Dm["kc_act"] = dscr("kc_act", [1024, CT], BF16)
    Dm["v_tok"] = dscr("v_tok", [SEQ, D], BF16)
    Dm["sigo"] = dscr("sigo", [D, T], BF16)
    Dm["sigg"] = dscr("sigg", [2 * D, T], BF16)
    Dm["gates"] = dscr("gates", [4, 4, SEQ], F32)
    Dm["bvec"] = dscr("bvec", [4, D], F32)
    Dm["h_f"] = dscr("h_f", [T, D], F32)
    Dm["hn"] = dscr("hn", [T, D], BF16)
    Dm["x1"] = dscr("x1", [T, D], F32)
    Dm["h2"] = dscr("h2", [T + 128, D], BF16)
    Dm["yacc"] = dscr("yacc", [T + 128, D], F32)
    DB = {k: Buf("d_" + k) for k in Dm}
    dbg = {}
    if debug:
        dbg["modT"] = nc.dram_tensor("dbg_modT", [128, 96], F32, kind="ExternalOutput").ap()
        dbg["tokS"] = nc.dram_tensor("dbg_tokS", [128, 2 * 3 * 34 * 4], F32, kind="ExternalOutput").ap()
        dbg["dec"] = nc.dram_tensor("dbg_dec", [128, 2 * 4 * 35], F32, kind="ExternalOutput").ap()

    with contextlib.ExitStack() as es0:
        def sbp(name, shape, dt):
            return es0.enter_context(nc.sbuf_tensor(name, list(shape), dt))
        cst = sbp("cst", [128, NCONST], F32); Bcst = Buf("cst")
        identb = sbp("identb", [128, 128], BF16); trib = sbp("trib", [128, 128], BF16)
        tritb = sbp("tritb", [128, 128], BF16); onesb = sbp("onesb", [128, 128], BF16)
        Bcb = Buf("cstb")
        modT = sbp("modT", [128, 48, 2], F32); Bmod = Buf("modT")
        vecs = sbp("vecs", [128, 8, 8], F32); Bvec = Buf("vecs")
        rstd1 = sbp("rstd1", [128, NS_], F32); Brstd1 = Buf("rstd1")
        rstd2 = sbp("rstd2", [128, NT], F32); Brstd2 = Buf("rstd2")
        tokS = sbp("tokS", [128, 2, 3, NS_, 4], F32); BtokS = Buf("tokS")
        decb = sbp("decb", [128, 2, 4, NS_ + 1], F32); Bdec = Buf("decb")
        ps_all = [es0.enter_context(nc.psum_tensor("ps%d" % i, [128, 512], F32)) for i in range(8)]
        Bps_all = [Buf("ps%d" % i) for i in range(8)]
        psF = ps_all[0:6]
        psB = [ps_all[6][:].bitcast(BF16), ps_all[7][:].bitcast(BF16)]
        BpsF = Bps_all[0:6]
        BpsB = Bps_all[6:8]
        rr = {"f": 0, "b": 0}

        def nextF():
            i = rr["f"]; rr["f"] = (i + 1) % 6
            return psF[i], BpsF[i]

        def nextB():
            i = rr["b"]; rr["b"] = (i + 1) % 2
            return psB[i], BpsB[i]

        S.dma("sp", cst[:], I["consts"], writes=[Bcst])
        S.dve(lambda e: e.tensor_copy(out=identb[:], in_=cst[:, C_ID:C_ID + 128]), reads=[Bcst], writes=[Bcb])
        S.dve(lambda e: e.tensor_copy(out=trib[:], in_=cst[:, C_TRI:C_TRI + 128]), reads=[Bcst], writes=[Bcb])
        S.dve(lambda e: e.tensor_copy(out=tritb[:], in_=cst[:, C_TRIT:C_TRIT + 128]), reads=[Bcst], writes=[Bcb])
        S.dve(lambda e: e.tensor_copy(out=onesb[:], in_=cst[:, C_ONES:C_ONES + 128]), reads=[Bcst], writes=[Bcb])
        identf = cst[:, C_ID:C_ID + 128]
        vpA = sbp("vpA", [128, 108], F32); vpB = sbp("vpB", [128, 80], F32); Bvp = Buf("vp")
        if True:
            es_win = contextlib.ExitStack()
            VA = es_win.enter_context(nc.sbuf_tensor("VA", [108, 128], F32)); VB = es_win.enter_context(nc.sbuf_tensor("VB", [80, 128], F32))
            BVA = Buf("VA"); BVB = Buf("VB")
            def rows(ap):
                return ap.rearrange("(k p) -> k p", p=128)
            for (r0, r1, src) in ((0, 8, rows(I["c"])), (8, 16, rows(I["c_ctx"])), (16, 64, rows(I["b_mod"])), (64, 72, rows(I["norm1_g"])),
                                  (72, 80, rows(I["norm2_g"])), (80, 84, rows(I["pool_scale"])), (84, 92, rows(I["mlstm_norm_g"])),
                                  (92, 108, rows(I["conv_b"]))):
                S.dma("sp", VA[r0:r1, :], src, writes=[BVA])
            S.dma("sp", VB[:, :], I["conv_w"].rearrange("j (c p) -> (j c) p", p=128), writes=[BVB])
            pvA, BpvA = nextF()
            S.pe(lambda e: e.transpose(out=pvA[:, 0:108], in_=VA[:, :], identity=cst[0:108, C_ID:C_ID + 108]), reads=[BVA, Bcst], writes=[BpvA])
            S.dve(lambda e: e.tensor_copy(out=vpA[:], in_=pvA[:, 0:108]), reads=[BpvA], writes=[Bvp])
            pvB, BpvB = nextF()
            S.pe(lambda e: e.transpose(out=pvB[:, 0:80], in_=VB[:, :], identity=cst[0:80, C_ID:C_ID + 80]), reads=[BVB, Bcst], writes=[BpvB])
            S.dve(lambda e: e.tensor_copy(out=vpB[:], in_=pvB[:, 0:80]), reads=[BpvB], writes=[Bvp])

        def dump(name, ap, bufs):
            if not debug:
                return
            t_ = nc.dram_tensor("dbg_" + name, list(ap.shape), ap.dtype, kind="ExternalOutput").ap()
            S.dma("sp", t_, ap, reads=bufs)
        win = es_win.enter_context(nc.sbuf_tensor("win", [128, 8, INW], BF16)); Bwin = [Buf("win%d" % i) for i in range(len(WIN_BLOCKS))]
        K.__dict__.update(locals())
        phase0(K)
        if stop_after >= 1:
            phase1(K)
        es_win.close()
        if stop_after >= 3:
            phase23(K)
        if stop_after >= 4:
            phase4(K)
        if stop_after >= 5:
            phase5(K)
        if stop_after >= 6:
            phase6(K)
        if debug:
            S.dma("sp", dbg["modT"], modT[:].rearrange("p a b -> p (a b)"), reads=[Bmod])
            S.dma("sp", dbg["tokS"], tokS[:].rearrange("p a b c d -> p (a b c d)"), reads=[BtokS])
            S.dma("sp", dbg["dec"], decb[:].rearrange("p a b c -> p (a b c)"), reads=[Bdec])
        S.fence()
        S.emit()
    return nc


def phase0(K):
    nc, S, I = K.nc, K.S, K.I
    with contextlib.ExitStack() as es:
        def sb(name, shape, dt):
            return es.enter_context(nc.sbuf_tensor(name, list(shape), dt))
        NCH = 3
        CW = 6 * D // NCH
        wmb = [sb("wm%d" % i, [128, 8, CW], BF16) for i in range(2)]; Bwmb = [Buf("wm") for i in range(2)]
        cs = sb("cs", [128, 8, 2], BF16); Bcs = Buf("cs")
        vpA, Bvp = K.vpA, K.Bvp
        bm = vpA[:, 16:64]; Bbm = Bvp
        Bg = Bvp
        S.act(lambda e: e.activation(out=cs[:, :, 0], in_=vpA[:, 0:8], func=AF.Silu), reads=[Bvp], writes=[Bcs])
        S.act(lambda e: e.activation(out=cs[:, :, 1], in_=vpA[:, 8:16], func=AF.Silu), reads=[Bvp], writes=[Bcs])
        pm, Bpm = K.nextF()
        for ch in range(NCH):
            wb, Bwb = wmb[ch % 2], Bwmb[ch % 2]
            S.dma("pool", wb[:], I["w_mod"][:, ch * CW:(ch + 1) * CW].rearrange("(k p) n -> p k n", p=128), writes=[Bwb])
            if ch == NCH - 1:
                for bi_, (c0, c1) in enumerate(WIN_BLOCKS):
                    S.dma("pool", K.win[:, :, c0:c1], I["w_in"][:, c0:c1].rearrange("(k p) n -> p k n", p=128), writes=[K.Bwin[bi_]])
            for ocl in range(CW // 128):
                oc = ch * (CW // 128) + ocl
                for k in range(8):
                    S.pe(lambda e, oc=oc, k=k, ocl=ocl, wb=wb: e.matmul(pm[:, oc * 2:oc * 2 + 2], lhsT=wb[:, k, ocl * 128:(ocl + 1) * 128],
                                                                         rhs=cs[:, k, :], start=(k == 0), stop=(k == 7)),
                         reads=[Bwb, Bcs], writes=[Bpm])
        modT, Bmod, vecs, Bvec = K.modT, K.Bmod, K.vecs, K.Bvec
        for col in range(2):
            S.dve(lambda e, col=col: e.tensor_tensor(out=modT[:, :, col], in0=pm[:, col:96:2], in1=bm, op=ALU.add),
                  reads=[Bpm, Bbm], writes=[Bmod])
        def mv(v, col):
            return modT[:, v * 8:(v + 1) * 8, col]
        for (kind, col) in ((0, 0), (2, 1)):
            S.dve(lambda e, kind=kind, col=col: e.scalar_tensor_tensor(out=vecs[:, kind, :], in0=mv(1, col), scalar=1.0, in1=vpA[:, 64:72],
                                                                        op0=ALU.add, op1=ALU.mult), reads=[Bmod, Bg], writes=[Bvec])
            S.dve(lambda e, kind=kind, col=col: e.tensor_copy(out=vecs[:, kind + 1, :], in_=mv(0, col)), reads=[Bmod], writes=[Bvec])
        S.dve(lambda e: e.tensor_copy(out=vecs[:, 4, :], in_=mv(2, 0)), reads=[Bmod], writes=[Bvec])
        S.dve(lambda e: e.scalar_tensor_tensor(out=vecs[:, 5, :], in0=mv(4, 0), scalar=1.0, in1=vpA[:, 72:80], op0=ALU.add, op1=ALU.mult),
              reads=[Bmod, Bg], writes=[Bvec])
        S.dve(lambda e: e.tensor_copy(out=vecs[:, 6, :], in_=mv(3, 0)), reads=[Bmod], writes=[Bvec])
        S.dve(lambda e: e.tensor_copy(out=vecs[:, 7, :], in_=mv(5, 0)), reads=[Bmod], writes=[Bvec])
        vrow = sb("vrow", [8, 4, 128], F32); Bvrow = Buf("vrow")
        pv, Bpv = K.nextF()
        for r, kind in enumerate((4, 5, 6, 7)):
            S.pe(lambda e, r=r, kind=kind: e.transpose(out=pv[0:8, r * 128:(r + 1) * 128], in_=vecs[:, kind, :], identity=K.cst[:, C_ID:C_ID + 128]),
                 reads=[Bvec, K.Bcst], writes=[Bpv])
        S.dve(lambda e: e.tensor_copy(out=vrow[:], in_=pv[0:8, 0:512].rearrange("k (r p) -> k r p", p=128)), reads=[Bpv], writes=[Bvrow])
        for r in range(4):
            S.dma("pool", K.Dm["bvec"][r].rearrange("(k p) -> k p", p=128), vrow[:, r, :], reads=[Bvrow])
        S.fence()


def phase1(K):
    nc, S, I, Dm, DB = K.nc, K.S, K.I, K.Dm, K.DB
    vecs, Bvec = K.vecs, K.Bvec
    with contextlib.ExitStack() as es:
        def sb(name, shape, dt):
            return es.enter_context(nc.sbuf_tensor(name, list(shape), dt))
        win, Bwin = K.win, K.Bwin
        xg = [[sb("xg%d_%d" % (a, i), [128, D], F32) for i in range(4)] for a in range(2)]
        Bxg = [[Buf("xg") for i in range(4)] for a in range(2)]
        junk = sb("junk1", [128, D], F32); Bjunk = Buf("junk1", sync_all=True)
        ssq = [sb("ssq1_%d" % a, [128, 4], F32) for a in range(2)]; Bssq = [Buf("ssq1") for a in range(2)]
        xn = [sb("xn%d" % i, [128, D], BF16) for i in range(2)]; Bxn = [Buf("xn%d" % i) for i in range(2)]
        hxT = [sb("hxT%d" % i, [128, 8, 512], BF16) for i in range(2)]; BhxT = [Buf("hxT%d" % i) for i in range(2)]
        stF = [sb("stF%d" % i, [128, 512], F32) for i in range(2)]; BstF = [Buf("stF%d" % i) for i in range(2)]
        stB = [sb("stB%d" % i, [128, 512], BF16) for i in range(6)]; BstB = [Buf("stB%d" % i) for i in range(6)]
        cnt = {"x": 0, "f": 0, "b": 0}

        def xsrc(c):
            return I["ctx"][c * 128:(c + 1) * 128, :] if c < 2 else I["x"][(c - 2) * 128:(c - 1) * 128, :]

        def stage_b():
            i = cnt["b"] % 6; cnt["b"] += 1
            return stB[i], BstB[i]

        def stage_f():
            i = cnt["f"] % 2; cnt["f"] += 1
            return stF[i], BstF[i]

        groups = [("ctx", [0, 1])] + [("lat", [2 + 4 * g + t for t in range(4)]) for g in range(8)]

        def prep(gi):
            a = gi % 2
            tiles_ = groups[gi][1]
            S.dve(lambda e: e.memset(ssq[a][:], 0.0), writes=[Bssq[a]])
            for ti, c in enumerate(tiles_):
                S.dma("sp", xg[a][ti][:], xsrc(c), writes=[Bxg[a][ti]])
                S.act(lambda e, ti=ti: e.activation(out=junk[:], in_=xg[a][ti][:], func=AF.Square, accum_out=ssq[a][:, ti:ti + 1]),
                      reads=[Bxg[a][ti]], writes=[Bssq[a], Bjunk])
            n_ = len(tiles_)
            S.dve(lambda e: e.tensor_scalar(out=ssq[a][:, 0:n_], in0=ssq[a][:, 0:n_], scalar1=1.0 / D, scalar2=EPS, op0=ALU.mult, op1=ALU.add),
                  reads=[Bssq[a]], writes=[Bssq[a]])
            S.act(lambda e: e.activation(out=ssq[a][:, 0:n_], in_=ssq[a][:, 0:n_], func=AF.Sqrt), reads=[Bssq[a]], writes=[Bssq[a]])
            S.dve(lambda e: e.reciprocal(out=K.rstd1[:, tiles_[0]:tiles_[0] + n_], in_=ssq[a][:, 0:n_]), reads=[Bssq[a]], writes=[K.Brstd1])

        def make_hx(gi):
            gkind, tiles = groups[gi]
            hb, Bhb = hxT[gi % 2], BhxT[gi % 2]
            kind = 2 if gkind == "ctx" else 0
            for ti, c in enumerate(tiles):
                xb, Bxb = xn[c % 2], Bxn[c % 2]
                S.dve(lambda e, c=c, xb=xb, ti=ti: e.tensor_scalar(out=xb[:], in0=xg[gi % 2][ti][:], scalar1=K.rstd1[:, c:c + 1], scalar2=None,
                                                                 op0=ALU.mult), reads=[Bxg[gi % 2][ti], K.Brstd1], writes=[Bxb])
                pt, Bpt = K.nextB()
                for j in range(8):
                    S.pe(lambda e, j=j, xb=xb, pt=pt: e.transpose(out=pt[:, j * 128:(j + 1) * 128], in_=xb[:, j * 128:(j + 1) * 128],
                                                                   identity=K.identb[:]), reads=[Bxb, K.Bcb], writes=[Bpt])
                for j in range(8):
                    S.act(lambda e, j=j, pt=pt, hb=hb, ti=ti: e.activation(out=hb[:, j, ti * 128:(ti + 1) * 128], in_=pt[:, j * 128:(j + 1) * 128],
                                                                            func=AF.Identity, scale=vecs[:, kind, j:j + 1], bias=vecs[:, kind + 1, j:j + 1]),
                          reads=[Bpt, Bvec], writes=[Bhb])

        prep(0)
        prep(1)
        make_hx(0)
        for gi, (gkind, tiles) in enumerate(groups):
            N = 128 * len(tiles)
            hb, Bhb = hxT[gi % 2], BhxT[gi % 2]
            seq0 = tiles[0] * 128
            tok0 = seq0 - CT

            def proj_fm(oc, M=128, col0=None):
                ps, Bps = K.nextF()
                c0 = oc * 128 if col0 is None else col0
                for k in range(8):
                    S.pe(lambda e, k=k, ps=ps, c0=c0, M=M: e.matmul(ps[0:M, 0:N], lhsT=win[:, k, c0:c0 + M], rhs=hb[:, k, 0:N],
                                                                       start=(k == 0), stop=(k == 7)), reads=[win_buf(K, c0), Bhb], writes=[Bps])
                return ps, Bps

            if gkind == "lat":
                for oc in range(4):
                    ps, Bps = proj_fm(oc)
                    st, Bst = stage_f()
                    S.dve(lambda e, ps=ps, st=st: e.tensor_copy(out=st[:, 0:N], in_=ps[:, 0:N]), reads=[Bps], writes=[Bst])
                    S.dma("pool", Dm["u_pool"][oc * 128:(oc + 1) * 128, tok0:tok0 + N], st[:, 0:N], reads=[Bst])
            qk_chunks = range(4, 20) if gkind == "lat" else range(12, 20)
            for oc in qk_chunks:
                ps, Bps = proj_fm(oc)
                st, Bst = stage_b()
                if oc % 2 == 0:
                    S.dve(lambda e, ps=ps, st=st: e.tensor_copy(out=st[:, 0:N], in_=ps[:, 0:N]), reads=[Bps], writes=[Bst])
                else:
                    S.act(lambda e, ps=ps, st=st: e.activation(out=st[:, 0:N], in_=ps[:, 0:N], func=AF.Identity), reads=[Bps], writes=[Bst])
                if gkind == "lat":
                    S.dma("pool", Dm["qk_raw"][(oc - 4) * 128:(oc - 3) * 128, tok0:tok0 + N], st[:, 0:N], reads=[Bst])
                else:
                    S.dma("pool", Dm["kc_raw"][(oc - 12) * 128:(oc - 11) * 128, 0:N], st[:, 0:N], reads=[Bst])
            if gi + 1 < len(groups):
                make_hx(gi + 1)
            if gi + 2 < len(groups):
                prep(gi + 2)
            for ti, c in enumerate(tiles):
                for half in range(2):
                    ps, Bps = K.nextF()
                    for k in range(8):
                        S.pe(lambda e, k=k, ps=ps, ti=ti, half=half: e.matmul(ps[:, :], lhsT=hb[:, k, ti * 128:(ti + 1) * 128],
                                                                             rhs=win[:, k, V_OFF + half * 512:V_OFF + (half + 1) * 512],
                                                                             start=(k == 0), stop=(k == 7)), reads=[win_buf(K, V_OFF + half * 512), Bhb], writes=[Bps])
                    st, Bst = stage_b()
                    S.dve(lambda e, ps=ps, st=st: e.tensor_copy(out=st[:], in_=ps[:]), reads=[Bps], writes=[Bst])
                    S.dma("pool", Dm["v_tok"][c * 128:(c + 1) * 128, half * 512:(half + 1) * 512], st[:], reads=[Bst])
            for q in range(4):
                ps, Bps = proj_fm(0, M=4, col0=IF_OFF + q * 4)
                st, Bst = stage_f()
                S.dve(lambda e, ps=ps, st=st: e.tensor_copy(out=st[0:4, 0:N], in_=ps[0:4, 0:N]), reads=[Bps], writes=[Bst])
                S.dma("pool", Dm["gates"][q, :, seq0:seq0 + N], st[0:4, 0:N], reads=[Bst])
            if gkind == "lat":
                for oc in range(28, 52):
                    ps, Bps = proj_fm(oc, col0=(O_OFF + (oc - 28) * 128) if oc < 36 else (GATE_OFF + (oc - 36) * 128))
                    st, Bst = stage_b()
                    S.act(lambda e, ps=ps, st=st: e.activation(out=st[:, 0:N], in_=ps[:, 0:N], func=AF.Sigmoid), reads=[Bps], writes=[Bst])
                    if oc < 36:
                        S.dma("pool", Dm["sigo"][(oc - 28) * 128:(oc - 27) * 128, tok0:tok0 + N], st[:, 0:N], reads=[Bst])
                    else:
                        S.dma("pool", Dm["sigg"][(oc - 36) * 128:(oc - 35) * 128, tok0:tok0 + N], st[:, 0:N], reads=[Bst])
        S.fence()


def phase2_gen(K):
    nc, S, I, Dm = K.nc, K.S, K.I, K.Dm
    es = K.es23
    if True:
        def sb(name, shape, dt):
            return es.enter_context(nc.sbuf_tensor(name, list(shape), dt))
        Bcw = K.Bvp; Bcbias = K.Bvp
        cb = K.vpA[:, 92:108]

        class _CW:
            def __getitem__(self, key):
                _, cc, js = key
                return K.vpB[:, js.start * 16 + cc:js.start * 16 + cc + 1]
        cw = _CW()
        dg = sb("dg", [128, 16, 5, 128], BF16); Bdg = Buf("dg")
        xp = [sb("xp%d" % i, [128, T + 4], BF16) for i in range(2)]; Bxp = [Buf("xp%d" % i) for i in range(2)]
        xc = [sb("xpc%d" % i, [128, CT + 4], BF16) for i in range(2)]; Bxc = [Buf("xpc%d" % i) for i in range(2)]
        so = [sb("so%d" % i, [128, T], BF16) for i in range(2)]; Bso = [Buf("so%d" % i) for i in range(2)]
        soc = [sb("soc%d" % i, [128, CT], BF16) for i in range(2)]; Bsoc = [Buf("soc%d" % i) for i in range(2)]
        for cc in range(16):
            for j in range(5):
                S.dve(lambda e, cc=cc, j=j: e.tensor_scalar(out=dg[:, cc, j, :], in0=K.cst[:, C_ID:C_ID + 128], scalar1=cw[:, cc, j:j + 1],
                                                             scalar2=None, op0=ALU.mult), reads=[K.Bcst, Bcw], writes=[Bdg])
        for i in range(2):
            S.dve(lambda e, i=i: e.memset(xp[i][:, 0:2], 0.0), writes=[Bxp[i]])
            S.dve(lambda e, i=i: e.memset(xp[i][:, T + 2:T + 4], 0.0), writes=[Bxp[i]])
            S.dve(lambda e, i=i: e.memset(xc[i][:, 0:2], 0.0), writes=[Bxc[i]])
            S.dve(lambda e, i=i: e.memset(xc[i][:, CT + 2:CT + 4], 0.0), writes=[Bxc[i]])
        for cc in range(16):
            b = cc % 2
            S.dma("sp", xp[b][:, 2:T + 2], Dm["qk_raw"][cc * 128:(cc + 1) * 128, :], writes=[Bxp[b]])
            for g in range(8):
                ps, Bps = K.nextF()
                for j in range(5):
                    S.pe(lambda e, j=j, g=g, ps=ps, b=b, cc=cc: e.matmul(ps[:, :], lhsT=dg[:, cc, j, :], rhs=xp[b][:, g * 512 + j:g * 512 + j + 512],
                                                                         start=(j == 0), stop=(j == 4)), reads=[Bdg, Bxp[b]], writes=[Bps])
                S.act(lambda e, g=g, ps=ps, b=b, cc=cc: e.activation(out=so[b][:, g * 512:(g + 1) * 512], in_=ps[:, :], func=AF.Silu,
                                                                     bias=cb[:, cc:cc + 1]), reads=[Bps, Bcbias], writes=[Bso[b]])
            S.dma("pool", Dm["qk_act"][cc * 128:(cc + 1) * 128, :], so[b][:], reads=[Bso[b]])
            yield
            if cc >= 8:
                S.dma("sp", xc[b][:, 2:CT + 2], Dm["kc_raw"][(cc - 8) * 128:(cc - 7) * 128, :], writes=[Bxc[b]])
                ps, Bps = K.nextF()
                for j in range(5):
                    S.pe(lambda e, j=j, ps=ps, b=b, cc=cc: e.matmul(ps[:, 0:CT], lhsT=dg[:, cc, j, :], rhs=xc[b][:, j:j + CT],
                                                                    start=(j == 0), stop=(j == 4)), reads=[Bdg, Bxc[b]], writes=[Bps])
                S.act(lambda e, ps=ps, b=b, cc=cc: e.activation(out=soc[b][:], in_=ps[:, 0:CT], func=AF.Silu, bias=cb[:, cc:cc + 1]),
                      reads=[Bps, Bcbias], writes=[Bsoc[b]])
                S.dma("pool", Dm["kc_act"][(cc - 8) * 128:(cc - 7) * 128, :], soc[b][:], reads=[Bsoc[b]])
        yield


def phase3_gen(K):
    import math
    nc, S, I, Dm = K.nc, K.S, K.I, K.Dm
    tokS, BtokS, decb, Bdec = K.tokS, K.BtokS, K.decb, K.Bdec
    es = K.es23
    if True:
        def sb(name, shape, dt):
            return es.enter_context(nc.sbuf_tensor(name, list(shape), dt))
        G2 = sb("G2", [4, 2, SEQ], F32); BG = Buf("G2")
        R2b = sb("R2", [4, 2, SEQ], F32); BR2b = Buf("R2")
        nbif = sb("nbif", [4, 4], F32)
        XB = sb("XB", [4, SEQ], F32); BXB = Buf("XB")
        W = sb("W3", [4, SEQ], F32); BW = Buf("W3")
        E3 = sb("E3", [4, SEQ], F32); BE = Buf("E3")
        bif = sb("bif", [4, 4], F32); Bbif = Buf("bif")
        ones4 = sb("ones4", [4, SEQ], F32); Bo4 = Buf("ones4")
        dl = sb("dl3", [4, NS_ + 1], F32); Bdl = Buf("dl3")
        lkt = sb("lkt", [4, 1], F32); Blk = Buf("lkt")
        S.dma("sp", bif[:], I["b_if"].rearrange("(q h) -> h q", h=4), writes=[Bbif], allow_slow_non_contiguous=True)
        S.dve(lambda e: e.memset(ones4[:], 1.0), writes=[Bo4])
        S.dve(lambda e: e.tensor_scalar(out=nbif[:], in0=bif[:], scalar1=-1.0, scalar2=None, op0=ALU.mult), reads=[Bbif], writes=[Bbif])
        S.dve(lambda e: e.memset(lkt[:], math.log(1.0 / 16.0)), writes=[Blk])
        for d in range(2):
            S.dma("sp", G2[:], Dm["gates"][2 * d:2 * d + 2].rearrange("q h s -> h q s"), writes=[BG])
            S.act(lambda e, d=d: e.activation(out=G2[:, 1, :], in_=G2[:, 1, :], func=AF.Exp, scale=-1.0, bias=nbif[:, 2 * d + 1:2 * d + 2]),
                  reads=[BG, Bbif], writes=[BG])
            S.act(lambda e: e.activation(out=G2[:, 1, :], in_=G2[:, 1, :], func=AF.Ln, bias=1.0), reads=[BG], writes=[BG])
            if d == 0:
                R2, BR = G2, BG
            else:
                R2, BR = R2b, BR2b
                for q in range(2):
                    S.dve(lambda e, q=q: e.tensor_copy(out=R2[:, q, 0:CT], in_=G2[:, q, 0:CT][:, ::-1]), reads=[BG], writes=[BR])
                    S.dve(lambda e, q=q: e.tensor_copy(out=R2[:, q, CT:SEQ], in_=G2[:, q, CT:SEQ][:, ::-1]), reads=[BG], writes=[BR])
            S.dve(lambda e: e.tensor_tensor_scan(out=XB[:], data0=ones4[:], data1=R2[:, 1, :], initial=0.0, op0=ALU.mult, op1=ALU.add),
                  reads=[BR, Bo4], writes=[BXB])
            S.dve(lambda e, d=d: e.scalar_tensor_tensor(out=R2[:, 0, :], in0=R2[:, 0, :], scalar=bif[:, 2 * d:2 * d + 1], in1=XB[:], op0=ALU.add, op1=ALU.add),
                  reads=[BR, BXB, Bbif], writes=[BR])
            S.dve(lambda e: e.tensor_tensor_scan(out=R2[:, 1, :], data0=ones4[:], data1=R2[:, 0, :], initial=-1e30, op0=ALU.mult, op1=ALU.max),
                  reads=[BR, Bo4], writes=[BR])
            S.dve(lambda e: e.memset(dl[:], 0.0), writes=[Bdl])
            S.dve(lambda e: e.tensor_tensor(out=dl[:, 1:NS_], in0=R2[:, 1, 127:SEQ - 128:128], in1=R2[:, 1, 255:SEQ:128], op=ALU.subtract),
                  reads=[BR], writes=[Bdl])
            yield
            A3 = R2[:, 0, :].rearrange("p (c t) -> p c t", t=128)
            G3 = R2[:, 1, :].rearrange("p (c t) -> p c t", t=128)
            W3 = W[:].rearrange("p (c t) -> p c t", t=128)
            X3 = XB[:].rearrange("p (c t) -> p c t", t=128)
            ge_b = G3[:, :, 127:128].to_broadcast([4, NS_, 128])
            gn_b = G3[:, 1:NS_, 127:128].to_broadcast([4, NS_ - 1, 128])
            gl_b = G3[:, NS_ - 1:NS_, 127:128].to_broadcast([4, 1, 128])
            S.dve(lambda e: e.tensor_tensor(out=W3, in0=A3, in1=ge_b, op=ALU.subtract), reads=[BR], writes=[BW])
            S.dve(lambda e: e.tensor_tensor(out=X3, in0=X3, in1=ge_b, op=ALU.subtract), reads=[BR, BXB], writes=[BXB])
            S.dve(lambda e: e.tensor_tensor(out=A3[:, 0:NS_ - 1, :], in0=A3[:, 0:NS_ - 1, :], in1=gn_b, op=ALU.subtract), reads=[BR], writes=[BR])
            S.dve(lambda e: e.tensor_tensor(out=A3[:, NS_ - 1:NS_, :], in0=A3[:, NS_ - 1:NS_, :], in1=gl_b, op=ALU.subtract), reads=[BR], writes=[BR])
            yield
            S.act(lambda e: e.activation(out=W[:], in_=W[:], func=AF.Exp, bias=lkt[:, 0:1]), reads=[BW, Blk], writes=[BW])
            S.act(lambda e: e.activation(out=R2[:, 0, :], in_=R2[:, 0, :], func=AF.Exp, bias=lkt[:, 0:1]), reads=[BR, Blk], writes=[BR])
            S.act(lambda e: e.activation(out=XB[:], in_=XB[:], func=AF.Exp), reads=[BXB], writes=[BXB])
            S.act(lambda e: e.activation(out=dl[:], in_=dl[:], func=AF.Exp), reads=[Bdl], writes=[Bdl])
            yield
            srcs = [(W, BW, W[:]), (R2, BR, R2[:, 0, :]), (XB, BXB, XB[:])]
            if d == 1:
                dsts = [(G2, BG, G2[:, 0, :]), (G2, BG, G2[:, 1, :]), (E3, BE, E3[:])]
                for (st, Bs, sap), (dt_, Bd, dap) in zip(srcs, dsts):
                    S.dve(lambda e, sap=sap, dap=dap: e.tensor_copy(out=dap[:, 0:CT], in_=sap[:, 0:CT][:, ::-1]), reads=[Bs], writes=[Bd])
                    S.dve(lambda e, sap=sap, dap=dap: e.tensor_copy(out=dap[:, CT:SEQ], in_=sap[:, CT:SEQ][:, ::-1]), reads=[Bs], writes=[Bd])
                srcs = dsts
            for k, (st, Bs, sap) in enumerate(srcs):
                ps, Bps = K.nextF()
                for c in range(NS_):
                    S.pe(lambda e, c=c, ps=ps, sap=sap: e.transpose(out=ps[:, c * 4:(c + 1) * 4], in_=sap[:, c * 128:(c + 1) * 128],
                                                                    identity=K.cst[0:4, C_ID:C_ID + 4]), reads=[Bs, K.Bcst], writes=[Bps])
                S.dve(lambda e, d=d, k=k, ps=ps: e.tensor_copy(out=tokS[:, d, k, :, :], in_=ps[:, 0:NS_ * 4].rearrange("p (c h) -> p c h", h=4)),
                      reads=[Bps], writes=[BtokS])
            ps, Bps = K.nextF()
            for h in range(4):
                S.pe(lambda e, h=h, ps=ps: e.matmul(ps[:, h * (NS_ + 1):(h + 1) * (NS_ + 1)], lhsT=K.cst[0:4, C_SEL + h * 128:C_SEL + (h + 1) * 128],
                                                    rhs=dl[:], start=True, stop=True), reads=[Bdl, K.Bcst], writes=[Bps])
            S.dve(lambda e, d=d, ps=ps: e.tensor_copy(out=decb[:, d, :, :], in_=ps[:, 0:4 * (NS_ + 1)].rearrange("p (h r) -> p h r", h=4)),
                  reads=[Bps], writes=[Bdec])
        yield


def phase2(K):
    pass


def phase3(K):
    pass


def phase23(K):
    K.es23 = contextlib.ExitStack()
    g2 = phase2_gen(K)
    g3 = phase3_gen(K)
    alive = [g2, g3]
    while alive:
        for g in list(alive):
            try:
                next(g)
            except StopIteration:
                alive.remove(g)
    K.S.fence()
    K.es23.close()


def phase4(K):
    nc, S, I, Dm = K.nc, K.S, K.I, K.Dm
    tokS, BtokS, decb, Bdec = K.tokS, K.BtokS, K.decb, K.Bdec
    psF, psB = K.psF, K.psB
    with contextlib.ExitStack() as es:
        def sb(name, shape, dt):
            return es.enter_context(nc.sbuf_tensor(name, list(shape), dt))
        Va = sb("Va", [128, NS_, 4, 257], BF16); BVaT = [Buf("Va%d" % c) for c in range(NS_)]
        S.dve(lambda e: e.memset(Va[:, :, :, 256:257], 1.0), writes=BVaT)
        va_loaded = set()

        def va_load(c):
            if c in va_loaded:
                return
            va_loaded.add(c)
            S.dma("sp", Va[:, c, :, 0:256], Dm["v_tok"][c * 128:(c + 1) * 128, :].rearrange("p (h e) -> p h e", h=4), writes=[BVaT[c]])
        maskf = K.cst[:, C_TRI:C_TRI + 128]
        maskb = K.cst[:, C_TRIT:C_TRIT + 128]
        NB = 3
        qt = [[sb("qt%d_%d" % (d, i), [128, 8, 128], BF16) for i in range(NB)] for d in range(2)]
        kt = [[sb("kt%d_%d" % (d, i), [128, 8, 128], BF16) for i in range(NB)] for d in range(2)]
        ktok = [[sb("ktok%d_%d" % (d, i), [128, D], BF16) for i in range(NB)] for d in range(2)]
        Bktok = [[Buf("ktok") for i in range(NB)] for d in range(2)]
        ps, Bps = K.ps_all, K.Bps_all
        kTb = ps[7][:].bitcast(BF16)
        Bqt = [[Buf("qt") for i in range(NB)] for d in range(2)]
        Bkt = [[Buf("kt") for i in range(NB)] for d in range(2)]
        S32 = [[sb("S32_%d_%d" % (d, h), [128, 2, 257], F32) for h in range(4)] for d in range(2)]
        Sbf = [[sb("Sbf_%d_%d" % (d, h), [128, 2, 257], BF16) for h in range(4)] for d in range(2)]
        BS32 = [[Buf("S32") for h in range(4)] for d in range(2)]
        BSbf = [[Buf("Sbf") for h in range(4)] for d in range(2)]
        NR = 6
        Sm = [sb("Sm%d" % i, [128, 128], BF16) for i in range(NR)]; BSm = [Buf("Sm") for i in range(NR)]
        ktl = [sb("ktl%d" % i, [128, 256], BF16) for i in range(NR)]; Bktl = [Buf("ktl") for i in range(NR)]
        den = [sb("den%d" % i, [128, 2], F32) for i in range(NR)]; Bden = [Buf("den") for i in range(NR)]
        hb = [sb("hb%d" % i, [128, D], F32) for i in range(4)]; Bhb = [Buf("hb") for i in range(4)]
        hl = [sb("hl%d" % i, [128, D], F32) for i in range(3)]; Bhl = [Buf("hl") for i in range(3)]
        hnb = [sb("hnb%d" % i, [128, D], BF16) for i in range(2)]; Bhnb = [Buf("hnb") for i in range(2)]
        junk = sb("junk4", [128, 256], F32); Bjunk = Buf("junk4", sync_all=True)
        ssq = [sb("ssq4_%d" % i, [128, 4], F32) for i in range(4)]; Bssq = [Buf("ssq4") for i in range(4)]
        epi = {"n": 0, "q": [], "it": 0}
        Bh = [Buf("h_f%d" % c) for c in range(NS_)]
        first_done = [False] * NS_
        BsS = [K.Bps_all[0], K.Bps_all[1]]
        Bnum = [K.Bps_all[2], K.Bps_all[3], K.Bps_all[4]]
        BP = [K.Bps_all[5], K.Bps_all[6]]
        for d in range(2):
            for h in range(4):
                S.dve(lambda e, d=d, h=h: e.memset(S32[d][h][:], 0.0), writes=[BS32[d][h]])
                S.dve(lambda e, d=d, h=h: e.memset(Sbf[d][h][:], 0.0), writes=[BSbf[d][h]])
        items = []
        rot = {"hb": 0, "hl": 0}
        for step in range(NS_):
            for d in range(2):
                c = step if d == 0 else ((1 - step) if step < 2 else (35 - step))
                for h in range(4):
                    items.append(dict(step=step, d=d, c=c, h=h, lat=(c >= 2), n=len(items)))

        def tile_loads(step, d):
            c = step if d == 0 else ((1 - step) if step < 2 else (35 - step))
            bi = step % NB
            va_load(c)
            if c >= 2:
                tk = (c - 2) * 128
                S.dma("sp", qt[d][bi][:], Dm["qk_act"][0:D, tk:tk + 128].rearrange("(j p) t -> p j t", p=128), writes=[Bqt[d][bi]])
                S.dma("sp", kt[d][bi][:], Dm["qk_act"][D:2 * D, tk:tk + 128].rearrange("(j p) t -> p j t", p=128), writes=[Bkt[d][bi]])
            else:
                S.dma("sp", kt[d][bi][:], Dm["kc_act"][:, c * 128:(c + 1) * 128].rearrange("(j p) t -> p j t", p=128), writes=[Bkt[d][bi]])

        def tile_setup(it):
            step, d, c = it["step"], it["d"], it["c"]
            bi = step % NB
            if step == 0:
                tile_loads(step, d)
            if d == 0 and step + 1 < NS_:
                tile_loads(step + 1, 0)
                tile_loads(step + 1, 1)
            info = dict(bi=bi)
            if step < NS_ - 1:
                for j in range(8):
                    S.pe(lambda e, j=j: e.transpose(out=kTb[:, j * 128:(j + 1) * 128], in_=kt[d][bi][:, j, :], identity=K.identb[:]),
                         reads=[Bkt[d][bi], K.Bcb], writes=[Bps[7]])
                S.act(lambda e: e.activation(out=ktok[d][bi][:], in_=kTb[:, :], func=AF.Copy), reads=[Bps[7]], writes=[Bktok[d][bi]])
            if it["lat"]:
                hi = rot["hb"] % 4; rot["hb"] += 1
                info["hi"] = hi
                info["second"] = first_done[c]
                if info["second"]:
                    li_ = rot["hl"] % 3; rot["hl"] += 1
                    info["li"] = li_
                    info["deferred"] = Bh[c].last_w is None
                    if not info["deferred"]:
                        S.dma("sp", hl[li_][:], Dm["h_f"][(c - 2) * 128:(c - 1) * 128, :], reads=[Bh[c]], writes=[Bhl[li_]])
                else:
                    first_done[c] = True
            return info

        tinfo = {}

        def stageA(it):
            step, d, c, h, n = it["step"], it["d"], it["c"], it["h"], it["n"]
            if h == 0:
                tinfo[(step, d)] = tile_setup(it)
            bi = tinfo[(step, d)]["bi"]
            q_, k_, Bq_, Bk_ = qt[d][bi], kt[d][bi], Bqt[d][bi], Bkt[d][bi]
            sl = n % 2
            if it["lat"]:
                for j in range(2):
                    S.pe(lambda e, j=j: e.matmul(psF[sl][:, 0:128], lhsT=k_[:, 2 * h + j, :], rhs=q_[:, 2 * h + j, :],
                                                 start=(j == 0), stop=(j == 1)), reads=[Bq_, Bk_], writes=[BsS[sl]])

        def stageB(it):
            step, d, c, h, n = it["step"], it["d"], it["c"], it["h"], it["n"]
            sl = n % 2; r = n % NR
            mask = maskf if d == 0 else maskb
            if it["lat"]:
                S.dve(lambda e: e.scalar_tensor_tensor(out=Sm[r][:], in0=psF[sl][:, 0:128], scalar=tokS[:, d, 0, c, h:h + 1], in1=mask,
                                                       op0=ALU.mult, op1=ALU.mult), reads=[BsS[sl], BtokS, K.Bcst], writes=[BSm[r]])
            if step < NS_ - 1:
                bi = tinfo[(step, d)]["bi"]
                S.act(lambda e: e.activation(out=ktl[r][:], in_=ktok[d][bi][:, h * 256:(h + 1) * 256], func=AF.Copy, scale=tokS[:, d, 1, c, h:h + 1]),
                      reads=[Bktok[d][bi], BtokS], writes=[Bktl[r]])

        def stageC(it):
            step, d, c, h, n = it["step"], it["d"], it["c"], it["h"], it["n"]
            bi = tinfo[(step, d)]["bi"]
            q_, Bq_ = qt[d][bi], Bqt[d][bi]
            r = n % NR; st = n % 3; sp_ = n % 2
            pn = ps[2 + st]; pp = ps[5 + sp_]
            if it["lat"]:
                for j in range(2):
                    S.pe(lambda e, j=j: e.matmul(pn[:, 0:257], lhsT=q_[:, 2 * h + j, :], rhs=Sbf[d][h][:, j, :], start=(j == 0), stop=False),
                         reads=[Bq_, BSbf[d][h]], writes=[Bnum[st]])
                S.pe(lambda e: e.matmul(pn[:, 0:257], lhsT=Sm[r][:], rhs=Va[:, c, h, :], start=False, stop=True),
                     reads=[BSm[r], BVaT[c]], writes=[Bnum[st]])
            if step < NS_ - 1:
                for j in range(2):
                    S.pe(lambda e, j=j: e.matmul(pp[:, j * 256:(j + 1) * 256], lhsT=ktl[r][:, j * 128:(j + 1) * 128], rhs=Va[:, c, h, 0:256], start=True, stop=True),
                         reads=[Bktl[r], BVaT[c]], writes=[BP[sp_]])
                    S.pe(lambda e, j=j: e.matmul(pn[:, 260 + j:261 + j], lhsT=ktl[r][:, j * 128:(j + 1) * 128], rhs=Va[:, c, h, 256:257], start=True, stop=True),
                         reads=[Bktl[r], BVaT[c]], writes=[Bnum[st]])

        def stageD1(it):
            step, d, c, h, n = it["step"], it["d"], it["c"], it["h"], it["n"]
            r = n % NR; st = n % 3; sp_ = n % 2
            pn = ps[2 + st]; pp = ps[5 + sp_]
            upd = step < NS_ - 1
            dk = decb[:, d, h, step + 1:step + 2] if upd else None
            if it["lat"]:
                S.dve(lambda e: e.tensor_scalar(out=den[r][:, 0:1], in0=pn[:, 256:257], scalar1=tokS[:, d, 2, c, h:h + 1], scalar2=None, op0=ALU.max),
                      reads=[Bnum[st], BtokS], writes=[Bden[r]])
            if upd:
                S.dve(lambda e: e.scalar_tensor_tensor(out=S32[d][h][:, :, 0:256], in0=S32[d][h][:, :, 0:256], scalar=dk,
                                                       in1=pp[:, 0:512].rearrange("p (j e) -> p j e", j=2), op0=ALU.mult, op1=ALU.add),
                      reads=[BP[sp_], Bdec, BS32[d][h]], writes=[BS32[d][h]])
            if it["lat"]:
                S.dve(lambda e: e.scalar_tensor_tensor(out=den[r][:, 0:1], in0=pn[:, 256:257], scalar=-1.0, in1=den[r][:, 0:1], op0=ALU.mult, op1=ALU.max),
                      reads=[Bnum[st], Bden[r]], writes=[Bden[r]])
            if upd:
                S.dve(lambda e: e.scalar_tensor_tensor(out=S32[d][h][:, :, 256], in0=S32[d][h][:, :, 256], scalar=dk,
                                                       in1=pn[:, 260:262], op0=ALU.mult, op1=ALU.add),
                      reads=[Bnum[st], Bdec, BS32[d][h]], writes=[BS32[d][h]])
                S.pool(lambda e: e.tensor_copy(out=Sbf[d][h][:], in_=S32[d][h][:]), reads=[BS32[d][h]], writes=[BSbf[d][h]])
            if it["lat"]:
                S.dve(lambda e: e.reciprocal(out=den[r][:, 1:2], in_=den[r][:, 0:1]), reads=[Bden[r]], writes=[Bden[r]])

        def stageD2(it):
            step, d, c, h, n = it["step"], it["d"], it["c"], it["h"], it["n"]
            r = n % NR; st = n % 3
            pn = ps[2 + st]
            ti = tinfo[(step, d)]
            if it["lat"] and h == 0 and ti["second"] and ti["deferred"]:
                assert Bh[c].last_w is not None
                S.dma("sp", hl[ti["li"]][:], Dm["h_f"][(c - 2) * 128:(c - 1) * 128, :], reads=[Bh[c]], writes=[Bhl[ti["li"]]])
            if it["lat"]:
                hbuf, Bhbuf = hb[ti["hi"]], Bhb[ti["hi"]]
                if not ti["second"]:
                    S.act(lambda e: e.activation(out=hbuf[:, h * 256:(h + 1) * 256], in_=pn[:, 0:256], func=AF.Copy, scale=den[r][:, 1:2]),
                          reads=[Bnum[st], Bden[r]], writes=[Bhbuf])
                else:
                    li_ = ti["li"]
                    S.dve(lambda e: e.scalar_tensor_tensor(out=hbuf[:, h * 256:(h + 1) * 256], in0=pn[:, 0:256], scalar=den[r][:, 1:2],
                                                           in1=hl[li_][:, h * 256:(h + 1) * 256], op0=ALU.mult, op1=ALU.add),
                          reads=[Bnum[st], Bden[r], Bhl[li_]], writes=[Bhbuf])
            if it["lat"] and h == 3:
                hbuf, Bhbuf = hb[ti["hi"]], Bhb[ti["hi"]]
                if not ti["second"]:
                    S.dma("pool", Dm["h_f"][(c - 2) * 128:(c - 1) * 128, :], hbuf[:], reads=[Bhbuf], writes=[Bh[c]])
                else:
                    si = epi["n"] % 4; epi["n"] += 1
                    cc = c

                    def e1():
                        S.dve(lambda e: e.memset(ssq[si][:], 0.0), writes=[Bssq[si]])
                        for hh in range(4):
                            S.act(lambda e, hh=hh: e.activation(out=junk[:], in_=hbuf[:, hh * 256:(hh + 1) * 256], func=AF.Square, accum_out=ssq[si][:, hh:hh + 1]),
                                  reads=[Bhbuf], writes=[Bssq[si], Bjunk])

                    def e2():
                        S.dve(lambda e: e.tensor_scalar(out=ssq[si][:], in0=ssq[si][:], scalar1=1.0 / 256, scalar2=EPS, op0=ALU.mult, op1=ALU.add),
                              reads=[Bssq[si]], writes=[Bssq[si]])

                    def e3():
                        S.act(lambda e: e.activation(out=ssq[si][:], in_=ssq[si][:], func=AF.Sqrt), reads=[Bssq[si]], writes=[Bssq[si]])

                    def e4():
                        S.dve(lambda e: e.reciprocal(out=ssq[si][:], in_=ssq[si][:]), reads=[Bssq[si]], writes=[Bssq[si]])

                    def e5():
                        for hh in range(4):
                            S.act(lambda e, hh=hh: e.activation(out=hnb[si % 2][:, hh * 256:(hh + 1) * 256], in_=hbuf[:, hh * 256:(hh + 1) * 256],
                                                                func=AF.Copy, scale=ssq[si][:, hh:hh + 1]),
                                  reads=[Bhbuf, Bssq[si]], writes=[Bhnb[si % 2]])
                        S.dma("pool", Dm["hn"][(cc - 2) * 128:(cc - 1) * 128, :], hnb[si % 2][:], reads=[Bhnb[si % 2]])
                    for k_, fn_ in enumerate((e1, e2, e3, e4, e5)):
                        epi["q"].append((epi["it"] + k_, fn_))

        NI = len(items)
        stageA(items[0]); stageA(items[1]); stageB(items[0])
        for n in range(NI + 2):
            if n + 2 < NI:
                stageA(items[n + 2])
            if n + 1 < NI:
                stageB(items[n + 1])
            if n < NI:
                stageC(items[n])
            if 1 <= n <= NI:
                stageD1(items[n - 1])
            epi["it"] = n
            if 2 <= n <= NI + 1:
                stageD2(items[n - 2])
            due = [f for (t_, f) in epi["q"] if t_ <= n]
            epi["q"] = [(t_, f) for (t_, f) in epi["q"] if t_ > n]
            for f in due:
                f()
        for (t_, f) in sorted(epi["q"], key=lambda x: x[0]):
            f()
        S.fence()


def phase5(K):
    nc, S, I, Dm = K.nc, K.S, K.I, K.Dm
    with contextlib.ExitStack() as es:
        def sb(name, shape, dt):
            return es.enter_context(nc.sbuf_tensor(name, list(shape), dt))
        aT = [sb("aT%d" % g, [128, T], BF16) for g in range(4)]; BaT = [Buf("aT") for g in range(4)]
        wpo = sb("wpo", [128, 4, D], BF16); wmo = sb("wmo", [128, 8, D], BF16); wout = sb("wout", [128, 8, D], BF16)
        Bw5 = Buf("w5")
        S.dma("pool", wpo[:], I["w_pool_out"].rearrange("(g p) n -> p g n", p=128), writes=[Bw5])
        S.dma("pool", wmo[:], I["w_mlstm_out"].rearrange("(g p) n -> p g n", p=128), writes=[Bw5])
        S.dma("pool", wout[:], I["w_out"].rearrange("(g p) n -> p g n", p=128), writes=[Bw5])
        mixb = sb("mixb", [128, 4, 128], BF16); Bmix = Buf("mixb")
        S.dma("pool", mixb[:], I["pool_mix"].rearrange("g c d -> c g d"), writes=[Bmix])
        psc = K.vpA[:, 80:84]; Bpsc = K.Bvp
        gmn = K.vpA[:, 84:92]; Bgmn = K.Bvp
        g1bc = sb("g1bc", [128, D], F32); Bg1 = Buf("g1bc")
        S.dma("sp", g1bc[:], Dm["bvec"][0:1, :].partition_broadcast(128), writes=[Bg1])
        with contextlib.ExitStack() as es2:
            def sb2(name, shape, dt):
                return es2.enter_context(nc.sbuf_tensor(name, list(shape), dt))
            U = sb2("U5", [128, 80, 80], F32); BU = Buf("U5")
            PA = sb2("PA5", [128, 80, 80], F32); BPA = Buf("PA5")
            PB = sb2("PB5", [128, 80, 80], F32); BPB = Buf("PB5")
            icn = sb2("icn", [128, T], F32); Bicn = Buf("icn")
            tmp = sb2("tmp5", [128, T], F32); Btmp = Buf("tmp5")
            apre = sb2("apre", [128, T], BF16); Bap = Buf("apre")
            S.dve(lambda e: e.memset(U[:], 0.0), writes=[BU])
            for gq in range(4):
                n = gq + 1
                if gq == 1:
                    for j in range(8):
                        S.dve(lambda e, j=j: e.tensor_scalar(out=wmo[:, j, :], in0=wmo[:, j, :], scalar1=gmn[:, j:j + 1], scalar2=None, op0=ALU.mult),
                              reads=[Bw5, Bgmn], writes=[Bw5])
                    for j in range(8):
                        S.dve(lambda e, j=j: e.tensor_tensor(out=wout[:, j, :], in0=wout[:, j, :], in1=g1bc[:], op=ALU.mult),
                              reads=[Bw5, Bg1], writes=[Bw5])
                S.dma("sp", tmp[:], Dm["u_pool"][gq * 128:(gq + 1) * 128, :], writes=[Btmp])
                S.act(lambda e: e.activation(out=U[:, 8:72, 8:72], in_=tmp[:].rearrange("p (r c) -> p r c", c=64), func=AF.Copy),
                      reads=[Btmp], writes=[BU])
                S.dma("sp", icn[:], I["invcnt"][gq:gq + 1, :].partition_broadcast(128), writes=[Bicn])
                lo = [0] * (n + 1); hi = [0] * (n + 1)
                lo[n], hi[n] = 0, 64
                for k in range(n - 1, 0, -1):
                    sh = 2 ** (k - 1)
                    lo[k], hi[k] = lo[k + 1] - sh, hi[k + 1] + sh
                bufs = [(PA, BPA), (PB, BPB)]
                src, Bsrc = U, BU
                bi = 0
                for axis in (1, 0):
                    for k in range(1, n + 1):
                        dst, Bdst = bufs[bi]; bi ^= 1
                        a, b = lo[k] + 8, hi[k] + 8
                        if k == 1:
                            s0, s1 = -1, 0
                        else:
                            s0, s1 = -(2 ** (k - 2)), 2 ** (k - 2)
                        if axis == 1:
                            o = dst[:, :, a:b]; i0 = src[:, :, a + s0:b + s0]; i1 = src[:, :, a + s1:b + s1]
                        else:
                            o = dst[:, a:b, 8:72]; i0 = src[:, a + s0:b + s0, 8:72]; i1 = src[:, a + s1:b + s1, 8:72]
                        S.dve(lambda e, o=o, i0=i0, i1=i1: e.tensor_tensor(out=o, in0=i0, in1=i1, op=ALU.add), reads=[Bsrc], writes=[Bdst])
                        src, Bsrc = dst, Bdst
                S.dve(lambda e: e.tensor_tensor(out=tmp[:].rearrange("p (r c) -> p r c", c=64), in0=src[:, 8:72, 8:72],
                                                in1=icn[:].rearrange("p (r c) -> p r c", c=64), op=ALU.mult), reads=[Bsrc, Bicn], writes=[Btmp])
                S.dve(lambda e: e.tensor_tensor(out=apre[:].rearrange("p (r c) -> p r c", c=64), in0=tmp[:].rearrange("p (r c) -> p r c", c=64),
                                                in1=U[:, 8:72, 8:72], op=ALU.subtract), reads=[Btmp, BU], writes=[Bap])
                for g in range(8):
                    ps, Bps = K.nextF()
                    S.pe(lambda e: e.matmul(ps[:, :], lhsT=mixb[:, gq, :], rhs=apre[:, g * 512:(g + 1) * 512], start=True, stop=True),
                         reads=[Bmix, Bap], writes=[Bps])
                    S.act(lambda e: e.activation(out=aT[gq][:, g * 512:(g + 1) * 512], in_=ps[:, :], func=AF.Copy, scale=psc[:, gq:gq + 1]),
                          reads=[Bps, Bpsc], writes=[BaT[gq]])
        S.fence()
        for gq in range(4):
            K.dump("aT%d" % gq, aT[gq][:], [BaT[gq]])
        sga = sb("sga", [128, 8, 512], BF16); Bsga = Buf("sga")
        sgm = sb("sgm", [128, 8, 512], BF16); Bsgm = Buf("sgm")
        sgo = [sb("sgo%d" % i, [128, 8, 512], BF16) for i in range(2)]; Bsgo = [Buf("sgo") for i in range(2)]
        hnt = [sb("hnt%d" % i, [128, D], BF16) for i in range(8)]; Bhnt = [Buf("hnt") for i in range(8)]
        xt = [sb("x5_%d" % i, [128, D], F32) for i in range(2)]; Bxt = [Buf("x5") for i in range(2)]
        x1t = [sb("x1t%d" % i, [128, D], F32) for i in range(2)]; Bx1 = [Buf("x1t") for i in range(2)]
        mixA = sb("mixA", [128, 8, 512], BF16); BmixA = Buf("mixA")
        mTb = [sb("mT%d" % i, [128, 8, 512], BF16) for i in range(2)]; BmTb = [Buf("mT") for i in range(2)]
        tmpm = [sb("tmpm%d" % i, [128, 512], F32) for i in range(2)]; Btmpm = [Buf("tmpm") for i in range(2)]
        mixT = sb("mixT", [128, 8, 512], BF16); BmixT = Buf("mixT")
        junk = sb("junk5", [128, D], F32); Bjunk = Buf("junk5", sync_all=True)
        ssq = sb("ssq5", [128, NT], F32); Bssq = Buf("ssq5")
        S.dve(lambda e: e.memset(ssq[:], 0.0), writes=[Bssq])

        def loads_b(g):
            t0 = g * 512
            S.dma("sp", sgo[g % 2][:], Dm["sigo"][:, t0:t0 + 512].rearrange("(j p) t -> p j t", p=128), writes=[Bsgo[g % 2]])
            for ti in range(4):
                hi_ = (g % 2) * 4 + ti
                S.dma("sp", hnt[hi_][:], Dm["hn"][t0 + ti * 128:t0 + (ti + 1) * 128, :], writes=[Bhnt[hi_]])

        def loads_ac(g):
            t0 = g * 512
            S.dma("sp", sga[:], Dm["sigg"][0:D, t0:t0 + 512].rearrange("(j p) t -> p j t", p=128), writes=[Bsga])
            S.dma("sp", sgm[:], Dm["sigg"][D:2 * D, t0:t0 + 512].rearrange("(j p) t -> p j t", p=128), writes=[Bsgm])

        def stage_a(g):
            t0 = g * 512
            for dm in range(8):
                ps, Bps = K.nextF()
                for gq in range(4):
                    S.pe(lambda e, gq=gq: e.matmul(ps[:, :], lhsT=wpo[:, gq, dm * 128:(dm + 1) * 128], rhs=aT[gq][:, t0:t0 + 512],
                                                   start=(gq == 0), stop=(gq == 3)), reads=[Bw5, BaT[gq]], writes=[Bps])
                S.dve(lambda e: e.tensor_tensor(out=mixA[:, dm, :], in0=ps[:, :], in1=sga[:, dm, :], op=ALU.mult), reads=[Bps, Bsga], writes=[BmixA])

        def stage_b(g):
            mT, BmT = mTb[g % 2], BmTb[g % 2]
            for ti in range(4):
                hi_ = (g % 2) * 4 + ti
                pt, Bpt = K.nextB()
                for j in range(8):
                    S.pe(lambda e, j=j: e.transpose(out=pt[:, j * 128:(j + 1) * 128], in_=hnt[hi_][:, j * 128:(j + 1) * 128], identity=K.identb[:]),
                         reads=[Bhnt[hi_], K.Bcb], writes=[Bpt])
                S.dve(lambda e: e.tensor_tensor(out=mT[:, :, ti * 128:(ti + 1) * 128], in0=pt[:, :].rearrange("p (j t) -> p j t", t=128),
                                                in1=sgo[g % 2][:, :, ti * 128:(ti + 1) * 128], op=ALU.mult),
                      reads=[Bpt, Bsgo[g % 2]], writes=[BmT])

        def stage_c(g):
            mT, BmT = mTb[g % 2], BmTb[g % 2]
            for dm in range(8):
                ps, Bps = K.nextF()
                for j in range(8):
                    S.pe(lambda e, j=j: e.matmul(ps[:, :], lhsT=wmo[:, j, dm * 128:(dm + 1) * 128], rhs=mT[:, j, :], start=(j == 0), stop=(j == 7)),
                         reads=[Bw5, BmT], writes=[Bps])
                tm, Btm = tmpm[dm % 2], Btmpm[dm % 2]
                S.dve(lambda e: e.tensor_tensor(out=tm[:], in0=ps[:, :], in1=sgm[:, dm, :], op=ALU.mult), reads=[Bps, Bsgm], writes=[Btm])
                S.dve(lambda e: e.tensor_tensor(out=mixT[:, dm, :], in0=tm[:], in1=mixA[:, dm, :], op=ALU.add), reads=[Btm, BmixA], writes=[BmixT])
            if g == 1:
                K.dump("mixA", mixA[:], [BmixA]); K.dump("mT", mT[:], [BmT]); K.dump("mixT", mixT[:], [BmixT])

        def stage_d(g):
            for ti in range(4):
                tile = g * 4 + ti
                xi = tile % 2
                S.dma("sp", xt[xi][:], I["x"][tile * 128:(tile + 1) * 128, :], writes=[Bxt[xi]])
                for half in range(2):
                    ps, Bps = K.nextF()
                    for j in range(8):
                        S.pe(lambda e, j=j: e.matmul(ps[:, :], lhsT=mixT[:, j, ti * 128:(ti + 1) * 128], rhs=wout[:, j, half * 512:(half + 1) * 512],
                                                     start=(j == 0), stop=(j == 7)), reads=[Bw5, BmixT], writes=[Bps])
                    S.dve(lambda e: e.tensor_tensor(out=x1t[xi][:, half * 512:(half + 1) * 512], in0=ps[:, :], in1=xt[xi][:, half * 512:(half + 1) * 512],
                                                    op=ALU.add), reads=[Bps, Bxt[xi]], writes=[Bx1[xi]])
                S.act(lambda e: e.activation(out=junk[:], in_=x1t[xi][:], func=AF.Square, accum_out=ssq[:, tile:tile + 1]),
                      reads=[Bx1[xi]], writes=[Bssq, Bjunk])
                S.dma("pool", Dm["x1"][tile * 128:(tile + 1) * 128, :], x1t[xi][:], reads=[Bx1[xi]])

        loads_b(0); loads_ac(0)
        stage_b(0)
        for g in range(8):
            if g + 1 < 8:
                loads_b(g + 1)
            stage_a(g)
            stage_c(g)
            if g + 1 < 8:
                stage_b(g + 1)
            stage_d(g)
            if g + 1 < 8:
                loads_ac(g + 1)
        S.dve(lambda e: e.tensor_scalar(out=ssq[:], in0=ssq[:], scalar1=1.0 / D, scalar2=EPS, op0=ALU.mult, op1=ALU.add), reads=[Bssq], writes=[Bssq])
        S.act(lambda e: e.activation(out=ssq[:], in_=ssq[:], func=AF.Sqrt), reads=[Bssq], writes=[Bssq])
        S.dve(lambda e: e.reciprocal(out=K.rstd2[:], in_=ssq[:]), reads=[Bssq], writes=[K.Brstd2])
        S.fence()


def phase6(K):
    nc, S, I, Dm = K.nc, K.S, K.I, K.Dm
    cst, Bcst = K.cst, K.Bcst
    identf = cst[:, C_ID:C_ID + 128]
    with contextlib.ExitStack() as es:
        def sb(name, shape, dt):
            return es.enter_context(nc.sbuf_tensor(name, list(shape), dt))
        aff = sb("aff", [128, NT, NE], F32); Baff = Buf("aff")
        L = sb("L6", [128, NT, NE, 5], BF16); BL = Buf("L6")
        cm1 = sb("cm1", [128, NT, NE], F32); Bcm1 = Buf("cm1")
        zt = sb("zt", [128, D], F32); Bzt = Buf("zt")
        Byacc = Buf("yacc")
        S.dve(lambda e: e.memset(zt[:], 0.0), writes=[Bzt])
        for r in range(33):
            S.dma("pool", Dm["yacc"][r * 128:(r + 1) * 128, :], zt[:], reads=[Bzt])
        S.dma("pool", Dm["h2"][T:T + 128, :], zt[:].bitcast(BF16)[:, 0:D], reads=[Bzt])
        with contextlib.ExitStack() as es2:
            def sb2(name, shape, dt):
                return es2.enter_context(nc.sbuf_tensor(name, list(shape), dt))
            bc = sb2("bc6", [128, 2, D], F32); Bbc = Buf("bc6")
            S.dma("sp", bc[:, 0, :], Dm["bvec"][1:2, :].partition_broadcast(128), writes=[Bbc])
            S.dma("sp", bc[:, 1, :], Dm["bvec"][2:3, :].partition_broadcast(128), writes=[Bbc])
            wr = sb2("wr", [128, 8, NE], BF16); Bwr = Buf("wr")
            S.dma("pool", wr[:], I["w_router"].rearrange("(j p) n -> p j n", p=128), writes=[Bwr])
            Apad = sb2("Apad", [128, NT, 128], F32); BApad = Buf("Apad")
            S.dve(lambda e: e.memset(Apad[:], 0.0), writes=[BApad])
            x1t = [sb2("x6_%d" % i, [128, D], F32) for i in range(2)]; Bx1 = [Buf("x6") for i in range(2)]
            hf = [sb2("hf6_%d" % i, [128, D], F32) for i in range(2)]; Bhf = [Buf("hf6") for i in range(2)]
            hbt = [sb2("hb6_%d" % i, [128, D], BF16) for i in range(2)]; Bhbt = [Buf("hb6") for i in range(2)]
            h2T = [sb2("h2T_%d" % i, [128, D], BF16) for i in range(2)]; Bh2T = [Buf("h2T") for i in range(2)]
            sm = [sb2("sm6_%d" % i, [128, 4], F32) for i in range(4)]; Bsm = [Buf("sm6") for i in range(4)]
            ex = [sb2("ex6_%d" % i, [128, NE], F32) for i in range(4)]; Bex = [Buf("ex6") for i in range(4)]
            def s1(tile):
                b = tile % 2
                S.dma("sp", x1t[b][:], Dm["x1"][tile * 128:(tile + 1) * 128, :], writes=[Bx1[b]])
                S.dve(lambda e: e.scalar_tensor_tensor(out=hf[b][:], in0=x1t[b][:], scalar=K.rstd2[:, tile:tile + 1], in1=bc[:, 0, :],
                                                       op0=ALU.mult, op1=ALU.mult), reads=[Bx1[b], K.Brstd2, Bbc], writes=[Bhf[b]])
                S.dve(lambda e: e.tensor_tensor(out=hbt[b][:], in0=hf[b][:], in1=bc[:, 1, :], op=ALU.add), reads=[Bhf[b], Bbc], writes=[Bhbt[b]])
                S.dma("pool", Dm["h2"][tile * 128:(tile + 1) * 128, :], hbt[b][:], reads=[Bhbt[b]])
                pt, Bpt = K.nextB()
                for j in range(8):
                    S.pe(lambda e, j=j: e.transpose(out=pt[:, j * 128:(j + 1) * 128], in_=hbt[b][:, j * 128:(j + 1) * 128], identity=K.identb[:]),
                         reads=[Bhbt[b], K.Bcb], writes=[Bpt])
                S.act(lambda e: e.activation(out=h2T[b][:], in_=pt[:], func=AF.Copy), reads=[Bpt], writes=[Bh2T[b]])

            def s2a(tile):
                b = tile % 2; b4 = tile % 4
                ps, Bps = K.nextF()
                for j in range(8):
                    S.pe(lambda e, j=j: e.matmul(ps[:, 0:NE], lhsT=h2T[b][:, j * 128:(j + 1) * 128], rhs=wr[:, j, :], start=(j == 0), stop=(j == 7)),
                         reads=[Bh2T[b], Bwr], writes=[Bps])
                S.dve(lambda e: e.reduce_max(out=sm[b4][:, 0:1], in_=ps[:, 0:NE], axis=AX.X), reads=[Bps], writes=[Bsm[b4]])
                S.dve(lambda e: e.memset(sm[b4][:, 2:3], 0.0), writes=[Bsm[b4]])
                S.dve(lambda e: e.tensor_scalar(out=sm[b4][:, 1:2], in0=sm[b4][:, 0:1], scalar1=-1.0, scalar2=None, op0=ALU.mult),
                      reads=[Bsm[b4]], writes=[Bsm[b4]])
                S.act(lambda e: e.activation(out=ex[b4][:], in_=ps[:, 0:NE], func=AF.Exp, bias=sm[b4][:, 1:2], accum_out=sm[b4][:, 2:3]),
                      reads=[Bps, Bsm[b4]], writes=[Bex[b4], Bsm[b4]])

            def s2b(tile):
                b4 = tile % 4
                seg = tile // 4
                S.dve(lambda e: e.reciprocal(out=sm[b4][:, 3:4], in_=sm[b4][:, 2:3]), reads=[Bsm[b4]], writes=[Bsm[b4]])
                S.dve(lambda e: e.tensor_scalar(out=aff[:, tile, :], in0=ex[b4][:], scalar1=sm[b4][:, 3:4], scalar2=None, op0=ALU.mult),
                      reads=[Bex[b4], Bsm[b4]], writes=[Baff])
                S.dve(lambda e: e.tensor_scalar(out=Apad[:, tile, seg * 16:(seg + 1) * 16], in0=ex[b4][:], scalar1=sm[b4][:, 3:4], scalar2=None, op0=ALU.mult),
                      reads=[Bex[b4], Bsm[b4]], writes=[BApad])

            s1(0)
            for tile in range(NT + 1):
                if tile + 1 < NT:
                    s1(tile + 1)
                if tile < NT:
                    s2a(tile)
                if tile >= 1:
                    s2b(tile - 1)
            affE = sb2("affE", [128, 512], F32); BaffE = Buf("affE")
            pa, Bpa = K.nextF()
            for q in range(4):
                for seg in range(8):
                    S.pe(lambda e, q=q, seg=seg: e.matmul(pa[:, q * 128:(q + 1) * 128], lhsT=Apad[:, seg * 4 + q, :], rhs=identf,
                                                          start=(seg == 0), stop=(seg == 7)), reads=[BApad, Bcst], writes=[Bpa])
            S.dve(lambda e: e.tensor_copy(out=affE[:], in_=pa[:]), reads=[Bpa], writes=[BaffE])
            lo = sb2("lo6", [128, 1], F32); Blo = Buf("lo6")
            mid = sb2("mid6", [128, 1], F32); Bmid = Buf("mid6")
            cmpt = sb2("cmp6", [128, 512], F32); Bcmp = Buf("cmp6")
            cnt = sb2("cnt6", [128, 1], F32); Bcnt = Buf("cnt6")
            ge = sb2("ge6", [128, 1], F32); Bge = Buf("ge6")
            S.dve(lambda e: e.memset(lo[:], 0.0), writes=[Blo])
            cmp3 = [sb2("cmp6_%d" % k, [128, 512], F32) for k in range(3)]; Bcmp3 = [Buf("cmp6") for k in range(3)]
            cnt3 = sb2("cnt6_3", [128, 3], F32); Bcnt3 = Buf("cnt6_3")
            ge3 = sb2("ge6_3", [128, 3], F32); Bge3 = Buf("ge6_3")
            w = 1.0
            for it in range(10):
                w *= 0.25
                for k in range(3):
                    S.dve(lambda e, w=w, k=k: e.tensor_scalar(out=cmp3[k][:], in0=affE[:], scalar1=lo[:, 0:1], scalar2=w * (k + 1), op0=ALU.subtract, op1=ALU.is_ge),
                          reads=[BaffE, Blo], writes=[Bcmp3[k]])
                for k in range(3):
                    S.dve(lambda e, k=k: e.reduce_sum(out=cnt3[:, k:k + 1], in_=cmp3[k][:], axis=AX.X), reads=[Bcmp3[k]], writes=[Bcnt3])
                pc, Bpc = K.nextF()
                S.pe(lambda e: e.matmul(pc[:, 0:3], lhsT=cst[:, C_MSUM:C_MSUM + 128], rhs=cnt3[:], start=True, stop=True), reads=[Bcnt3, Bcst], writes=[Bpc])
                S.dve(lambda e, w=w: e.tensor_scalar(out=ge3[:], in0=pc[:, 0:3], scalar1=float(CAP) - 0.5, scalar2=w, op0=ALU.is_ge, op1=ALU.mult),
                      reads=[Bpc], writes=[Bge3])
                S.dve(lambda e: e.reduce_sum(out=ge[:], in_=ge3[:], axis=AX.X), reads=[Bge3], writes=[Bge])
                S.dve(lambda e: e.tensor_tensor(out=lo[:], in0=lo[:], in1=ge[:], op=ALU.add), reads=[Blo, Bge], writes=[Blo])
            rhsd = sb2("rhsd", [128, NE], F32); Brhsd = Buf("rhsd")
            thrb = sb2("thrb", [128, NE], F32); Bthrb = Buf("thrb")
            S.dve(lambda e: e.tensor_scalar(out=rhsd[:], in0=cst[:, C_ID:C_ID + NE], scalar1=lo[:, 0:1], scalar2=None, op0=ALU.mult),
                  reads=[Blo, Bcst], writes=[Brhsd])
            pb_, Bpb_ = K.nextF()
            S.pe(lambda e: e.matmul(pb_[:, 0:NE], lhsT=cst[:, C_ONES:C_ONES + 128], rhs=rhsd[:], start=True, stop=True), reads=[Brhsd, Bcst], writes=[Bpb_])
            S.dve(lambda e: e.tensor_copy(out=thrb[:], in_=pb_[:, 0:NE]), reads=[Bpb_], writes=[Bthrb])
            mk = sb2("mk6", [128, NT, NE], F32); Bmk = Buf("mk6")
            mkb = sb2("mkb6", [128, NT * NE], BF16); Bmkb = Buf("mkb6")
            for i in range(NT):
                S.dve(lambda e, i=i: e.tensor_tensor(out=mk[:, i, :], in0=aff[:, i, :], in1=thrb[:], op=ALU.is_ge), reads=[Baff, Bthrb], writes=[Bmk])
            S.dve(lambda e: e.tensor_copy(out=mkb[:], in_=mk[:].rearrange("p i e -> p (i e)")), reads=[Bmk], writes=[Bmkb])
            pw, Bpw = K.nextF()
            S.pe(lambda e: e.matmul(pw[:, :], lhsT=K.trib[:], rhs=mkb[:], start=True, stop=True), reads=[Bmkb, K.Bcb], writes=[Bpw])
            ptot, Bptot = K.nextF()
            S.pe(lambda e: e.matmul(ptot[:, :], lhsT=K.onesb[:], rhs=mkb[:], start=True, stop=True), reads=[Bmkb, K.Bcb], writes=[Bptot])
            tot = sb2("tot6", [128, NT, NE], F32); Btot = Buf("tot6")
            H = [sb2("H6_%d" % i, [128, NT, NE], F32) for i in range(2)]; BH = [Buf("H6") for i in range(2)]
            S.dve(lambda e: e.tensor_copy(out=tot[:].rearrange("p i e -> p (i e)"), in_=ptot[:, :]), reads=[Bptot], writes=[Btot])
            S.dve(lambda e: e.tensor_copy(out=H[0][:], in_=tot[:]), reads=[Btot], writes=[BH[0]])
            cur = 0
            for sh in (1, 2, 4, 8, 16):
                nx = 1 - cur
                S.dve(lambda e, sh=sh, cur=cur, nx=nx: e.tensor_tensor(out=H[nx][:, sh:, :], in0=H[cur][:, sh:, :], in1=H[cur][:, 0:NT - sh, :], op=ALU.add),
                      reads=[BH[cur]], writes=[BH[nx]])
                S.dve(lambda e, sh=sh, cur=cur, nx=nx: e.tensor_copy(out=H[nx][:, 0:sh, :], in_=H[cur][:, 0:sh, :]), reads=[BH[cur]], writes=[BH[nx]])
                cur = nx
            S.dve(lambda e: e.tensor_tensor(out=cm1[:].rearrange("p i e -> p (i e)"), in0=pw[:, :], in1=H[cur][:].rearrange("p i e -> p (i e)"), op=ALU.add),
                  reads=[Bpw, BH[cur]], writes=[Bcm1])
            S.dve(lambda e: e.scalar_tensor_tensor(out=cm1[:], in0=cm1[:], scalar=-1.0, in1=tot[:], op0=ALU.add, op1=ALU.subtract),
                  reads=[Bcm1, Btot], writes=[Bcm1])
            am = sb2("am6", [128, NT, NE], F32); Bam = Buf("am6")
            S.dve(lambda e: e.tensor_scalar(out=L[:, :, :, 0], in0=mk[:], scalar1=cst[:, C_IOP:C_IOP + 1], scalar2=None, op0=ALU.mult),
                  reads=[Bmk, Bcst], writes=[BL])
            S.dve(lambda e: e.tensor_tensor(out=L[:, :, :, 1], in0=mk[:], in1=cst[:, C_TIDX:C_TIDX + 512].rearrange("p (i e) -> p i e", e=NE), op=ALU.mult),
                  reads=[Bmk, Bcst], writes=[BL])
            S.dve(lambda e: e.tensor_tensor(out=am[:], in0=mk[:], in1=aff[:], op=ALU.mult), reads=[Bmk, Baff], writes=[Bam])
            S.dve(lambda e: e.tensor_copy(out=L[:, :, :, 2], in_=am[:]), reads=[Bam], writes=[BL])
            S.dve(lambda e: e.tensor_tensor(out=L[:, :, :, 3], in0=am[:], in1=L[:, :, :, 2], op=ALU.subtract), reads=[Bam, BL], writes=[BL])
            S.dve(lambda e: e.tensor_copy(out=L[:, :, :, 4], in_=mk[:]), reads=[Bmk], writes=[BL])
            K.dump("aff", aff[:], [Baff]); K.dump("cm1", cm1[:], [Bcm1]); K.dump("mk", mk[:], [Bmk]); K.dump("thrb", thrb[:], [Bthrb])
            S.fence()
        bc2 = sb("bc7", [128, 2, D], F32); Bbc2 = Buf("bc7")
        es3 = contextlib.ExitStack()

        def sb3(name, shape, dt):
            return es3.enter_context(nc.sbuf_tensor(name, list(shape), dt))
        S.dma("sp", bc2[:, 0, :], Dm["bvec"][3:4, :].partition_broadcast(128), writes=[Bbc2])
        S.dma("sp", bc2[:, 1, :], I["final_g"].rearrange("(o n) -> o n", o=1).partition_broadcast(128), writes=[Bbc2])
        wg = [sb3("wg%d" % i, [128, 8, D], BF16) for i in range(2)]
        wu = [sb3("wu%d" % i, [128, 8, D], BF16) for i in range(2)]
        wd = [sb3("wd%d" % i, [128, 8, D], BF16) for i in range(2)]
        Bwg = [Buf("wg") for i in range(2)]; Bwu = [Buf("wu") for i in range(2)]; Bwd = [Buf("wd") for i in range(2)]
        NEQ = 8
        Eq = [sb3("Eq%d" % i, [128, 512], BF16) for i in range(NEQ)]; BEq = [Buf("Eq") for i in range(NEQ)]
        o5 = sb3("o5", [5, 512], F32); Bo5 = Buf("o5")
        sl = [sb3("sl%d" % i, [128, 4, 5], F32) for i in range(4)]; Bsl = [Buf("sl") for i in range(4)]
        idf = [sb3("idf%d" % i, [128, 4], F32) for i in range(4)]; Bidf = [Buf("idf") for i in range(4)]
        idx = [sb3("idx%d" % i, [128, 4], I32) for i in range(4)]; Bidx = [Buf("idx") for i in range(4)]
        afs = [sb3("afs%d" % i, [128, 4], F32) for i in range(4)]; Bafs = [Buf("afs") for i in range(4)]
        xe = [sb3("xe%d" % i, [128, 4, D], BF16) for i in range(3)]; Bxe = [[Buf("xe") for blk in range(4)] for i in range(3)]
        xeT2 = [sb3("xeT%d" % i, [128, 8, 512], BF16) for i in range(2)]; BxeT2 = [Buf("xeT") for i in range(2)]
        hid = sb3("hid", [128, 8, 512], BF16); Bhid = Buf("hid")
        sgt = [sb3("sgt%d" % i, [128, 512], F32) for i in range(2)]; Bsgt = [Buf("sgt") for i in range(2)]
        ye = [sb3("ye%d" % i, [128, D], F32) for i in range(3)]; Bye = [Buf("ye") for i in range(3)]
        ps, Bps = K.ps_all, K.Bps_all
        p5, Bp5 = ps[7], Bps[7]
        ptb, Bptb = ps[6][:].bitcast(BF16), Bps[6]
        st6 = {"eq": 0, "ye": 0, "scat_prev": [], "scat_cur": []}

        def load_gu(e_):
            b = e_ % 2
            S.dma("pool", wg[b][:], I["w_gate"][e_].rearrange("(j p) n -> p j n", p=128), writes=[Bwg[b]])
            S.dma("pool", wu[b][:], I["w_up"][e_].rearrange("(j p) n -> p j n", p=128), writes=[Bwu[b]])

        def load_d(e_):
            b = e_ % 2
            S.dma("pool", wd[b][:], I["w_down"][e_].rearrange("(j p) n -> p j n", p=128), writes=[Bwd[b]])

        eqslot = {}

        def idx_eq(e_, i_lo, i_hi):
            for i in range(i_lo, i_hi):
                q = st6["eq"] % NEQ; st6["eq"] += 1
                eqslot[(e_, i)] = q
                S.dve(lambda e, i=i, q=q: e.tensor_scalar(out=Eq[q][:], in0=cst[:, C_IOJ:C_IOJ + 512], scalar1=cm1[:, i, e_:e_ + 1], scalar2=None,
                                                          op0=ALU.is_equal), reads=[Bcm1, Bcst], writes=[BEq[q]])

        def idx_mm(e_, i_lo, i_hi):
            for i in range(i_lo, i_hi):
                q = eqslot[(e_, i)]
                S.pe(lambda e, i=i, q=q: e.matmul(p5[0:5, :], lhsT=L[:, i, e_, :], rhs=Eq[q][:], start=(i == 0), stop=(i == NT - 1)),
                     reads=[BL, BEq[q]], writes=[Bp5])

        def idx_part(e_, i_lo, i_hi):
            for i in range(i_lo, i_hi, 4):
                idx_eq(e_, i, i + 4)
                idx_mm(e_, i, i + 4)

        def idx_tail(e_):
            b = e_ % 4
            S.dve(lambda e: e.tensor_copy(out=o5[:], in_=p5[0:5, :]), reads=[Bp5], writes=[Bo5])
            pt5, Bpt5 = K.nextF()
            for blk in range(4):
                S.pe(lambda e, blk=blk: e.transpose(out=pt5[:, blk * 5:(blk + 1) * 5], in_=o5[0:5, blk * 128:(blk + 1) * 128], identity=cst[0:5, C_ID:C_ID + 5]),
                     reads=[Bo5, Bcst], writes=[Bpt5])
            S.dve(lambda e: e.tensor_copy(out=sl[b][:], in_=pt5[:, 0:20].rearrange("p (k c) -> p k c", c=5)), reads=[Bpt5], writes=[Bsl[b]])
            S.dve(lambda e: e.scalar_tensor_tensor(out=idf[b][:], in0=sl[b][:, :, 1], scalar=128.0, in1=sl[b][:, :, 0], op0=ALU.mult, op1=ALU.add),
                  reads=[Bsl[b]], writes=[Bidf[b]])
            S.dve(lambda e: e.tensor_scalar(out=afs[b][:], in0=sl[b][:, :, 4], scalar1=-float(T), scalar2=float(T), op0=ALU.mult, op1=ALU.add),
                  reads=[Bsl[b]], writes=[Bafs[b]])
            S.dve(lambda e: e.tensor_tensor(out=idf[b][:], in0=idf[b][:], in1=afs[b][:], op=ALU.add), reads=[Bidf[b], Bafs[b]], writes=[Bidf[b]])
            S.dve(lambda e: e.tensor_copy(out=idx[b][:], in_=idf[b][:]), reads=[Bidf[b]], writes=[Bidx[b]])
            S.dve(lambda e: e.tensor_tensor(out=afs[b][:], in0=sl[b][:, :, 2], in1=sl[b][:, :, 3], op=ALU.add), reads=[Bsl[b]], writes=[Bafs[b]])
            if e_ == 0:
                K.dump("idx0", idx[b][:], [Bidx[b]]); K.dump("afs0", afs[b][:], [Bafs[b]])

        def gather(e_):
            b = e_ % 4
            for blk in range(4):
                S.op("pool", lambda e, blk=blk: e.indirect_dma_start(out=xe[e_ % 3][:, blk, :], out_offset=None, in_=Dm["h2"],
                                                                       in_offset=bass.IndirectOffsetOnAxis(ap=idx[b][:, blk:blk + 1], axis=0)),
                     reads=[Bidx[b]], writes=[Bxe[e_ % 3][blk]], dma=True)

        def transpose_blk(e_, blk):
            xt_, Bxt_ = xeT2[e_ % 2], BxeT2[e_ % 2]
            for j in range(8):
                S.pe(lambda e, j=j: e.transpose(out=ptb[:, j * 128:(j + 1) * 128], in_=xe[e_ % 3][:, blk, j * 128:(j + 1) * 128], identity=K.identb[:]),
                     reads=[Bxe[e_ % 3][blk], K.Bcb], writes=[Bptb])
            S.act(lambda e: e.activation(out=xt_[:, :, blk * 128:(blk + 1) * 128], in_=ptb[:, :].rearrange("p (j t) -> p j t", t=128), func=AF.Copy),
                  reads=[Bptb], writes=[Bxt_])

        def ffn(e_):
            b = e_ % 2
            b3 = e_ % 4
            xeT, BxeT = xeT2[e_ % 2], BxeT2[e_ % 2]
            if e_ + 3 < NE:
                idx_eq(e_ + 3, 0, 4)
            for f in range(8):
                if e_ + 3 < NE:
                    if f < 7:
                        idx_eq(e_ + 3, 4 * (f + 1), 4 * (f + 1) + 4)
                    idx_mm(e_ + 3, 4 * f, 4 * f + 4)
                if e_ + 1 < NE and f % 2 == 1:
                    transpose_blk(e_ + 1, f // 2)
                pg, Bpg = K.nextF()
                for j in range(8):
                    S.pe(lambda e, j=j: e.matmul(pg[:, :], lhsT=wg[b][:, j, f * 128:(f + 1) * 128], rhs=xeT[:, j, :], start=(j == 0), stop=(j == 7)),
                         reads=[Bwg[b], BxeT], writes=[Bpg])
                pu, Bpu = K.nextF()
                for j in range(8):
                    S.pe(lambda e, j=j: e.matmul(pu[:, :], lhsT=wu[b][:, j, f * 128:(f + 1) * 128], rhs=xeT[:, j, :], start=(j == 0), stop=(j == 7)),
                         reads=[Bwu[b], BxeT], writes=[Bpu])
                s_ = f % 2
                S.act(lambda e: e.activation(out=sgt[s_][:], in_=pg[:, :], func=AF.Silu), reads=[Bpg], writes=[Bsgt[s_]])
                S.dve(lambda e: e.tensor_tensor(out=hid[:, f, :], in0=sgt[s_][:], in1=pu[:, :], op=ALU.mult), reads=[Bsgt[s_], Bpu], writes=[Bhid])
            st6["scat_cur"] = []
            for blk in range(4):
                y_ = st6["ye"] % 3; st6["ye"] += 1
                for half in range(2):
                    pd, Bpd = K.nextF()
                    for f in range(8):
                        S.pe(lambda e, f=f: e.matmul(pd[:, :], lhsT=hid[:, f, blk * 128:(blk + 1) * 128], rhs=wd[b][:, f, half * 512:(half + 1) * 512],
                                                     start=(f == 0), stop=(f == 7)), reads=[Bwd[b], Bhid], writes=[Bpd])
                    S.act(lambda e: e.activation(out=ye[y_][:, half * 512:(half + 1) * 512], in_=pd[:, :], func=AF.Copy, scale=afs[b3][:, blk:blk + 1]),
                          reads=[Bpd, Bafs[b3]], writes=[Bye[y_]])
                rec = S.op("pool", lambda e, blk=blk: e.indirect_dma_start(out=Dm["yacc"], out_offset=bass.IndirectOffsetOnAxis(ap=idx[b3][:, blk:blk + 1], axis=0),
                                                                             in_=ye[y_][:], in_offset=None, compute_op=ALU.add),
                           reads=[Bye[y_], Bidx[b3]], dma=True, after=st6["scat_prev"])
                st6["scat_cur"].append(rec)
            st6["scat_prev"] = st6["scat_cur"]
            if e_ + 2 < NE:
                load_gu(e_ + 2)
                load_d(e_ + 2)

        idx_part(0, 0, NT); idx_tail(0); gather(0)
        load_gu(0); load_d(0)
        idx_part(1, 0, NT); idx_tail(1); gather(1)
        load_gu(1); load_d(1)
        idx_part(2, 0, NT); idx_tail(2); gather(2)
        for blk in range(4):
            transpose_blk(0, blk)
        for e_ in range(NE):
            ffn(e_)
            if e_ + 3 < NE:
                idx_tail(e_ + 3)
                gather(e_ + 3)
        S.fence()
        es3.close()
        NYB = 6
        ya = [sb("ya%d" % i, [128, D], F32) for i in range(NYB)]; Bya = [Buf("ya") for i in range(NYB)]
        ot = [sb("ot%d" % i, [128, D], F32) for i in range(NYB)]; Bot = [Buf("ot") for i in range(NYB)]
        junk = sb("junk6", [128, D], F32); Bjunk = Buf("junk6", sync_all=True)
        sqall = sb("sqall", [128, NT], F32); Bsqall = Buf("sqall")
        rsall = sb("rsall", [128, NT], F32); Brsall = Buf("rsall")
        S.dve(lambda e: e.memset(sqall[:], 0.0), writes=[Bsqall])
        NXB = 8
        xa = [sb("xa6_%d" % i, [128, D], F32) for i in range(NXB)]; Bxa = [Buf("xa") for i in range(NXB)]

        def d1(tile):
            b = tile % NYB; bx = tile % NXB
            S.dma("sp", xa[bx][:], Dm["x1"][tile * 128:(tile + 1) * 128, :], writes=[Bxa[bx]])
            S.dma("sp", ya[b][:], Dm["yacc"][tile * 128:(tile + 1) * 128, :], writes=[Bya[b]])
            S.dve(lambda e: e.tensor_tensor(out=ya[b][:], in0=ya[b][:], in1=bc2[:, 0, :], op=ALU.mult), reads=[Bya[b], Bbc2], writes=[Bya[b]])
            S.dve(lambda e: e.tensor_tensor(out=xa[bx][:], in0=xa[bx][:], in1=ya[b][:], op=ALU.add), reads=[Bxa[bx], Bya[b]], writes=[Bxa[bx]])
            S.act(lambda e: e.activation(out=junk[:], in_=xa[bx][:], func=AF.Square, accum_out=sqall[:, tile:tile + 1]), reads=[Bxa[bx]], writes=[Bsqall, Bjunk])

        def d2(tile_lo, tile_hi):
            sl_ = slice(tile_lo, tile_hi)
            S.dve(lambda e: e.tensor_scalar(out=rsall[:, sl_], in0=sqall[:, sl_], scalar1=1.0 / D, scalar2=EPS, op0=ALU.mult, op1=ALU.add),
                  reads=[Bsqall], writes=[Brsall])
            S.act(lambda e: e.activation(out=rsall[:, sl_], in_=rsall[:, sl_], func=AF.Sqrt), reads=[Brsall], writes=[Brsall])
            S.dve(lambda e: e.reciprocal(out=rsall[:, sl_], in_=rsall[:, sl_]), reads=[Brsall], writes=[Brsall])

        def d3(tile):
            b = tile % NYB; bx = tile % NXB
            S.dve(lambda e: e.scalar_tensor_tensor(out=ot[b][:], in0=xa[bx][:], scalar=rsall[:, tile:tile + 1], in1=bc2[:, 1, :], op0=ALU.mult, op1=ALU.mult),
                  reads=[Bxa[bx], Brsall, Bbc2], writes=[Bot[b]])
            S.dma("pool", K.out_d[tile * 128:(tile + 1) * 128, :], ot[b][:], reads=[Bot[b]])

        d1(0); d1(1); d1(2); d1(3)
        for g in range(NT // 2):
            if g + 2 < NT // 2:
                d1(2 * g + 4); d1(2 * g + 5)
            d2(2 * g, 2 * g + 2)
            d3(2 * g); d3(2 * g + 1)
        S.fence()


_W_KEYS = ["w_mod", "b_mod", "norm1_g", "norm2_g", "w_in", "conv_w", "conv_b", "b_if", "pool_mix", "pool_scale", "mlstm_norm_g",
           "w_pool_out", "w_mlstm_out", "w_out", "w_router", "w_gate", "w_up", "w_down"]


def kernel(**inputs):
    B = inputs["x"].shape[0]
    nc = build()
    shared = {k: np.ascontiguousarray(np.asarray(inputs[k], dtype=np.float32)[0]) for k in _W_KEYS}
    shared["final_g"] = np.ascontiguousarray(np.asarray(inputs["final_g"], dtype=np.float32))
    shared["c_ctx"] = np.ascontiguousarray(np.asarray(inputs["c_ctx"], dtype=np.float32))
    shared["consts"] = make_consts()
    shared["invcnt"] = make_invcnt()
    in_maps = []
    for b in range(B):
        m = dict(shared)
        m["x"] = np.ascontiguousarray(np.asarray(inputs["x"], dtype=np.float32)[b])
        m["c"] = np.ascontiguousarray(np.asarray(inputs["c"], dtype=np.float32)[b])
        m["ctx"] = np.ascontiguousarray(np.asarray(inputs["ctx"], dtype=np.float32)[b])
        in_maps.append(m)
    res = run_bass_kernel_spmd(nc, in_maps, core_ids=list(range(B)))
    return np.stack([np.asarray(r["out"], dtype=np.float32) for r in res.results], axis=0)
```
